# Optimizing a Trainium2 kernel written in Bass

```python
import math
import jax, jax.numpy as jnp
from jax import lax
import numpy as np

D_MODEL = 4096
BATCH = 1
SEQ = 8192
DEPTH = 1

N_MEM = 256
Q_BLOCK = 128
NORM_EPS = 1e-6
NEG_INF = -1e30

DIFF_HEADS = 6
DIFF_HEAD_DIM = 128
DIFF_V_DIM = 2 * DIFF_HEAD_DIM
DIFF_MAPS = 2 * DIFF_HEADS
DIFF_QK_WIDTH = DIFF_MAPS * DIFF_HEAD_DIM
DIFF_WIDTH = DIFF_HEADS * DIFF_V_DIM

MLA_HEADS = 12
MLA_Q_RANK = 1536
MLA_KV_RANK = 512
MLA_NOPE_DIM = 128
MLA_ROPE_DIM = 64
MLA_QK_DIM = MLA_NOPE_DIM + MLA_ROPE_DIM
MLA_V_DIM = 128
MLA_WIDTH = MLA_HEADS * MLA_V_DIM
ROPE_THETA = 10000.0

MEM_HEADS = 4
MEM_HEAD_DIM = 256
MEM_WIDTH = MEM_HEADS * MEM_HEAD_DIM

N_BRANCHES = 3

REL_BUCKETS = 32
REL_MAX_DIST = 128

N_GROUPS = 8
EXPERTS_PER_GROUP = 8
N_EXPERTS = N_GROUPS * EXPERTS_PER_GROUP
TOP_K = 2
EXPERT_FF = 512
MOE_BLOCK = 128

IN_SPLITS = (DIFF_QK_WIDTH, DIFF_QK_WIDTH, DIFF_WIDTH, MLA_Q_RANK, MLA_KV_RANK,
             MLA_ROPE_DIM, MEM_WIDTH, N_BRANCHES * D_MODEL)
IN_COLS = sum(IN_SPLITS)

kernel_name = "hybrid_diffattn_mla_memxattn_hiermoe"


def rms_norm(x, g):
    xf = x.astype(jnp.float32)
    y = xf * lax.rsqrt(jnp.mean(xf * xf, axis=-1, keepdims=True) + NORM_EPS)
    return (y * g.astype(jnp.float32)).astype(x.dtype)


def rotary(x, positions):
    half = x.shape[-1] // 2
    inv_freq = ROPE_THETA ** (-jnp.arange(half, dtype=jnp.float32) / half)
    ang = positions.astype(jnp.float32)[..., None] * inv_freq
    cos = jnp.cos(ang)[:, :, None, :]
    sin = jnp.sin(ang)[:, :, None, :]
    xf = x.astype(jnp.float32)
    x1, x2 = xf[..., :half], xf[..., half:]
    return jnp.concatenate([x1 * cos - x2 * sin, x2 * cos + x1 * sin], axis=-1).astype(x.dtype)


def t5_bucket(dist):
    n = jnp.maximum(dist, 0)
    max_exact = REL_BUCKETS // 2
    nf = jnp.maximum(n, 1).astype(jnp.float32)
    large = max_exact + (jnp.log(nf / max_exact) / math.log(REL_MAX_DIST / max_exact)
                         * (REL_BUCKETS - max_exact)).astype(jnp.int32)
    large = jnp.minimum(large, REL_BUCKETS - 1)
    return jnp.where(n < max_exact, n, large)


def t5_bias(pos_q, pos_k, table):
    bucket = t5_bucket(pos_q[:, :, None] - pos_k[:, None, :])
    return jnp.transpose(table.astype(jnp.float32)[bucket], (0, 3, 1, 2))


def to_blocks(t):
    b, h, s, d = t.shape
    return t.reshape(b, h, s // Q_BLOCK, Q_BLOCK, d).transpose(2, 0, 1, 3, 4)


def from_blocks(t):
    nb, b, h, l, d = t.shape
    return t.transpose(1, 0, 3, 2, 4).reshape(b, nb * l, h, d)


def pos_blocks(positions):
    b, s = positions.shape
    return positions.reshape(b, s // Q_BLOCK, Q_BLOCK).transpose(1, 0, 2)


def causal_softmax(s, pos_q, pos_k):
    mask = pos_k[:, None, None, :] <= pos_q[:, None, :, None]
    return jax.nn.softmax(jnp.where(mask, s, NEG_INF), axis=-1)


def diff_attention(q, k, v, positions, rel_bias, lam_q1, lam_k1, lam_q2, lam_k2,
                   q_g, k_g, subln_g, layer_idx):
    b, s, _ = q.shape
    q = rms_norm(q.reshape(b, s, DIFF_MAPS, DIFF_HEAD_DIM), q_g).transpose(0, 2, 1, 3)
    k = rms_norm(k.reshape(b, s, DIFF_MAPS, DIFF_HEAD_DIM), k_g).transpose(0, 2, 1, 3)
    v = v.reshape(b, s, DIFF_HEADS, DIFF_V_DIM).transpose(0, 2, 1, 3)
    lam_init = 0.8 - 0.6 * math.exp(-0.3 * layer_idx)
    lam = (jnp.exp(jnp.sum(lam_q1.astype(jnp.float32) * lam_k1.astype(jnp.float32)))
           - jnp.exp(jnp.sum(lam_q2.astype(jnp.float32) * lam_k2.astype(jnp.float32)))
           + lam_init)
    scale = DIFF_HEAD_DIM ** -0.5

    def block(args):
        qb, pb = args
        sc = jnp.einsum('bmqd,bmkd->bmqk', qb, k).astype(jnp.float32) * scale
        sc = sc + t5_bias(pb, positions, rel_bias)
        p = causal_softmax(sc, pb, positions).reshape(b, 2, DIFF_HEADS, Q_BLOCK, s)
        a = (p[:, 0] - lam * p[:, 1]).astype(v.dtype)
        return jnp.einsum('bhqk,bhkv->bhqv', a, v)

    o = from_blocks(lax.map(block, (to_blocks(q), pos_blocks(positions))))
    o = rms_norm(o, subln_g) * (1.0 - lam_init)
    return o.reshape(b, s, DIFF_WIDTH)


def latent_attention(c_q, c_kv, k_rope, positions, cq_g, ckv_g, w_uq, w_ukv, q_g, k_g):
    b, s, _ = c_q.shape
    q = (rms_norm(c_q, cq_g) @ w_uq).reshape(b, s, MLA_HEADS, MLA_QK_DIM)
    kv = (rms_norm(c_kv, ckv_g) @ w_ukv).reshape(b, s, MLA_HEADS, MLA_NOPE_DIM + MLA_V_DIM)
    k_nope, v = kv[..., :MLA_NOPE_DIM], kv[..., MLA_NOPE_DIM:]
    k = jnp.concatenate(
        [k_nope, jnp.broadcast_to(k_rope[:, :, None, :], (b, s, MLA_HEADS, MLA_ROPE_DIM))], axis=-1)
    q = rms_norm(q, q_g)
    k = rms_norm(k, k_g)
    q = jnp.concatenate([q[..., :MLA_NOPE_DIM], rotary(q[..., MLA_NOPE_DIM:], positions)], axis=-1)
    k = jnp.concatenate([k[..., :MLA_NOPE_DIM], rotary(k[..., MLA_NOPE_DIM:], positions)], axis=-1)
    q = q.transpose(0, 2, 1, 3)
    k = k.transpose(0, 2, 1, 3)
    v = v.transpose(0, 2, 1, 3)
    scale = MLA_QK_DIM ** -0.5

    def block(args):
        qb, pb = args
        sc = jnp.einsum('bhqd,bhkd->bhqk', qb, k).astype(jnp.float32) * scale
        p = causal_softmax(sc, pb, positions).astype(v.dtype)
        return jnp.einsum('bhqk,bhkv->bhqv', p, v)

    o = from_blocks(lax.map(block, (to_blocks(q), pos_blocks(positions))))
    return o.reshape(b, s, MLA_WIDTH)


def memory_attention(q, mem, mem_g, w_mem_kv, q_g, k_g):
    b, s, _ = q.shape
    n = mem.shape[1]
    q = rms_norm(q.reshape(b, s, MEM_HEADS, MEM_HEAD_DIM), q_g)
    kv = rms_norm(mem, mem_g) @ w_mem_kv
    k = rms_norm(kv[..., :MEM_WIDTH].reshape(b, n, MEM_HEADS, MEM_HEAD_DIM), k_g)
    v = kv[..., MEM_WIDTH:].reshape(b, n, MEM_HEADS, MEM_HEAD_DIM)
    sc = jnp.einsum('bshd,bnhd->bhsn', q, k).astype(jnp.float32) * (MEM_HEAD_DIM ** -0.5)
    p = jax.nn.softmax(sc, axis=-1).astype(v.dtype)
    return jnp.einsum('bhsn,bnhd->bshd', p, v).reshape(b, s, MEM_WIDTH)


def hierarchical_moe(h, w_rg, b_rg, w_re, b_re, w_gate, w_up, w_down):
    t, d = h.shape
    p_group = jax.nn.softmax((h @ w_rg).astype(jnp.float32) + b_rg.astype(jnp.float32), axis=-1)
    pg_top, g_idx = lax.top_k(p_group, 1)
    logits_e = ((h @ w_re).astype(jnp.float32) + b_re.astype(jnp.float32)).reshape(
        t, N_GROUPS, EXPERTS_PER_GROUP)
    logits_sel = logits_e[jnp.arange(t), g_idx[:, 0]]
    p_e = jax.nn.softmax(logits_sel, axis=-1)
    pe_top, e_idx = lax.top_k(p_e, TOP_K)
    gate = pg_top * pe_top / jnp.sum(pe_top, axis=-1, keepdims=True)
    expert = g_idx * EXPERTS_PER_GROUP + e_idx

    a = t * TOP_K
    flat_e = expert.reshape(a).astype(jnp.int32)
    flat_tok = jnp.repeat(jnp.arange(t, dtype=jnp.int32), TOP_K)
    flat_w = gate.reshape(a)
    order = jnp.argsort(flat_e)
    sorted_e = flat_e[order]
    counts = jnp.bincount(flat_e, length=N_EXPERTS)
    padded = (counts + MOE_BLOCK - 1) // MOE_BLOCK * MOE_BLOCK
    pad_end = jnp.cumsum(padded)
    pad_start = pad_end - padded
    grp_start = jnp.cumsum(counts) - counts
    dest = pad_start[sorted_e] + jnp.arange(a, dtype=jnp.int32) - grp_start[sorted_e]
    n_blocks = (a + MOE_BLOCK - 1) // MOE_BLOCK + N_EXPERTS
    p_rows = n_blocks * MOE_BLOCK
    buf_tok = jnp.zeros((p_rows,), jnp.int32).at[dest].set(flat_tok[order])
    buf_w = jnp.zeros((p_rows,), jnp.float32).at[dest].set(flat_w[order])
    block_e = jnp.minimum(
        jnp.searchsorted(pad_end, jnp.arange(n_blocks, dtype=pad_end.dtype) * MOE_BLOCK, side='right'),
        N_EXPERTS - 1)

    def expert_block(args):
        tok, e = args
        xb = h[tok]
        act = jax.nn.silu(xb @ w_gate[e]) * (xb @ w_up[e])
        return act @ w_down[e]

    yb = lax.map(expert_block, (buf_tok.reshape(n_blocks, MOE_BLOCK), block_e)).reshape(p_rows, d)
    yb = yb * buf_w[:, None].astype(yb.dtype)
    return jnp.zeros((t, d), h.dtype).at[buf_tok].add(yb)


def setup_inputs(seed: int = 0) -> dict:
    key = jax.random.key(seed)
    ks = iter(jax.random.split(key, 48))

    def nrm(shape, scale):
        return jax.random.normal(next(ks), shape, jnp.float32) * scale

    def gain(shape):
        return 1.0 + nrm(shape, 0.02)

    L, D = DEPTH, D_MODEL
    return {
        "x": nrm((BATCH, SEQ, D), 1.0),
        "mem": nrm((BATCH, N_MEM, D), 1.0),
        "positions": jnp.broadcast_to(jnp.arange(SEQ, dtype=jnp.int32), (BATCH, SEQ)),
        "rel_bias": nrm((REL_BUCKETS, DIFF_MAPS), 0.1),
        "mix_norm_g": gain((L, D)),
        "w_in": nrm((L, D, IN_COLS), D ** -0.5),
        "diff_q_norm_g": gain((L, DIFF_HEAD_DIM)),
        "diff_k_norm_g": gain((L, DIFF_HEAD_DIM)),
        "diff_lambda_q1": nrm((L, DIFF_HEAD_DIM), 0.1),
        "diff_lambda_k1": nrm((L, DIFF_HEAD_DIM), 0.1),
        "diff_lambda_q2": nrm((L, DIFF_HEAD_DIM), 0.1),
        "diff_lambda_k2": nrm((L, DIFF_HEAD_DIM), 0.1),
        "diff_subln_g": gain((L, DIFF_V_DIM)),
        "mla_cq_norm_g": gain((L, MLA_Q_RANK)),
        "mla_ckv_norm_g": gain((L, MLA_KV_RANK)),
        "mla_w_uq": nrm((L, MLA_Q_RANK, MLA_HEADS * MLA_QK_DIM), MLA_Q_RANK ** -0.5),
        "mla_w_ukv": nrm((L, MLA_KV_RANK, MLA_HEADS * (MLA_NOPE_DIM + MLA_V_DIM)), MLA_KV_RANK ** -0.5),
        "mla_q_norm_g": gain((L, MLA_QK_DIM)),
        "mla_k_norm_g": gain((L, MLA_QK_DIM)),
        "mem_norm_g": gain((L, D)),
        "mem_w_kv": nrm((L, D, 2 * MEM_WIDTH), D ** -0.5),
        "mem_q_norm_g": gain((L, MEM_HEAD_DIM)),
        "mem_k_norm_g": gain((L, MEM_HEAD_DIM)),
        "w_o_diff": nrm((L, DIFF_WIDTH, D), DIFF_WIDTH ** -0.5),
        "w_o_mla": nrm((L, MLA_WIDTH, D), MLA_WIDTH ** -0.5),
        "w_o_mem": nrm((L, MEM_WIDTH, D), MEM_WIDTH ** -0.5),
        "w_out": nrm((L, D, D), D ** -0.5),
        "ffn_norm_g": gain((L, D)),
        "w_route_group": nrm((L, D, N_GROUPS), D ** -0.5),
        "b_route_group": nrm((L, N_GROUPS), 0.01),
        "w_route_expert": nrm((L, D, N_EXPERTS), D ** -0.5),
        "b_route_expert": nrm((L, N_EXPERTS), 0.01),
        "w_exp_gate": nrm((L, N_EXPERTS, D, EXPERT_FF), D ** -0.5),
        "w_exp_up": nrm((L, N_EXPERTS, D, EXPERT_FF), D ** -0.5),
        "w_exp_down": nrm((L, N_EXPERTS, EXPERT_FF, D), EXPERT_FF ** -0.5),
    }


def reference(x, mem, positions, rel_bias, mix_norm_g, w_in, diff_q_norm_g, diff_k_norm_g,
              diff_lambda_q1, diff_lambda_k1, diff_lambda_q2, diff_lambda_k2, diff_subln_g,
              mla_cq_norm_g, mla_ckv_norm_g, mla_w_uq, mla_w_ukv, mla_q_norm_g, mla_k_norm_g,
              mem_norm_g, mem_w_kv, mem_q_norm_g, mem_k_norm_g, w_o_diff, w_o_mla, w_o_mem,
              w_out, ffn_norm_g, w_route_group, b_route_group, w_route_expert, b_route_expert,
              w_exp_gate, w_exp_up, w_exp_down):
    b, s, d = x.shape
    offsets = [int(o) for o in np.cumsum(IN_SPLITS)[:-1]]
    for l in range(DEPTH):
        h = rms_norm(x, mix_norm_g[l])
        proj = h @ w_in[l]
        dq, dk, dv, c_q, c_kv, k_rope, mq, gate_logits = jnp.split(proj, offsets, axis=-1)
        y_diff = diff_attention(dq, dk, dv, positions, rel_bias,
                                diff_lambda_q1[l], diff_lambda_k1[l], diff_lambda_q2[l],
                                diff_lambda_k2[l], diff_q_norm_g[l], diff_k_norm_g[l],
                                diff_subln_g[l], l) @ w_o_diff[l]
        y_mla = latent_attention(c_q, c_kv, k_rope, positions, mla_cq_norm_g[l], mla_ckv_norm_g[l],
                                 mla_w_uq[l], mla_w_ukv[l], mla_q_norm_g[l],
                                 mla_k_norm_g[l]) @ w_o_mla[l]
        y_mem = memory_attention(mq, mem, mem_norm_g[l], mem_w_kv[l], mem_q_norm_g[l],
                                 mem_k_norm_g[l]) @ w_o_mem[l]
        gates = jax.nn.sigmoid(gate_logits.astype(jnp.float32)).astype(x.dtype).reshape(
            b, s, N_BRANCHES, d)
        mixed = gates[:, :, 0] * y_diff + gates[:, :, 1] * y_mla + gates[:, :, 2] * y_mem
        x = x + mixed @ w_out[l]
        h2 = rms_norm(x, ffn_norm_g[l]).reshape(b * s, d)
        y_ffn = hierarchical_moe(h2, w_route_group[l], b_route_group[l], w_route_expert[l],
                                 b_route_expert[l], w_exp_gate[l], w_exp_up[l], w_exp_down[l])
        x = x + y_ffn.reshape(b, s, d)
    return x
```

```python
import math
from contextlib import ExitStack
import numpy as np
import concourse.bass as bass
import concourse.mybir as mybir
from concourse.bass_utils import run_bass_kernel_spmd

F32 = mybir.dt.float32
BF16 = mybir.dt.bfloat16
I32 = mybir.dt.int32
AF = mybir.ActivationFunctionType
ALU = mybir.AluOpType
AX = mybir.AxisListType

NCORES = 8
T = 8192
TL = 1024
D = 4096
KC = 32
NMEM = 256
EPS = 1e-6
IN_COLS = 20032
C_DQ, C_DK, C_DV, C_CQ, C_CKV, C_KR, C_MQ, C_G = 0, 1536, 3072, 4608, 6144, 6656, 6720, 7744
R_SLOT = 256
NSLOT = 8 * R_SLOT
SEM_CAP = 24000


class Buf:
    __slots__ = ("name", "writes", "reads", "dsem", "dcount")

    def __init__(self, name):
        self.name = name
        self.writes = {}
        self.reads = {}
        self.dsem = None
        self.dcount = 0


class TT:
    def __init__(self, t, name):
        self.t = t
        self.b = Buf(name)
        self.psum = False

    def __getitem__(self, k):
        return self.t[k]


class Eng:
    def __init__(self, fw, name, h):
        self.fw, self.name, self.h = fw, name, h
        self.sem = None
        self.count = 0
        self.waited = {}
        self.allsems = []

    def new_sem(self):
        self.sem = self.fw.alloc_sem(self.name)
        self.count = 0
        self.allsems.append(self.sem)


class FW:
    def __init__(self, nc, stack):
        self.nc = nc
        self.stack = stack
        self.nsem = 0
        self.pe = Eng(self, "pe", nc.tensor)
        self.act = Eng(self, "act", nc.scalar)
        self.dve = Eng(self, "dve", nc.vector)
        self.pool = Eng(self, "pool", nc.gpsimd)
        self.sp = Eng(self, "sp", nc.sync)
        self.engs = [self.pe, self.act, self.dve, self.pool, self.sp]
        for e in self.engs:
            e.new_sem()
        self.ninst = 0
        self.dma_bufs = []
        self.sem_pool = []
        self.retired = []
        self.uid = 0

    def alloc_sem(self, name):
        self.nsem += 1
        return self.stack.enter_context(self.nc.semaphore(f"s{self.nsem}_{name}"))

    def tile(self, st, name, shape, dt):
        self.uid += 1
        nm = f"{name}_{self.uid}"
        tt = TT(st.enter_context(self.nc.sbuf_tensor(nm, shape, dt)), nm)
        if st is not self.stack:
            st.callback(self.release, tt)
        return tt

    def release(self, tt):
        b = tt.b
        if b.dsem is not None:
            if b in self.dma_bufs:
                self.dma_bufs.remove(b)
            if b.dcount < SEM_CAP - 4000:
                self.sem_pool.append((b.dsem, b.dcount))
            else:
                self.retired.append((b.dsem, b.dcount))
            b.dsem = None

    def ptile(self, st, name, shape, dt):
        self.uid += 1
        nm = f"{name}_{self.uid}"
        tt = TT(st.enter_context(self.nc.psum_tensor(nm, shape, dt)), nm)
        tt.psum = True
        return tt

    def _wait(self, eng, deps):
        for key, (sem, val) in deps.items():
            if eng.waited.get(key, 0) < val:
                eng.h.wait_ge(sem, val)
                eng.waited[key] = val

    def _deps(self, reads, writes, acc):
        deps = {}

        def add(d):
            for k, sv in d.items():
                if k not in deps or deps[k][1] < sv[1]:
                    deps[k] = sv
        for b in reads:
            add(b.b.writes)
            if b.psum:
                add(b.b.reads)
        for b in writes:
            add(b.b.reads)
            add(b.b.writes)
        return deps

    def op(self, eng, fn, reads=(), writes=(), acc=False, merge=False):
        deps = self._deps(reads, writes, acc)
        if acc:
            deps.pop(id(eng.sem), None)
        self._wait(eng, deps)
        if eng.count >= SEM_CAP:
            eng.new_sem()
        inst = fn()
        inst.then_inc(eng.sem, 1)
        eng.count += 1
        self.ninst += 1
        ev = (eng.sem, eng.count)
        key = id(eng.sem)
        for w in writes:
            if merge or acc:
                w.b.writes[key] = ev
            else:
                w.b.writes = {key: ev}
                w.b.reads = {}
        for r in reads:
            r.b.reads[key] = ev
        return inst

    def dma(self, eng, out, in_, reads=(), writes=(), sb=None, merge=False, **kw):
        deps = self._deps(reads, writes, False)
        self._wait(eng, deps)
        b = sb.b
        if b.dsem is not None and b.dcount >= SEM_CAP:
            self.retired.append((b.dsem, b.dcount))
            self.dma_bufs.remove(b)
            b.dsem = None
        if b.dsem is None:
            if self.sem_pool:
                b.dsem, b.dcount = self.sem_pool.pop()
            else:
                b.dsem = self.alloc_sem("d")
                b.dcount = 0
            self.dma_bufs.append(b)
        inst = eng.h.dma_start(out=out, in_=in_, **kw)
        inst.then_inc(b.dsem, 16)
        b.dcount += 16
        self.ninst += 1
        ev = (b.dsem, b.dcount)
        key = id(b.dsem)
        for w in writes:
            if merge:
                w.b.writes[key] = ev
            else:
                w.b.writes = {key: ev}
                w.b.reads = {}
        for r in reads:
            r.b.reads[key] = ev
        return inst

    def barrier(self):
        deps = {}
        for e in self.engs:
            if e.count > 0:
                deps[id(e.sem)] = (e.sem, e.count)
        for b in self.dma_bufs:
            if b.dcount > 0:
                deps[id(b.dsem)] = (b.dsem, b.dcount)
        for (s_, c_) in self.retired:
            deps[id(s_)] = (s_, c_)
        for e in self.engs:
            self._wait(e, deps)


def t5_thresholds():
    n = np.arange(0, 4096, dtype=np.int32)
    max_exact = 16
    nf = np.maximum(n, 1).astype(np.float32)
    large = max_exact + (np.log(nf / np.float32(max_exact)) / np.float32(math.log(128 / 16))
                         * np.float32(16)).astype(np.int32)
    large = np.minimum(large, 31)
    bucket = np.where(n < max_exact, n, large)
    thr = []
    for b in range(1, 32):
        thr.append(int(np.argmax(bucket >= b)))
    return thr


def build(stop_after=99, dbg=False, skip=()):
    nc = bass.Bass("TRN2", target_bir_lowering=False)

    def din(name, shape, dt=F32):
        return nc.dram_tensor(name, list(shape), dt, kind="ExternalInput").ap()

    def dscr(name, shape, dt):
        kind = "ExternalOutput" if dbg else "Internal"
        return nc.dram_tensor(name, list(shape), dt, kind=kind).ap()

    x_full = din("x_full", [T, D])
    x_own = din("x_own", [TL, D])
    mem = din("mem", [NMEM, D])
    pos_full = din("pos_full", [1, T], I32)
    pos_own = din("pos_own", [1, TL], I32)
    rel_bias = din("rel_bias", [1, 32 * 12])
    mix_g = din("mix_norm_g", [128, KC])
    w_in = din("w_in", [D, IN_COLS])
    dq_g = din("diff_q_norm_g", [128, 1])
    dk_g = din("diff_k_norm_g", [128, 1])
    lam_v = din("lam_v", [1, 4 * 128])
    subln_g = din("diff_subln_g", [1, 256])
    cq_g = din("mla_cq_norm_g", [128, 12])
    ckv_g = din("mla_ckv_norm_g", [128, 4])
    w_uq = din("mla_w_uq", [1536, 2304])
    w_ukv = din("mla_w_ukv", [512, 3072])
    mq_g = din("mla_q_norm_g", [192, 1])
    mk_g = din("mla_k_norm_g", [192, 1])
    mem_g = din("mem_norm_g", [128, KC])
    mem_wkv = din("mem_w_kv", [D, 2048])
    memq_g = din("mem_q_norm_g", [128, 2])
    memk_g = din("mem_k_norm_g", [128, 2])
    w_o_diff = din("w_o_diff", [1536, D])
    w_o_mla = din("w_o_mla", [1536, D])
    w_o_mem = din("w_o_mem", [1024, D])
    w_out = din("w_out", [D, D])
    ffn_g = din("ffn_norm_g", [128, KC])
    w_rt = din("w_rt", [D, 72])
    b_rt = din("b_rt", [1, 72])
    if stop_after >= 6:
        w_eg = din("w_exp_gate", [64, D, 512])
        w_eu = din("w_exp_up", [64, D, 512])
        w_ed = din("w_exp_down", [64 * 512, D])
    c_ident = din("c_ident", [128, 128])
    c_rot = din("c_rot", [64, 64])
    c_tri = din("c_tri", [128, 128])
    c_iota = din("c_iota", [128, NSLOT])
    c_invf = din("c_invf", [64, 1])
    c_goff = din("c_goff", [128, 8])
    out = nc.dram_tensor("out", [TL, D], F32, kind="ExternalOutput").ap()

    hT_all = dscr("hT_all", [KC, 128, T], BF16)
    QdT = dscr("QdT", [12, 128, TL], BF16)
    KdT = dscr("KdT", [12, 128, T], BF16)
    Vd1 = dscr("Vd1", [6, T, 257], BF16)
    QmT = dscr("QmT", [12, 192, TL], BF16)
    KmT = dscr("KmT", [12, 192, T], BF16)
    Vm1 = dscr("Vm1", [12, T, 129], BF16)
    QeT = dscr("QeT", [4, 256, TL], BF16)
    gT = dscr("gT", [96, 128, TL], F32)
    x1s = dscr("x1s", [TL, D], F32)
    ys = dscr("ys", [NSLOT, D], F32)

    thr = t5_thresholds()

    with ExitStack() as top:
        f = FW(nc, top)
        V, A, P, G = f.dve, f.act, f.pe, f.pool
        SP = f.sp
        dram = {n: TT(None, n) for n in ["hT_all", "QdT", "KdT", "Vd1", "QmT", "KmT", "Vm1", "QeT", "gT", "x1s", "ys", "out"]}

        def tl(st, name, shape, dt=F32):
            return f.tile(st, name, shape, dt)

        ident_f = tl(top, "ident_f", [128, 128])
        ident = tl(top, "ident", [128, 128], BF16)
        ones = tl(top, "ones", [128, 128], BF16)
        rotT = tl(top, "rotT", [64, 64], BF16)
        rot_f = tl(top, "rot_f", [64, 64])
        mixg = tl(top, "mixg", [128, KC])
        dqg = tl(top, "dqg", [128, 1])
        dkg = tl(top, "dkg", [128, 1])
        cqg = tl(top, "cqg", [128, 12])
        ckvg = tl(top, "ckvg", [128, 4])
        mqgA = tl(top, "mqgA", [128, 1])
        mqgB = tl(top, "mqgB", [64, 1])
        mkgA = tl(top, "mkgA", [128, 1])
        mkgB = tl(top, "mkgB", [64, 1])
        memqg = tl(top, "memqg", [128, 2])
        invf = tl(top, "invf", [64, 1])
        memkg = tl(top, "memkg", [128, 2])
        memg = tl(top, "memg", [128, KC])
        ffng = tl(top, "ffng", [128, KC])
        for dst, src in [(ident_f, c_ident), (rot_f, c_rot), (mixg, mix_g), (dqg, dq_g), (dkg, dk_g), (cqg, cq_g),
                         (ckvg, ckv_g), (mqgA, mq_g[0:128, :]), (mqgB, mq_g[128:192, :]), (mkgA, mk_g[0:128, :]),
                         (mkgB, mk_g[128:192, :]), (memqg, memq_g), (invf, c_invf), (memkg, memk_g), (memg, mem_g), (ffng, ffn_g)]:
            f.dma(SP, dst[:], src, writes=[dst], sb=dst)
        f.op(V, lambda: nc.vector.tensor_copy(out=ident[:], in_=ident_f[:]), reads=[ident_f], writes=[ident])
        f.op(V, lambda: nc.vector.tensor_copy(out=rotT[:], in_=rot_f[:]), reads=[rot_f], writes=[rotT])
        f.op(V, lambda: nc.vector.memset(ones[:], 1.0), writes=[ones])

        NQB = TL // 128
        NTT_ALL = T // 512
        NTT_OWN = TL // 512
        hT_ownD = dscr("hT_ownD", [KC, 128, TL], BF16)
        dram["hT_ownD"] = TT(None, "hT_ownD")
        epsb = tl(top, "epsb", [128, 1])
        f.op(V, lambda: nc.vector.memset(epsb[:], EPS), writes=[epsb])

        def rstd_from(ps_ss, out_t, inv_n, np_=128, n=512):
            f.op(A, lambda: nc.scalar.activation(out=out_t[0:np_, 0:n], in_=ps_ss[0:np_, 0:n], func=AF.Sqrt,
                                                  scale=inv_n, bias=epsb[0:np_, 0:1]),
                 reads=[ps_ss, epsb], writes=[out_t])
            f.op(V, lambda: nc.vector.reciprocal(out=out_t[0:np_, 0:n], in_=out_t[0:np_, 0:n]),
                 reads=[out_t], writes=[out_t])

        def phase_norm(src, ntiles, gain_t, dstT, dkey, ss_out=None):
            with ExitStack() as st:
                xts = [tl(st, f"xt{i}", [128, D]) for i in range(2)]
                xbs = [tl(st, f"xb{i}", [128, D], BF16) for i in range(2)]
                sss = [tl(st, f"ss{i}", [128, 1]) for i in range(2)]
                junk = tl(st, "junk", [128, D], BF16)
                hts = [tl(st, f"ht{i}", [128, KC, 128], BF16) for i in range(2)]
                pts = [f.ptile(st, f"pt{i}", [128, 8, 128], BF16) for i in range(4)]
                for i in range(ntiles):
                    xt, xb, ss, ht = xts[i % 2], xbs[i % 2], sss[i % 2], hts[i % 2]
                    f.dma(SP, xt[:], src[i * 128:(i + 1) * 128, :], writes=[xt], sb=xt)
                    f.op(A, lambda: nc.scalar.activation(out=junk[:], in_=xt[:], func=AF.Square, accum_out=ss[:]),
                         reads=[xt], writes=[junk, ss])
                    f.op(A, lambda: nc.scalar.activation(out=ss[:], in_=ss[:], func=AF.Sqrt, scale=1.0 / D, bias=epsb[:, 0:1]),
                         reads=[ss, epsb], writes=[ss])
                    f.op(V, lambda: nc.vector.reciprocal(out=ss[:], in_=ss[:]), reads=[ss], writes=[ss])
                    f.op(A, lambda: nc.scalar.activation(out=xb[:], in_=xt[:], func=AF.Copy, scale=ss[:, 0:1]),
                         reads=[xt, ss], writes=[xb])
                    for q in range(4):
                        pt = pts[q]
                        for k in range(8):
                            c = q * 8 + k
                            f.op(P, lambda: nc.tensor.transpose(out=pt[:, k, :], in_=xb[:, c * 128:(c + 1) * 128], identity=ident[:]),
                                 reads=[xb, ident], writes=[pt], acc=True)
                        gb = gain_t[:, q * 8:(q + 1) * 8].unsqueeze(2).to_broadcast([128, 8, 128])
                        f.op(V, lambda: nc.vector.tensor_tensor(out=ht[:, q * 8:(q + 1) * 8, :], in0=pt[:], in1=gb, op=ALU.mult),
                             reads=[pt, gain_t], writes=[ht], merge=True)
                    f.dma(SP, dstT[:, :, i * 128:(i + 1) * 128].rearrange("c p t -> p c t"), ht[:],
                          reads=[ht], writes=[dram[dkey]], sb=ht, merge=True)
                f.barrier()

        if "p1" not in skip:
            phase_norm(x_full, T // 128, mixg, hT_all, "hT_all")
            phase_norm(x_own, TL // 128, mixg, hT_ownD, "hT_ownD")
        if stop_after <= 1:
            return finish(nc, f, dram)

        pan_serial = TT(None, "pan_serial")

        def load_panel(pan, W, r0, nk, c0, ncols):
            f.dma(G, pan[:, 0:nk, 0:ncols], W[r0:r0 + nk * 128, c0:c0 + ncols].rearrange("(k p) c -> p k c", p=128),
                  writes=[pan, pan_serial], sb=pan)

        def load_hT(ht, srcT, t0, nk=KC, n=512, k0=0):
            f.dma(SP, ht[:, 0:nk, 0:n], srcT[k0:k0 + nk, :, t0:t0 + n].rearrange("c p t -> p c t"),
                  reads=[dram_of[id(srcT)]] if id(srcT) in dram_of else [], writes=[ht], sb=ht)

        dram_of = {id(hT_all): dram["hT_all"], id(hT_ownD): dram["hT_ownD"]}

        def mm_fm(ps, np_, pan, c0, ht, nk, n=512, first=True, last=True, extra_reads=()):
            for k in range(nk):
                f.op(P, lambda: nc.tensor.matmul(ps[0:np_, 0:n], lhsT=pan[:, k, c0:c0 + np_], rhs=ht[:, k, 0:n],
                                                  start=(first and k == 0), stop=(last and k == nk - 1)),
                     reads=[pan, ht], writes=[ps], acc=True)

        def mm_tm(ps, ht, t0, pan, c0, ncols, nk):
            for k in range(nk):
                f.op(P, lambda: nc.tensor.matmul(ps[:, 0:ncols], lhsT=ht[:, k, t0:t0 + 128], rhs=pan[:, k, c0:c0 + ncols],
                                                  start=(k == 0), stop=(k == nk - 1)),
                     reads=[pan, ht], writes=[ps], acc=True)

        def fm_rms(st_tiles, chunks, dim, gains, outs, pre_ss=None, n=512):
            sq, pss, rstd = st_tiles
            nchk = len(chunks)
            for i, (ps, np_) in enumerate(chunks):
                f.op(A, lambda: nc.scalar.activation(out=sq[0:np_, 0:n], in_=ps[0:np_, 0:n], func=AF.Square),
                     reads=[ps], writes=[sq])
                f.op(P, lambda: nc.tensor.matmul(pss[:, 0:n], lhsT=ones[0:np_, :], rhs=sq[0:np_, 0:n],
                                                  start=(i == 0 and pre_ss is None), stop=(i == nchk - 1)),
                     reads=[ones, sq], writes=[pss], acc=True)
            rstd_from(pss, rstd, 1.0 / dim, n=n)
            for (ps, np_), g, (o, _) in zip(chunks, gains, outs):
                f.op(V, lambda: nc.vector.scalar_tensor_tensor(out=o[0:np_, 0:n], in0=ps[0:np_, 0:n], scalar=g, in1=rstd[0:np_, 0:n],
                                                                op0=ALU.mult, op1=ALU.mult),
                     reads=[ps, rstd], writes=[o])
            return rstd

        def pass_fm_rms(srcT, ntt, c0, nmaps, grp, dim, gain_fn, dstT, dkey, row_fn, W=None, n=512):
            W = w_in if W is None else W
            with ExitStack() as st:
                pans = [tl(st, f"pan{i}", [128, KC, 512], BF16) for i in range(2)]
                hts = [tl(st, f"ht{i}", [128, KC, 512], BF16) for i in range(2)]
                sq = tl(st, "sq", [128, 512], BF16)
                rstd = tl(st, "rstd", [128, 512])
                obs = [tl(st, f"ob{i}", [128, 512], BF16) for i in range(4)]
                pss = f.ptile(st, "pss", [128, 512], F32)
                psc = [f.ptile(st, f"psc{i}", [128, 512], F32) for i in range(4)]
                npan = (nmaps + 3) // 4
                it = 0
                for pi in range(npan):
                    pan = pans[pi % 2]
                    ncol = min(512, (nmaps - pi * 4) * 128)
                    load_panel(pan, W, 0, KC, c0 + pi * 512, ncol)
                    for tt in range(ntt):
                        ht = hts[it % 2]
                        it += 1
                        load_hT(ht, srcT, tt * n, n=n)
                        for sg in range(0, ncol // 128, grp):
                            chunks = []
                            for s_ in range(grp):
                                ps = psc[(sg + s_) % 4]
                                mm_fm(ps, 128, pan, (sg + s_) * 128, ht, KC, n=n)
                                chunks.append((ps, 128))
                            outs = [(obs[(sg + s_) % 4], 128) for s_ in range(grp)]
                            fm_rms((sq, pss, rstd), chunks, dim, [gain_fn(s_) for s_ in range(grp)], outs, n=n)
                            for s_ in range(grp):
                                m = pi * 4 + sg + s_
                                f.dma(SP, row_fn(m)[:, tt * n:(tt + 1) * n], outs[s_][0][:, 0:n],
                                      reads=[outs[s_][0]], writes=[dram[dkey]], sb=outs[s_][0], merge=True)
                f.barrier()

        if "p2" not in skip:
            pass_fm_rms(hT_ownD, NTT_OWN, C_DQ, 12, 1, 128, lambda s_: dqg[:, 0:1], QdT, "QdT", lambda m: QdT[m])
            pass_fm_rms(hT_all, NTT_ALL, C_DK, 12, 1, 128, lambda s_: dkg[:, 0:1], KdT, "KdT", lambda m: KdT[m])
            pass_fm_rms(hT_ownD, NTT_OWN, C_MQ, 8, 2, 256, lambda s_: memqg[:, s_:s_ + 1], QeT, "QeT",
                        lambda m: QeT[m // 2, (m % 2) * 128:(m % 2) * 128 + 128, :])
        if stop_after <= 2:
            return finish(nc, f, dram)

        if "p3" not in skip:
            with ExitStack() as st:
                pans = [tl(st, f"pan{i}", [128, KC, 512], BF16) for i in range(2)]
                hts = [tl(st, f"ht{i}", [128, KC, 512], BF16) for i in range(2)]
                gos = [tl(st, f"go{i}", [128, 512]) for i in range(4)]
                v1s = [tl(st, f"v1{i}", [128, 2, 257], BF16) for i in range(2)]
                psc = [f.ptile(st, f"psc{i}", [128, 512], F32) for i in range(4)]
                for v1 in v1s:
                    f.op(V, lambda: nc.vector.memset(v1[:], 1.0), writes=[v1])
                it = 0
                for pi in range(24):
                    pan = pans[pi % 2]
                    load_panel(pan, w_in, 0, KC, C_G + pi * 512, 512)
                    for tt in range(NTT_OWN):
                        ht = hts[it % 2]
                        it += 1
                        load_hT(ht, hT_ownD, tt * 512)
                        for sc in range(4):
                            ps, go = psc[sc], gos[sc]
                            mm_fm(ps, 128, pan, sc * 128, ht, KC)
                            f.op(A, lambda: nc.scalar.activation(out=go[:], in_=ps[:], func=AF.Sigmoid), reads=[ps], writes=[go])
                            f.dma(SP, gT[pi * 4 + sc, :, tt * 512:(tt + 1) * 512], go[:], reads=[go], writes=[dram["gT"]], sb=go, merge=True)
                for pi in range(3):
                    pan = pans[pi % 2]
                    load_panel(pan, w_in, 0, KC, C_DV + pi * 512, 512)
                    for tt in range(NTT_ALL):
                        ht = hts[it % 2]
                        it += 1
                        load_hT(ht, hT_all, tt * 512)
                        for tb in range(4):
                            ps, v1 = psc[tb], v1s[tb % 2]
                            mm_tm(ps, ht, tb * 128, pan, 0, 512, KC)
                            f.op(V, lambda: nc.vector.tensor_copy(out=v1[:, :, 0:256], in_=ps[:].rearrange("p (h c) -> p h c", h=2)),
                                 reads=[ps], writes=[v1])
                            r0 = tt * 512 + tb * 128
                            f.dma(SP, Vd1[pi * 2:pi * 2 + 2, r0:r0 + 128, :].rearrange("h t c -> t h c"), v1[:],
                                  reads=[v1], writes=[dram["Vd1"]], sb=v1, merge=True)
                f.barrier()
        if stop_after <= 3:
            return finish(nc, f, dram)

        NCg = NCORES
        NEM = NCg + 1
        OT_d = dscr("OT_d", [KC, 128, TL], BF16)
        dram["OT_d"] = TT(None, "OT_d")
        att_st = ExitStack()
        EM = tl(att_st, "EM", [128, 12, NEM * 128], BF16)
        MK = tl(att_st, "MK", [128, NEM * 128], BF16)
        rb = tl(att_st, "rb", [128, 32 * 12])
        lam = tl(att_st, "lam", [128, 1])
        nlam = tl(att_st, "nlam", [128, 1])
        sublng = tl(att_st, "sublng", [128, 256])
        with ExitStack() as st:
            posq_i = tl(st, "posq_i", [128, 128], I32)
            posk_i = tl(st, "posk_i", [128, NEM], I32)
            posq = tl(st, "posq", [128, 128])
            posk = tl(st, "posk", [128, NEM])
            dist = tl(st, "dist", [128, NEM * 128])
            mask = tl(st, "mask", [128, NEM * 128])
            ind = tl(st, "ind", [128, NEM * 128])
            acc = tl(st, "acc", [128, 12, NEM * 128])
            delta = tl(st, "delta", [128, 31 * 12])
            base = tl(st, "base", [128, 12])
            lv = tl(st, "lv", [128, 512])
            lp = tl(st, "lp", [128, 256])
            ls = tl(st, "ls", [128, 2])
            f.dma(SP, posq_i[:], pos_own[0:1, 128:256].partition_broadcast(128), writes=[posq_i], sb=posq_i)
            f.dma(SP, posk_i[:], pos_full[0, (NCg - 1) * 128:(2 * NCg) * 128].rearrange("(i p) -> p i", p=128),
                  writes=[posk_i], sb=posk_i, allow_slow_non_contiguous=True)
            f.dma(SP, rb[:], rel_bias[0:1, :].partition_broadcast(128), writes=[rb], sb=rb)
            f.dma(SP, lv[:], lam_v[0:1, :].partition_broadcast(128), writes=[lv], sb=lv)
            f.dma(SP, sublng[:], subln_g[0:1, :].partition_broadcast(128), writes=[sublng], sb=sublng)
            f.op(V, lambda: nc.vector.tensor_copy(out=posq[:], in_=posq_i[:]), reads=[posq_i], writes=[posq])
            f.op(V, lambda: nc.vector.tensor_copy(out=posk[:], in_=posk_i[:]), reads=[posk_i], writes=[posk])
            for i in range(NEM):
                f.op(V, lambda: nc.vector.tensor_scalar(out=dist[:, i * 128:(i + 1) * 128], in0=posq[:], scalar1=posk[:, i:i + 1],
                                                          scalar2=None, op0=ALU.subtract), reads=[posq, posk], writes=[dist], merge=True)
            f.op(V, lambda: nc.vector.tensor_single_scalar(out=mask[:], in_=dist[:], scalar=0.0, op=ALU.is_ge), reads=[dist], writes=[mask])
            f.op(V, lambda: nc.vector.tensor_copy(out=MK[:], in_=mask[:]), reads=[mask], writes=[MK])
            f.op(V, lambda: nc.vector.tensor_tensor(out=delta[:], in0=rb[:, 12:32 * 12], in1=rb[:, 0:31 * 12], op=ALU.subtract),
                 reads=[rb], writes=[delta])
            f.op(V, lambda: nc.vector.tensor_tensor(out=base[:], in0=rb[:, 0:12], in1=rb[:, 31 * 12:32 * 12], op=ALU.subtract),
                 reads=[rb], writes=[base])
            f.op(G, lambda: nc.gpsimd.memset(acc[:], 0.0), writes=[acc])
            for b in range(1, 32):
                f.op(V, lambda: nc.vector.tensor_single_scalar(out=ind[:], in_=dist[:], scalar=float(thr[b - 1]), op=ALU.is_ge),
                     reads=[dist], writes=[ind])
                for m in range(12):
                    eng = V
                    h_ = nc.vector
                    f.op(eng, lambda: h_.scalar_tensor_tensor(out=acc[:, m, :], in0=ind[:], scalar=delta[:, (b - 1) * 12 + m:(b - 1) * 12 + m + 1],
                                                               in1=acc[:, m, :], op0=ALU.mult, op1=ALU.add),
                         reads=[ind, delta, acc], writes=[acc], merge=True)
            for m in range(12):
                f.op(A, lambda: nc.scalar.activation(out=acc[:, m, :], in_=acc[:, m, :], func=AF.Exp, bias=base[:, m:m + 1]),
                     reads=[acc, base], writes=[acc], merge=True)
                f.op(V, lambda: nc.vector.tensor_tensor(out=EM[:, m, :], in0=acc[:, m, :], in1=mask[:], op=ALU.mult),
                     reads=[acc, mask], writes=[EM], merge=True)
            f.op(V, lambda: nc.vector.tensor_tensor(out=lp[:].rearrange("p (a c) -> p a c", a=2),
                                                      in0=lv[:].rearrange("p (a b c) -> p a b c", a=2, b=2)[:, :, 0, :],
                                                      in1=lv[:].rearrange("p (a b c) -> p a b c", a=2, b=2)[:, :, 1, :], op=ALU.mult),
                 reads=[lv], writes=[lp])
            f.op(V, lambda: nc.vector.reduce_sum(out=ls[:], in_=lp[:].rearrange("p (a c) -> p a c", a=2), axis=AX.X),
                 reads=[lp], writes=[ls])
            f.op(A, lambda: nc.scalar.activation(out=ls[:], in_=ls[:], func=AF.Exp), reads=[ls], writes=[ls])
            f.op(V, lambda: nc.vector.tensor_tensor(out=lam[:], in0=ls[:, 0:1], in1=ls[:, 1:2], op=ALU.subtract), reads=[ls], writes=[lam])
            f.op(V, lambda: nc.vector.tensor_scalar(out=nlam[:], in0=lam[:], scalar1=0.2, scalar2=-1.0, op0=ALU.add, op1=ALU.mult),
                 reads=[lam], writes=[nlam])
            f.op(V, lambda: nc.vector.tensor_scalar(out=sublng[:], in0=sublng[:], scalar1=0.8, scalar2=None, op0=ALU.mult),
                 reads=[sublng], writes=[sublng])
            f.barrier()

        def attention(st, QT_list, KT_list, V1, dv, nkb_fn, scale, bias_ap_fn, em_fn, units, epilogue):
            sps = [f.ptile(st, f"sps{i}", [128, 4, 128], F32) for i in range(2)]
            ops_ = [f.ptile(st, f"ops{i}", [128, 512], F32) for i in range(4)]
            pTs = [tl(st, f"pT{i}", [128, 4, 128], BF16) for i in range(3)]
            gi = 0
            for j in range(NQB):
                nkb = nkb_fn(j)
                for u in range(units):
                    ops = ops_[(j * units + u) % 4]
                    for g0 in range(0, nkb, 4):
                        sp, pT = sps[gi % 2], pTs[gi % 3]
                        gi += 1
                        nb = min(4, nkb - g0)
                        for i in range(nb):
                            kb = g0 + i
                            nch = len(QT_list[u])
                            for ci in range(nch):
                                qt, np_ = QT_list[u][ci]
                                kt, _ = KT_list[u][ci]
                                f.op(P, lambda: nc.tensor.matmul(sp[:, i, :], lhsT=kt[0:np_, kb * 128:(kb + 1) * 128],
                                                                  rhs=qt[0:np_, j * 128:(j + 1) * 128], start=(ci == 0), stop=(ci == nch - 1)),
                                     reads=[kt, qt], writes=[sp], acc=True)
                        bap = bias_ap_fn(u)
                        f.op(A, lambda: nc.scalar.activation(out=pT[:, 0:nb, :], in_=sp[:, 0:nb, :], func=AF.Exp, scale=scale,
                                                              **({"bias": bap} if bap is not None else {})),
                             reads=[sp], writes=[pT])
                        for i in range(nb):
                            kb = g0 + i
                            em = em_fn(u, j, kb)
                            if em is not None:
                                f.op(V, lambda: nc.vector.tensor_tensor(out=pT[:, i, :], in0=pT[:, i, :], in1=em, op=ALU.mult),
                                     reads=[pT, EM, MK], writes=[pT], merge=True)
                        for i in range(nb):
                            kb = g0 + i
                            f.op(P, lambda: nc.tensor.matmul(ops[:, 0:dv + 1], lhsT=pT[:, i, :], rhs=V1[:, kb, :],
                                                              start=(kb == 0), stop=(kb == nkb - 1)),
                                 reads=[pT, V1], writes=[ops], acc=True)
                    epilogue(j, u, ops)

        def run_diff():
            for h in range(6):
                with ExitStack() as st:
                    KT = [tl(st, f"KT{i}", [128, T], BF16) for i in range(2)]
                    QT = [tl(st, f"QT{i}", [128, TL], BF16) for i in range(2)]
                    V1 = tl(st, "V1", [128, T // 128, 257], BF16)
                    o1 = tl(st, "o1", [128, 256])
                    ob = tl(st, "ob", [128, 256], BF16)
                    rr = tl(st, "rr", [128, 4])
                    jk = tl(st, "jk", [128, 256])
                    otb = tl(st, "otb", [128, 2, 128], BF16)
                    tps = f.ptile(st, "tps", [128, 2, 128], BF16)
                    for i, m in enumerate((h, h + 6)):
                        f.dma(SP, KT[i][:], KdT[m], reads=[dram["KdT"]], writes=[KT[i]], sb=KT[i])
                        f.dma(SP, QT[i][:], QdT[m], reads=[dram["QdT"]], writes=[QT[i]], sb=QT[i])
                    f.dma(SP, V1[:], Vd1[h].rearrange("(b p) c -> p b c", p=128), reads=[dram["Vd1"]], writes=[V1], sb=V1)
                    state = {}

                    def epi(j, u, ops):
                        state[u] = ops
                        if u == 0:
                            return
                        O1, O2 = state[0], state[1]
                        f.op(V, lambda: nc.vector.reciprocal(out=rr[:, 0:1], in_=O1[:, 256:257]), reads=[O1], writes=[rr])
                        f.op(V, lambda: nc.vector.reciprocal(out=rr[:, 1:2], in_=O2[:, 256:257]), reads=[O2], writes=[rr])
                        f.op(V, lambda: nc.vector.tensor_tensor(out=rr[:, 1:2], in0=rr[:, 1:2], in1=nlam[:], op=ALU.mult), reads=[rr, nlam], writes=[rr])
                        f.op(V, lambda: nc.vector.tensor_scalar(out=o1[:], in0=O1[:, 0:256], scalar1=rr[:, 0:1], scalar2=None, op0=ALU.mult),
                             reads=[O1, rr], writes=[o1])
                        f.op(V, lambda: nc.vector.scalar_tensor_tensor(out=o1[:], in0=O2[:, 0:256], scalar=rr[:, 1:2], in1=o1[:],
                                                                        op0=ALU.mult, op1=ALU.add), reads=[O2, rr, o1], writes=[o1])
                        f.op(A, lambda: nc.scalar.activation(out=jk[:], in_=o1[:], func=AF.Square, accum_out=rr[:, 2:3]), reads=[o1], writes=[jk, rr])
                        f.op(A, lambda: nc.scalar.activation(out=rr[:, 2:3], in_=rr[:, 2:3], func=AF.Sqrt, scale=1.0 / 256, bias=epsb[:, 0:1]),
                             reads=[rr, epsb], writes=[rr])
                        f.op(V, lambda: nc.vector.reciprocal(out=rr[:, 3:4], in_=rr[:, 2:3]), reads=[rr], writes=[rr])
                        f.op(V, lambda: nc.vector.scalar_tensor_tensor(out=ob[:], in0=o1[:], scalar=rr[:, 3:4], in1=sublng[:],
                                                                        op0=ALU.mult, op1=ALU.mult), reads=[o1, rr, sublng], writes=[ob])
                        for c2 in range(2):
                            f.op(P, lambda: nc.tensor.transpose(out=tps[:, c2, :], in_=ob[:, c2 * 128:(c2 + 1) * 128], identity=ident[:]),
                                 reads=[ob, ident], writes=[tps], acc=True)
                        f.op(V, lambda: nc.vector.tensor_copy(out=otb[:], in_=tps[:]), reads=[tps], writes=[otb])
                        f.dma(SP, OT_d[2 * h:2 * h + 2, :, j * 128:(j + 1) * 128].rearrange("c p t -> p c t"), otb[:],
                              reads=[otb], writes=[dram["OT_d"]], sb=otb, merge=True)

                    def em_fn(u, j, kb):
                        i = kb - NCg * j
                        if i < -1:
                            return None
                        m = h + 6 * u
                        return EM[:, m, (i + 1) * 128:(i + 2) * 128]

                    attention(st, [[(QT[0], 128)], [(QT[1], 128)]], [[(KT[0], 128)], [(KT[1], 128)]], V1, 256,
                              lambda j: NCg * j + NCg, 128 ** -0.5, lambda u: rb[:, 31 * 12 + h + 6 * u:31 * 12 + h + 6 * u + 1],
                              em_fn, 2, epi)
                    f.barrier()

        if "diff" not in skip:
            run_diff()
        if stop_after <= 4:
            return finish(nc, f, dram)

        TWO_PI = 2.0 * math.pi
        CW1 = 6.28125
        CW2 = TWO_PI - CW1

        def trig(st_t, pos_ap, t0, cosT, sinT):
            pi_, pf, ang, kk, ki, mm = st_t
            f.dma(SP, pi_[:], pos_ap[0:1, t0:t0 + 512].partition_broadcast(64), writes=[pi_], sb=pi_)
            f.op(V, lambda: nc.vector.tensor_copy(out=pf[:], in_=pi_[:]), reads=[pi_], writes=[pf])
            for which, outT in ((0, sinT), (1, cosT)):
                f.op(V, lambda: nc.vector.tensor_scalar(out=ang[:], in0=pf[:], scalar1=invf[:, 0:1], scalar2=(0.5 * math.pi if which else 0.0),
                                                          op0=ALU.mult, op1=ALU.add), reads=[pf, invf], writes=[ang])
                f.op(V, lambda: nc.vector.tensor_scalar(out=kk[:], in0=ang[:], scalar1=1.0 / TWO_PI, scalar2=None, op0=ALU.mult),
                     reads=[ang], writes=[kk])
                f.op(V, lambda: nc.vector.tensor_copy(out=ki[:], in_=kk[:]), reads=[kk], writes=[ki])
                f.op(V, lambda: nc.vector.tensor_copy(out=kk[:], in_=ki[:]), reads=[ki], writes=[kk])
                f.op(V, lambda: nc.vector.scalar_tensor_tensor(out=ang[:], in0=kk[:], scalar=-CW1, in1=ang[:], op0=ALU.mult, op1=ALU.add),
                     reads=[kk, ang], writes=[ang])
                f.op(V, lambda: nc.vector.scalar_tensor_tensor(out=ang[:], in0=kk[:], scalar=-CW2, in1=ang[:], op0=ALU.mult, op1=ALU.add),
                     reads=[kk, ang], writes=[ang])
                f.op(V, lambda: nc.vector.tensor_single_scalar(out=mm[:], in_=ang[:], scalar=math.pi, op=ALU.is_gt), reads=[ang], writes=[mm])
                f.op(V, lambda: nc.vector.scalar_tensor_tensor(out=ang[:], in0=mm[:], scalar=-TWO_PI, in1=ang[:], op0=ALU.mult, op1=ALU.add),
                     reads=[mm, ang], writes=[ang])
                f.op(V, lambda: nc.vector.tensor_single_scalar(out=mm[:], in_=ang[:], scalar=-math.pi, op=ALU.is_lt), reads=[ang], writes=[mm])
                f.op(V, lambda: nc.vector.scalar_tensor_tensor(out=ang[:], in0=mm[:], scalar=TWO_PI, in1=ang[:], op0=ALU.mult, op1=ALU.add),
                     reads=[mm, ang], writes=[ang])
                f.op(V, lambda: nc.vector.tensor_scalar(out=ang[:], in0=ang[:], scalar1=3.14159, scalar2=-3.14159, op0=ALU.min, op1=ALU.max),
                     reads=[ang], writes=[ang])
                f.op(A, lambda: nc.scalar.activation(out=outT[:], in_=ang[:], func=AF.Sin), reads=[ang], writes=[outT])

        def trig_tiles(st):
            return (tl(st, "pi_", [64, 512], I32), tl(st, "pf", [64, 512]), tl(st, "ang", [64, 512]), tl(st, "kk", [64, 512]),
                    tl(st, "ki", [64, 512], I32), tl(st, "mm", [64, 512]))

        def rotary(xf, xb, psR, cosT, sinT, t1, outb):
            f.op(A, lambda: nc.scalar.copy(out=xb[:], in_=xf[0:64, :]), reads=[xf], writes=[xb])
            f.op(P, lambda: nc.tensor.matmul(psR[0:64, :], lhsT=rotT[:], rhs=xb[:], start=True, stop=True), reads=[rotT, xb], writes=[psR], acc=True)
            f.op(V, lambda: nc.vector.tensor_tensor(out=t1[:], in0=xf[0:64, :], in1=cosT[:], op=ALU.mult), reads=[xf, cosT], writes=[t1])
            f.op(V, lambda: nc.vector.tensor_tensor(out=xf[0:64, :], in0=psR[0:64, :], in1=sinT[:], op=ALU.mult), reads=[psR, sinT], writes=[xf])
            f.op(V, lambda: nc.vector.tensor_tensor(out=outb[0:64, :], in0=t1[:], in1=xf[0:64, :], op=ALU.add), reads=[t1, xf], writes=[outb])

        def lat_norm(pb, pss, pans, ht, nch, gain_t, raw, sq, rstd, outb):
            for cch in range(nch):
                ps = pb[cch % 4]
                mm_fm(ps, 128, pans[cch // 4], (cch % 4) * 128, ht, KC)
                f.op(A, lambda: nc.scalar.activation(out=sq[:], in_=ps[:], func=AF.Square), reads=[ps], writes=[sq])
                f.op(P, lambda: nc.tensor.matmul(pss[:, :], lhsT=ones[:, :], rhs=sq[:], start=(cch == 0), stop=(cch == nch - 1)),
                     reads=[ones, sq], writes=[pss], acc=True)
                f.op(V, lambda: nc.vector.tensor_scalar(out=raw[:, cch, :], in0=ps[:], scalar1=gain_t[:, cch:cch + 1], scalar2=None, op0=ALU.mult),
                     reads=[ps, gain_t], writes=[raw], merge=True)
            rstd_from(pss, rstd, 1.0 / (nch * 128))
            f.op(V, lambda: nc.vector.tensor_tensor(out=outb[:, 0:nch, :], in0=raw[:, 0:nch, :],
                                                      in1=rstd[:].unsqueeze(1).to_broadcast([128, nch, 512]), op=ALU.mult),
                 reads=[raw, rstd], writes=[outb])

        cqnT_d = dscr("cqnT_d", [12, 128, TL], BF16)
        dram["cqnT_d"] = TT(None, "cqnT_d")
        dram_of[id(cqnT_d)] = dram["cqnT_d"]
        with ExitStack() as st:
            pans = [tl(st, f"pan{i}", [128, KC, 512], BF16) for i in range(3)]
            ht = tl(st, "ht", [128, KC, 512], BF16)
            raw = tl(st, "raw", [128, 12, 512])
            cqns = [tl(st, f"cqn{i}", [128, 12, 512], BF16) for i in range(1)] * 2
            sq = tl(st, "sq", [128, 512], BF16)
            rstd = tl(st, "rstd", [128, 512])
            pb = [f.ptile(st, f"pb{i}", [128, 512], F32) for i in range(6)]
            for i in range(3):
                load_panel(pans[i], w_in, 0, KC, C_CQ + i * 512, 512)
            if stop_after <= 4.25:
                f.barrier()
                return finish(nc, f, dram)
            for tt in range(NTT_OWN):
                cqn = cqns[tt % 2]
                load_hT(ht, hT_ownD, tt * 512)
                if stop_after <= 4.27:
                    f.barrier()
                    return finish(nc, f, dram)
                lat_norm(pb, pb[5], pans, ht, 12, cqg, raw, sq, rstd, cqn)
                if stop_after <= 4.29:
                    f.barrier()
                    return finish(nc, f, dram)
                f.dma(SP, cqnT_d[:, :, tt * 512:(tt + 1) * 512].rearrange("c p t -> p c t"), cqn[:],
                      reads=[cqn], writes=[dram["cqnT_d"]], sb=cqn, merge=True)
            f.barrier()
        if stop_after <= 4.3:
            return finish(nc, f, dram)
        with ExitStack() as st:
            wq = tl(st, "wq", [128, 12, 2304], BF16)
            cqns = [tl(st, f"cqn{i}", [128, 12, 512], BF16) for i in range(2)]
            sq = tl(st, "sq", [128, 512], BF16)
            rstd2 = tl(st, "rstd2", [128, 512])
            cosT, sinT = tl(st, "cosT", [64, 512]), tl(st, "sinT", [64, 512])
            tt_ = trig_tiles(st)
            oAs = [tl(st, f"oA{i}", [128, 512], BF16) for i in range(2)]
            oBf = tl(st, "oBf", [64, 512])
            xb = tl(st, "xb", [64, 512], BF16)
            t1 = tl(st, "t1", [64, 512])
            oBs = [tl(st, f"oB{i}", [64, 512], BF16) for i in range(2)]
            pb = [f.ptile(st, f"pb{i}", [128, 512], F32) for i in range(8)]
            load_panel(wq, w_uq, 0, 12, 0, 2304)
            for tt in range(NTT_OWN):
                cqn = cqns[tt % 2]
                load_hT(cqn, cqnT_d, tt * 512, nk=12)
                trig(tt_, pos_own, tt * 512, cosT, sinT)
                for h in range(12):
                    psA, psB = pb[h % 2], pb[2 + h % 2]
                    oA, oB = oAs[h % 2], oBs[h % 2]
                    mm_fm(psA, 128, wq, h * 192, cqn, 12)
                    mm_fm(psB, 64, wq, h * 192 + 128, cqn, 12)
                    fm_rms((sq, pb[5], rstd2), [(psA, 128), (psB, 64)], 192, [mqgA[:, 0:1], mqgB[:, 0:1]], [(oA, 128), (oBf, 64)])
                    rotary(oBf, xb, pb[6], cosT, sinT, t1, oB)
                    f.dma(SP, QmT[h, 0:128, tt * 512:(tt + 1) * 512], oA[:], reads=[oA], writes=[dram["QmT"]], sb=oA, merge=True)
                    f.dma(SP, QmT[h, 128:192, tt * 512:(tt + 1) * 512], oB[:], reads=[oB], writes=[dram["QmT"]], sb=oB, merge=True)
            f.barrier()

        if stop_after <= 4.6:
            return finish(nc, f, dram)
        with ExitStack() as st:
            pan = tl(st, "pan", [128, KC, 576], BF16)
            hts = [tl(st, f"ht{i}", [128, KC, 512], BF16) for i in range(2)]
            wkv = tl(st, "wkv", [128, 4, 3072], BF16)
            raw = tl(st, "raw", [128, 4, 512])
            ckn = tl(st, "ckn", [128, 4, 512], BF16)
            sq = tl(st, "sq", [128, 512], BF16)
            sqk = tl(st, "sqk", [64, 512], BF16)
            rstd = tl(st, "rstd", [128, 512])
            rstd2 = tl(st, "rstd2", [128, 512])
            cosT, sinT = tl(st, "cosT", [64, 512]), tl(st, "sinT", [64, 512])
            tt_ = trig_tiles(st)
            krf = tl(st, "krf", [64, 512])
            krot = tl(st, "krot", [64, 512])
            krb = tl(st, "krb", [64, 512], BF16)
            xb = tl(st, "xb", [64, 512], BF16)
            t1 = tl(st, "t1", [64, 512])
            oAs = [tl(st, f"oA{i}", [128, 512], BF16) for i in range(2)]
            oBs = [tl(st, f"oB{i}", [64, 512], BF16) for i in range(2)]
            v1s = [tl(st, f"v1{i}", [128, 4, 129], BF16) for i in range(2)]
            pb = [f.ptile(st, f"pb{i}", [128, 512], F32) for i in range(8)]
            for v1 in v1s:
                f.op(V, lambda: nc.vector.memset(v1[:], 1.0), writes=[v1])
            load_panel(pan, w_in, 0, KC, C_CKV, 576)
            load_panel(wkv, w_ukv, 0, 4, 0, 3072)
            wkv_v = wkv[:].rearrange("p k (h c) -> p k h c", c=256)
            vi = 0
            for tt in range(NTT_ALL):
                ht = hts[tt % 2]
                load_hT(ht, hT_all, tt * 512)
                trig(tt_, pos_full, tt * 512, cosT, sinT)
                lat_norm(pb, pb[5], [pan], ht, 4, ckvg, raw, sq, rstd, ckn)
                mm_fm(pb[4], 64, pan, 512, ht, KC)
                f.op(A, lambda: nc.scalar.activation(out=sqk[:], in_=pb[4][0:64, :], func=AF.Square), reads=[pb[4]], writes=[sqk])
                f.op(V, lambda: nc.vector.tensor_scalar(out=krf[:], in0=pb[4][0:64, :], scalar1=mkgB[:, 0:1], scalar2=None, op0=ALU.mult),
                     reads=[pb[4], mkgB], writes=[krf])
                rotary(krf, xb, pb[6], cosT, sinT, t1, krb)
                f.op(V, lambda: nc.vector.tensor_copy(out=krot[:], in_=krb[:]), reads=[krb], writes=[krot])
                for h in range(12):
                    psN = pb[h % 2]
                    oA, oB = oAs[h % 2], oBs[h % 2]
                    mm_fm(psN, 128, wkv, h * 256, ckn, 4)
                    f.op(A, lambda: nc.scalar.activation(out=sq[:], in_=psN[:], func=AF.Square), reads=[psN], writes=[sq])
                    f.op(P, lambda: nc.tensor.matmul(pb[5][:, :], lhsT=ones[:, :], rhs=sq[:], start=True, stop=False),
                         reads=[ones, sq], writes=[pb[5]], acc=True)
                    f.op(P, lambda: nc.tensor.matmul(pb[5][:, :], lhsT=ones[0:64, :], rhs=sqk[:], start=False, stop=True),
                         reads=[ones, sqk], writes=[pb[5]], acc=True)
                    rstd_from(pb[5], rstd2, 1.0 / 192)
                    f.op(V, lambda: nc.vector.scalar_tensor_tensor(out=oA[:], in0=psN[:], scalar=mkgA[:, 0:1], in1=rstd2[:], op0=ALU.mult, op1=ALU.mult),
                         reads=[psN, mkgA, rstd2], writes=[oA])
                    f.op(V, lambda: nc.vector.tensor_tensor(out=oB[:], in0=krot[:], in1=rstd2[0:64, :], op=ALU.mult), reads=[krot, rstd2], writes=[oB])
                    f.dma(SP, KmT[h, 0:128, tt * 512:(tt + 1) * 512], oA[:], reads=[oA], writes=[dram["KmT"]], sb=oA, merge=True)
                    f.dma(SP, KmT[h, 128:192, tt * 512:(tt + 1) * 512], oB[:], reads=[oB], writes=[dram["KmT"]], sb=oB, merge=True)
                for tb in range(4):
                    for hg in range(3):
                        psV, v1 = pb[2 + (vi % 2)], v1s[vi % 2]
                        vi += 1
                        for k in range(4):
                            f.op(P, lambda: nc.tensor.matmul(psV[:, :].rearrange("p (h c) -> p h c", c=128), lhsT=ckn[:, k, tb * 128:(tb + 1) * 128],
                                                              rhs=wkv_v[:, k, hg * 4:(hg + 1) * 4, 128:256], start=(k == 0), stop=(k == 3)),
                                 reads=[ckn, wkv], writes=[psV], acc=True)
                        f.op(V, lambda: nc.vector.tensor_copy(out=v1[:, :, 0:128], in_=psV[:, :].rearrange("p (h c) -> p h c", c=128)),
                             reads=[psV], writes=[v1])
                        r0 = tt * 512 + tb * 128
                        f.dma(SP, Vm1[hg * 4:(hg + 1) * 4, r0:r0 + 128, :].rearrange("h t c -> t h c"), v1[:],
                              reads=[v1], writes=[dram["Vm1"]], sb=v1, merge=True)
            f.barrier()
        if stop_after <= 5:
            return finish(nc, f, dram)

        def run_mla():
            for h in range(12):
                with ExitStack() as st:
                    KnT = tl(st, "KnT", [128, T], BF16)
                    KrT = tl(st, "KrT", [64, T], BF16)
                    QnT = tl(st, "QnT", [128, TL], BF16)
                    QrT = tl(st, "QrT", [64, TL], BF16)
                    V1 = tl(st, "V1", [128, T // 128, 129], BF16)
                    rr = tl(st, "rr", [128, 1])
                    ob = tl(st, "ob", [128, 128], BF16)
                    otb = tl(st, "otb", [128, 128], BF16)
                    tps = f.ptile(st, "tps", [128, 128], BF16)
                    f.dma(SP, KnT[:], KmT[h, 0:128, :], reads=[dram["KmT"]], writes=[KnT], sb=KnT)
                    f.dma(SP, KrT[:], KmT[h, 128:192, :], reads=[dram["KmT"]], writes=[KrT], sb=KrT)
                    f.dma(SP, QnT[:], QmT[h, 0:128, :], reads=[dram["QmT"]], writes=[QnT], sb=QnT)
                    f.dma(SP, QrT[:], QmT[h, 128:192, :], reads=[dram["QmT"]], writes=[QrT], sb=QrT)
                    f.dma(SP, V1[:], Vm1[h].rearrange("(b p) c -> p b c", p=128), reads=[dram["Vm1"]], writes=[V1], sb=V1)

                    def epi(j, u, O):
                        f.op(V, lambda: nc.vector.reciprocal(out=rr[:], in_=O[:, 128:129]), reads=[O], writes=[rr])
                        f.op(V, lambda: nc.vector.tensor_scalar(out=ob[:], in0=O[:, 0:128], scalar1=rr[:, 0:1], scalar2=None, op0=ALU.mult),
                             reads=[O, rr], writes=[ob])
                        f.op(P, lambda: nc.tensor.transpose(out=tps[:], in_=ob[:], identity=ident[:]), reads=[ob, ident], writes=[tps], acc=True)
                        f.op(V, lambda: nc.vector.tensor_copy(out=otb[:], in_=tps[:]), reads=[tps], writes=[otb])
                        f.dma(SP, OT_d[12 + h, :, j * 128:(j + 1) * 128], otb[:], reads=[otb], writes=[dram["OT_d"]], sb=otb, merge=True)

                    def em_fn(u, j, kb):
                        i = kb - NCg * j
                        if i < 0:
                            return None
                        return MK[:, (i + 1) * 128:(i + 2) * 128]

                    attention(st, [[(QnT, 128), (QrT, 64)]], [[(KnT, 128), (KrT, 64)]], V1, 128,
                              lambda j: NCg * j + NCg, 192 ** -0.5, lambda u: None, em_fn, 1, epi)
                    f.barrier()

        if "mla" not in skip:
            run_mla()

        hT_memD = dscr("hT_memD", [KC, 128, NMEM], BF16)
        KeT = dscr("KeT", [4, 256, NMEM], BF16)
        Ve1 = dscr("Ve1", [4, NMEM, 257], BF16)
        for nm, ap in (("hT_memD", hT_memD), ("KeT", KeT), ("Ve1", Ve1)):
            dram[nm] = TT(None, nm)
            dram_of[id(ap)] = dram[nm]
        phase_norm(mem, NMEM // 128, memg, hT_memD, "hT_memD")
        pass_fm_rms(hT_memD, 1, 0, 8, 2, 256, lambda s_: memkg[:, s_:s_ + 1], KeT, "KeT",
                    lambda m: KeT[m // 2, (m % 2) * 128:(m % 2) * 128 + 128, :], W=mem_wkv, n=NMEM)
        with ExitStack() as st:
            pans = [tl(st, f"pan{i}", [128, KC, 512], BF16) for i in range(2)]
            ht = tl(st, "ht", [128, KC, NMEM], BF16)
            v1s = [tl(st, f"v1{i}", [128, 2, 257], BF16) for i in range(2)]
            psc = [f.ptile(st, f"psc{i}", [128, 512], F32) for i in range(2)]
            for v1 in v1s:
                f.op(V, lambda: nc.vector.memset(v1[:], 1.0), writes=[v1])
            load_hT(ht, hT_memD, 0, n=NMEM)
            vi = 0
            for pi in range(2):
                load_panel(pans[pi], mem_wkv, 0, KC, 1024 + pi * 512, 512)
                for tb in range(NMEM // 128):
                    ps, v1 = psc[vi % 2], v1s[vi % 2]
                    vi += 1
                    mm_tm(ps, ht, tb * 128, pans[pi], 0, 512, KC)
                    f.op(V, lambda: nc.vector.tensor_copy(out=v1[:, :, 0:256], in_=ps[:].rearrange("p (h c) -> p h c", h=2)),
                         reads=[ps], writes=[v1])
                    f.dma(SP, Ve1[pi * 2:pi * 2 + 2, tb * 128:(tb + 1) * 128, :].rearrange("h t c -> t h c"), v1[:],
                          reads=[v1], writes=[dram["Ve1"]], sb=v1, merge=True)
            f.barrier()
        for e in range(4):
            with ExitStack() as st:
                KT = [tl(st, f"KT{i}", [128, NMEM], BF16) for i in range(2)]
                QT = [tl(st, f"QT{i}", [128, TL], BF16) for i in range(2)]
                V1 = tl(st, "V1", [128, NMEM // 128, 257], BF16)
                rr = tl(st, "rr", [128, 1])
                ob = tl(st, "ob", [128, 256], BF16)
                otb = tl(st, "otb", [128, 2, 128], BF16)
                tps = f.ptile(st, "tps", [128, 2, 128], BF16)
                for i in range(2):
                    f.dma(SP, KT[i][:], KeT[e, i * 128:(i + 1) * 128, :], reads=[dram["KeT"]], writes=[KT[i]], sb=KT[i])
                    f.dma(SP, QT[i][:], QeT[e, i * 128:(i + 1) * 128, :], reads=[dram["QeT"]], writes=[QT[i]], sb=QT[i])
                f.dma(SP, V1[:], Ve1[e].rearrange("(b p) c -> p b c", p=128), reads=[dram["Ve1"]], writes=[V1], sb=V1)

                def epi(j, u, O):
                    f.op(V, lambda: nc.vector.reciprocal(out=rr[:], in_=O[:, 256:257]), reads=[O], writes=[rr])
                    f.op(V, lambda: nc.vector.tensor_scalar(out=ob[:], in0=O[:, 0:256], scalar1=rr[:, 0:1], scalar2=None, op0=ALU.mult),
                         reads=[O, rr], writes=[ob])
                    for c2 in range(2):
                        f.op(P, lambda: nc.tensor.transpose(out=tps[:, c2, :], in_=ob[:, c2 * 128:(c2 + 1) * 128], identity=ident[:]),
                             reads=[ob, ident], writes=[tps], acc=True)
                    f.op(V, lambda: nc.vector.tensor_copy(out=otb[:], in_=tps[:]), reads=[tps], writes=[otb])
                    f.dma(SP, OT_d[24 + 2 * e:26 + 2 * e, :, j * 128:(j + 1) * 128].rearrange("c p t -> p c t"), otb[:],
                          reads=[otb], writes=[dram["OT_d"]], sb=otb, merge=True)

                attention(st, [[(QT[0], 128), (QT[1], 128)]], [[(KT[0], 128), (KT[1], 128)]], V1, 256,
                          lambda j: NMEM // 128, 256 ** -0.5, lambda u: None, lambda u, j, kb: None, 1, epi)
                f.barrier()
        f.barrier()
        att_st.close()
        if stop_after <= 6:
            return finish(nc, f, dram)

        mixT_d = dscr("mixT_d", [KC, 128, TL], BF16)
        dram["mixT_d"] = TT(None, "mixT_d")
        dram_of[id(mixT_d)] = dram["mixT_d"]
        dram_of[id(OT_d)] = dram["OT_d"]
        with ExitStack() as st:
            pw = [[tl(st, f"pw{i}{b}", [128, nk_, 512], BF16) for b, nk_ in enumerate((12, 12, 8))] for i in range(2)]
            ots = [tl(st, f"ot{i}", [128, KC, 512], BF16) for i in range(2)]
            gts = [[tl(st, f"gt{i}{b}", [128, 512]) for b in range(3)] for i in range(2)]
            mx = tl(st, "mx", [128, 512])
            mbs = [tl(st, f"mb{i}", [128, 512], BF16) for i in range(2)]
            psc = [f.ptile(st, f"psc{i}", [128, 512], F32) for i in range(6)]
            it = 0
            gi = 0
            for dp in range(8):
                pws = pw[dp % 2]
                for b, (Wb, nk_) in enumerate(((w_o_diff, 12), (w_o_mla, 12), (w_o_mem, 8))):
                    load_panel(pws[b], Wb, 0, nk_, dp * 512, 512)
                for tt in range(NTT_OWN):
                    ot = ots[it % 2]
                    it += 1
                    load_hT(ot, OT_d, tt * 512)
                    for sc in range(4):
                        dc = dp * 4 + sc
                        gt = gts[gi % 2]
                        mb = mbs[gi % 2]
                        pss_ = psc[(gi % 2) * 3:(gi % 2) * 3 + 3]
                        gi += 1
                        for b, (k0, nk_) in enumerate(((0, 12), (12, 12), (24, 8))):
                            f.dma(SP, gt[b][:], gT[b * 32 + dc, :, tt * 512:(tt + 1) * 512], reads=[dram["gT"]], writes=[gt[b]], sb=gt[b])
                            for k in range(nk_):
                                f.op(P, lambda: nc.tensor.matmul(pss_[b][:, :], lhsT=pws[b][:, k, sc * 128:(sc + 1) * 128], rhs=ot[:, k0 + k, :],
                                                                  start=(k == 0), stop=(k == nk_ - 1)),
                                     reads=[pws[b], ot], writes=[pss_[b]], acc=True)
                        f.op(V, lambda: nc.vector.tensor_tensor(out=mx[:], in0=pss_[0][:], in1=gt[0][:], op=ALU.mult), reads=[pss_[0], gt[0]], writes=[mx])
                        f.op(V, lambda: nc.vector.tensor_tensor(out=gt[1][:], in0=pss_[1][:], in1=gt[1][:], op=ALU.mult), reads=[pss_[1], gt[1]], writes=[gt[1]])
                        f.op(V, lambda: nc.vector.tensor_tensor(out=gt[2][:], in0=pss_[2][:], in1=gt[2][:], op=ALU.mult), reads=[pss_[2], gt[2]], writes=[gt[2]])
                        f.op(G, lambda: nc.gpsimd.tensor_tensor(out=mx[:], in0=mx[:], in1=gt[1][:], op=ALU.add), reads=[mx, gt[1]], writes=[mx])
                        f.op(G, lambda: nc.gpsimd.tensor_tensor(out=mb[:], in0=mx[:], in1=gt[2][:], op=ALU.add), reads=[mx, gt[2]], writes=[mb])
                        f.dma(SP, mixT_d[dc, :, tt * 512:(tt + 1) * 512], mb[:], reads=[mb], writes=[dram["mixT_d"]], sb=mb, merge=True)
            f.barrier()
        with ExitStack() as st:
            pans = [tl(st, f"pan{i}", [128, KC, 512], BF16) for i in range(2)]
            mts = [tl(st, f"mt{i}", [128, KC, 512], BF16) for i in range(2)]
            xts = [tl(st, f"xt{i}", [128, 512]) for i in range(2)]
            psc = [f.ptile(st, f"psc{i}", [128, 512], F32) for i in range(2)]
            it = 0
            xi = 0
            for dp in range(8):
                pan = pans[dp % 2]
                load_panel(pan, w_out, 0, KC, dp * 512, 512)
                for tt in range(NTT_OWN):
                    mt = mts[it % 2]
                    it += 1
                    load_hT(mt, mixT_d, tt * 512)
                    for tb in range(4):
                        ps, xt = psc[xi % 2], xts[xi % 2]
                        xi += 1
                        r0 = tt * 512 + tb * 128
                        f.dma(SP, xt[:], x_own[r0:r0 + 128, dp * 512:(dp + 1) * 512], writes=[xt], sb=xt)
                        mm_tm(ps, mt, tb * 128, pan, 0, 512, KC)
                        f.op(V, lambda: nc.vector.tensor_tensor(out=xt[:], in0=ps[:], in1=xt[:], op=ALU.add), reads=[ps, xt], writes=[xt])
                        f.dma(SP, x1s[r0:r0 + 128, dp * 512:(dp + 1) * 512], xt[:], reads=[xt], writes=[dram["x1s"]], sb=xt, merge=True)
            f.barrier()
        if stop_after <= 7:
            return finish(nc, f, dram)

        RS = R_SLOT
        NSB = NSLOT // 128
        SBG = RS // 128
        h2T_d = dscr("h2T_d", [KC, 128, TL], BF16)
        h2gT_d = dscr("h2gT_d", [8, KC, 128, RS], BF16)
        actT_d = dscr("actT_d", [64, 4, 128, RS], BF16)
        for nm, ap in (("h2T_d", h2T_d), ("actT_d", actT_d), ("h2gT_d", h2gT_d)):
            dram[nm] = TT(None, nm)
            dram_of[id(ap)] = dram[nm]
        with ExitStack() as s0:
            Perm = tl(s0, "Perm", [128, NQB, NSLOT], BF16)
            gws = tl(s0, "gws", [128, NSB, 64])
            with ExitStack() as s1:
                xb_all = tl(s1, "xb_all", [128, NQB, D], BF16)
                with ExitStack() as st:
                    xts = [tl(st, f"xt{i}", [128, D]) for i in range(2)]
                    sss = [tl(st, f"ss{i}", [128, 1]) for i in range(2)]
                    junk = tl(st, "junk", [128, D], BF16)
                    hts = [tl(st, f"ht{i}", [128, KC, 128], BF16) for i in range(2)]
                    pts = [f.ptile(st, f"pt{i}", [128, 8, 128], BF16) for i in range(4)]
                    for i in range(NQB):
                        xt, ss, ht = xts[i % 2], sss[i % 2], hts[i % 2]
                        f.dma(SP, xt[:], x1s[i * 128:(i + 1) * 128, :], writes=[xt], sb=xt)
                        f.op(A, lambda: nc.scalar.activation(out=junk[:], in_=xt[:], func=AF.Square, accum_out=ss[:]), reads=[xt], writes=[junk, ss])
                        f.op(A, lambda: nc.scalar.activation(out=ss[:], in_=ss[:], func=AF.Sqrt, scale=1.0 / D, bias=epsb[:, 0:1]), reads=[ss, epsb], writes=[ss])
                        f.op(V, lambda: nc.vector.reciprocal(out=ss[:], in_=ss[:]), reads=[ss], writes=[ss])
                        f.op(A, lambda: nc.scalar.activation(out=xb_all[:, i, :], in_=xt[:], func=AF.Copy, scale=ss[:, 0:1]), reads=[xt, ss], writes=[xb_all], merge=True)
                        for q in range(4):
                            pt = pts[q]
                            for k in range(8):
                                c = q * 8 + k
                                f.op(P, lambda: nc.tensor.transpose(out=pt[:, k, :], in_=xb_all[:, i, c * 128:(c + 1) * 128], identity=ident[:]),
                                     reads=[xb_all, ident], writes=[pt], acc=True)
                            gb = ffng[:, q * 8:(q + 1) * 8].unsqueeze(2).to_broadcast([128, 8, 128])
                            f.op(V, lambda: nc.vector.tensor_tensor(out=ht[:, q * 8:(q + 1) * 8, :], in0=pt[:], in1=gb, op=ALU.mult),
                                 reads=[pt, ffng], writes=[ht], merge=True)
                        f.dma(SP, h2T_d[:, :, i * 128:(i + 1) * 128].rearrange("c p t -> p c t"), ht[:], reads=[ht], writes=[dram["h2T_d"]], sb=ht, merge=True)
                    f.barrier()
                with ExitStack() as st:
                    wrt = tl(st, "wrt", [128, KC, 72], BF16)
                    brt = tl(st, "brt", [128, 72])
                    hbs = [tl(st, f"hb{i}", [128, KC, 128], BF16) for i in range(2)]
                    gw = tl(st, "gw", [128, NQB, 64])
                    gwh = tl(st, "gwh", [128, NQB, 64], BF16)
                    gwr = tl(st, "gwr", [128, NQB, 64])
                    gwl = tl(st, "gwl", [128, NQB, 64], BF16)
                    GmA = tl(st, "GmA", [128, NQB, 8])
                    Gmb = tl(st, "Gmb", [128, NQB, 8], BF16)
                    tri_f = tl(st, "tri_f", [128, 128])
                    tri_b = tl(st, "tri_b", [128, 128], BF16)
                    goff = tl(st, "goff", [128, 8])
                    iota = tl(st, "iota", [128, NSLOT])
                    slot = tl(st, "slot", [128, NQB])
                    tmp8 = tl(st, "tmp8", [128, 8])
                    lg = tl(st, "lg", [128, 72])
                    sm = tl(st, "sm", [128, 16])
                    t8 = tl(st, "t8", [128, 8])
                    ex8 = tl(st, "ex8", [128, 8])
                    lem = tl(st, "lem", [128, 64])
                    lem2 = tl(st, "lem2", [128, 64])
                    mk1 = tl(st, "mk1", [128, 64])
                    mk2 = tl(st, "mk2", [128, 64])
                    psr = f.ptile(st, "psr", [128, 512], F32)
                    psk = f.ptile(st, "psk", [128, 512], F32)
                    psg_ = [f.ptile(st, f"psgw{i}", [128, 512], F32) for i in range(2)]
                    load_panel(wrt, w_rt, 0, KC, 0, 72)
                    f.dma(SP, brt[:], b_rt[0:1, :].partition_broadcast(128), writes=[brt], sb=brt)
                    f.dma(SP, tri_f[:], c_tri, writes=[tri_f], sb=tri_f)
                    f.dma(SP, goff[:], c_goff, writes=[goff], sb=goff)
                    f.dma(SP, iota[:], c_iota, writes=[iota], sb=iota)
                    f.op(V, lambda: nc.vector.tensor_copy(out=tri_b[:], in_=tri_f[:]), reads=[tri_f], writes=[tri_b])
                    S = lambda i: sm[:, i:i + 1]
                    for tb in range(NQB):
                        hb = hbs[tb % 2]
                        load_hT(hb, h2T_d, tb * 128, n=128)
                        mm_tm(psr, hb, 0, wrt, 0, 72, KC)
                        f.op(V, lambda: nc.vector.tensor_tensor(out=lg[:], in0=psr[:, 0:72], in1=brt[:], op=ALU.add), reads=[psr, brt], writes=[lg])
                        f.op(V, lambda: nc.vector.reduce_max(out=S(0), in_=lg[:, 0:8], axis=AX.X), reads=[lg], writes=[sm])
                        f.op(V, lambda: nc.vector.tensor_scalar(out=GmA[:, tb, :], in0=lg[:, 0:8], scalar1=S(0), scalar2=None, op0=ALU.is_equal), reads=[lg, sm], writes=[GmA], merge=True)
                        f.op(V, lambda: nc.vector.tensor_scalar(out=S(1), in0=S(0), scalar1=-1.0, scalar2=None, op0=ALU.mult), reads=[sm], writes=[sm])
                        f.op(A, lambda: nc.scalar.activation(out=ex8[:], in_=lg[:, 0:8], func=AF.Exp, bias=S(1), accum_out=S(2)), reads=[lg, sm], writes=[ex8, sm])
                        f.op(V, lambda: nc.vector.reciprocal(out=S(3), in_=S(2)), reads=[sm], writes=[sm])
                        f.op(V, lambda: nc.vector.tensor_scalar(out=t8[:], in0=GmA[:, tb, :], scalar1=-1.0, scalar2=1e30, op0=ALU.add, op1=ALU.mult), reads=[GmA], writes=[t8])
                        f.op(V, lambda: nc.vector.tensor_tensor(out=lem[:].rearrange("p (g e) -> p g e", g=8), in0=lg[:, 8:72].rearrange("p (g e) -> p g e", g=8),
                                                                  in1=t8[:].unsqueeze(2).to_broadcast([128, 8, 8]), op=ALU.add), reads=[lg, t8], writes=[lem])
                        f.op(V, lambda: nc.vector.reduce_max(out=S(4), in_=lem[:], axis=AX.X), reads=[lem], writes=[sm])
                        f.op(V, lambda: nc.vector.tensor_scalar(out=mk1[:], in0=lem[:], scalar1=S(4), scalar2=None, op0=ALU.is_equal), reads=[lem, sm], writes=[mk1])
                        f.op(V, lambda: nc.vector.scalar_tensor_tensor(out=lem2[:], in0=mk1[:], scalar=-1e30, in1=lem[:], op0=ALU.mult, op1=ALU.add), reads=[mk1, lem], writes=[lem2])
                        f.op(V, lambda: nc.vector.reduce_max(out=S(5), in_=lem2[:], axis=AX.X), reads=[lem2], writes=[sm])
                        f.op(V, lambda: nc.vector.tensor_scalar(out=mk2[:], in0=lem2[:], scalar1=S(5), scalar2=None, op0=ALU.is_equal), reads=[lem2, sm], writes=[mk2])
                        f.op(V, lambda: nc.vector.tensor_scalar(out=S(6), in0=S(4), scalar1=-1.0, scalar2=None, op0=ALU.mult), reads=[sm], writes=[sm])
                        f.op(A, lambda: nc.scalar.activation(out=S(7), in_=S(5), func=AF.Exp, bias=S(6)), reads=[sm], writes=[sm])
                        f.op(V, lambda: nc.vector.tensor_scalar(out=S(8), in0=S(7), scalar1=1.0, scalar2=None, op0=ALU.add), reads=[sm], writes=[sm])
                        f.op(V, lambda: nc.vector.reciprocal(out=S(9), in_=S(8)), reads=[sm], writes=[sm])
                        f.op(V, lambda: nc.vector.tensor_tensor(out=S(10), in0=S(3), in1=S(9), op=ALU.mult), reads=[sm], writes=[sm])
                        f.op(V, lambda: nc.vector.tensor_tensor(out=S(11), in0=S(10), in1=S(7), op=ALU.mult), reads=[sm], writes=[sm])
                        f.op(V, lambda: nc.vector.tensor_scalar(out=gw[:, tb, :], in0=mk1[:], scalar1=S(10), scalar2=None, op0=ALU.mult), reads=[mk1, sm], writes=[gw], merge=True)
                        f.op(V, lambda: nc.vector.scalar_tensor_tensor(out=gw[:, tb, :], in0=mk2[:], scalar=S(11), in1=gw[:, tb, :], op0=ALU.mult, op1=ALU.add),
                             reads=[mk2, sm, gw], writes=[gw], merge=True)
                    f.op(V, lambda: nc.vector.tensor_copy(out=gwh[:], in_=gw[:]), reads=[gw], writes=[gwh])
                    f.op(V, lambda: nc.vector.tensor_tensor(out=gwr[:], in0=gw[:], in1=gwh[:], op=ALU.subtract), reads=[gw, gwh], writes=[gwr])
                    f.op(V, lambda: nc.vector.tensor_copy(out=gwl[:], in_=gwr[:]), reads=[gwr], writes=[gwl])
                    f.op(V, lambda: nc.vector.tensor_copy(out=Gmb[:], in_=GmA[:]), reads=[GmA], writes=[Gmb])
                    for tb in range(NQB):
                        f.op(P, lambda: nc.tensor.matmul(psk[:, 0:8], lhsT=tri_b[:], rhs=Gmb[:, tb, :], start=True, stop=(tb == 0)),
                             reads=[tri_b, Gmb], writes=[psk], acc=True)
                        for t2 in range(tb):
                            f.op(P, lambda: nc.tensor.matmul(psk[:, 0:8], lhsT=ones[:], rhs=Gmb[:, t2, :], start=False, stop=(t2 == tb - 1)),
                                 reads=[ones, Gmb], writes=[psk], acc=True)
                        f.op(V, lambda: nc.vector.tensor_tensor(out=tmp8[:], in0=psk[:, 0:8], in1=goff[:], op=ALU.add), reads=[psk, goff], writes=[tmp8])
                        f.op(V, lambda: nc.vector.tensor_tensor(out=tmp8[:], in0=tmp8[:], in1=GmA[:, tb, :], op=ALU.mult), reads=[tmp8, GmA], writes=[tmp8])
                        f.op(V, lambda: nc.vector.reduce_sum(out=slot[:, tb:tb + 1], in_=tmp8[:], axis=AX.X), reads=[tmp8], writes=[slot], merge=True)
                        f.op(V, lambda: nc.vector.tensor_scalar(out=Perm[:, tb, :], in0=iota[:], scalar1=slot[:, tb:tb + 1], scalar2=None, op0=ALU.is_equal),
                             reads=[iota, slot], writes=[Perm], merge=True)
                    for sb in range(NSB):
                        pg2 = psg_[sb % 2]
                        n_ = 0
                        for part in (gwh, gwl):
                            for tb in range(NQB):
                                f.op(P, lambda: nc.tensor.matmul(pg2[:, 0:64], lhsT=Perm[:, tb, sb * 128:(sb + 1) * 128], rhs=part[:, tb, :],
                                                                  start=(n_ == 0), stop=(n_ == 2 * NQB - 1)), reads=[Perm, part], writes=[pg2], acc=True)
                                n_ += 1
                        f.op(V, lambda: nc.vector.tensor_copy(out=gws[:, sb, :], in_=pg2[:, 0:64]), reads=[pg2], writes=[gws], merge=True)
                    f.barrier()
                with ExitStack() as st:
                    hgs = [tl(st, f"hg{i}", [128, 4, RS], BF16) for i in range(2)]
                    pps = [f.ptile(st, f"pp{i}", [128, 512], F32) for i in range(4)]
                    hi = 0
                    for g in range(8):
                        for c4 in range(0, KC, 4):
                            hg = hgs[hi % 2]
                            hi += 1
                            for cc in range(4):
                                c = c4 + cc
                                pp = pps[cc]
                                for tb in range(NQB):
                                    f.op(P, lambda: nc.tensor.matmul(pp[:, 0:RS], lhsT=xb_all[:, tb, c * 128:(c + 1) * 128], rhs=Perm[:, tb, g * RS:(g + 1) * RS],
                                                                      start=(tb == 0), stop=(tb == NQB - 1)), reads=[xb_all, Perm], writes=[pp], acc=True)
                                f.op(V, lambda: nc.vector.tensor_scalar(out=hg[:, cc, :], in0=pp[:, 0:RS], scalar1=ffng[:, c:c + 1], scalar2=None, op0=ALU.mult),
                                     reads=[pp, ffng], writes=[hg], merge=True)
                            f.dma(SP, h2gT_d[g, c4:c4 + 4, :, :].rearrange("c p t -> p c t"), hg[:], reads=[hg], writes=[dram["h2gT_d"]], sb=hg, merge=True)
                    f.barrier()
            with ExitStack() as st:
                pans = [tl(st, f"pan{i}", [128, KC, 512], BF16) for i in range(3)]
                h2gs = [tl(st, f"h2g{i}", [128, KC, RS], BF16) for i in range(2)]
                sgs = [tl(st, f"sg{i}", [128, 512]) for i in range(2)]
                acts = [tl(st, f"act{i}", [128, 512], BF16) for i in range(2)]
                atbs = [tl(st, f"atb{i}", [128, 4, 128], BF16) for i in range(2)]
                psg = [f.ptile(st, f"psg{i}", [128, 512], F32) for i in range(2)]
                psu = [f.ptile(st, f"psu{i}", [128, 512], F32) for i in range(2)]
                tpss = [f.ptile(st, f"tpa{i}", [128, 4, 128], BF16) for i in range(2)]
                pi_ = 0
                ci = 0
                for g in range(8):
                    h2g = h2gs[g % 2]
                    f.dma(SP, h2g[:], h2gT_d[g].rearrange("c p t -> p c t"), writes=[h2g], sb=h2g)
                    for el in range(8):
                        e = g * 8 + el
                        pg_, pu_ = pans[pi_ % 3], pans[(pi_ + 1) % 3]
                        pi_ += 2
                        load_panel(pg_, w_eg[e], 0, KC, 0, 512)
                        load_panel(pu_, w_eu[e], 0, KC, 0, 512)
                        for sl in range(SBG):
                            sb = g * SBG + sl
                            pG, pU, sg, act, atb, tpa = psg[ci % 2], psu[ci % 2], sgs[ci % 2], acts[ci % 2], atbs[ci % 2], tpss[ci % 2]
                            ci += 1
                            mm_tm(pG, h2g, sl * 128, pg_, 0, 512, KC)
                            mm_tm(pU, h2g, sl * 128, pu_, 0, 512, KC)
                            f.op(A, lambda: nc.scalar.activation(out=sg[:], in_=pG[:], func=AF.Silu), reads=[pG], writes=[sg])
                            f.op(V, lambda: nc.vector.scalar_tensor_tensor(out=act[:], in0=pU[:], scalar=gws[:, sb, e:e + 1], in1=sg[:], op0=ALU.mult, op1=ALU.mult),
                                 reads=[pU, gws, sg], writes=[act])
                            for k in range(4):
                                f.op(P, lambda: nc.tensor.transpose(out=tpa[:, k, :], in_=act[:, k * 128:(k + 1) * 128], identity=ident[:]),
                                     reads=[act, ident], writes=[tpa], acc=True)
                            f.op(V, lambda: nc.vector.tensor_copy(out=atb[:], in_=tpa[:]), reads=[tpa], writes=[atb])
                            f.dma(SP, actT_d[e, :, :, sl * 128:(sl + 1) * 128].rearrange("c p t -> p c t"), atb[:],
                                  reads=[atb], writes=[dram["actT_d"]], sb=atb, merge=True)
                f.barrier()
            if stop_after <= 8:
                return finish(nc, f, dram)
            with ExitStack() as st:
                PermT = tl(st, "PermT", [128, NSB, TL], BF16)
                ysh = tl(st, "ysh", [128, NSB, 512], BF16)
                ysl = tl(st, "ysl", [128, NSB, 512], BF16)
                ysr = tl(st, "ysr", [128, 512])
                wds = [tl(st, f"wd{i}", [128, 4, 512], BF16) for i in range(3)]
                ats = [tl(st, f"at{i}", [128, 4, RS], BF16) for i in range(3)]
                xts = [tl(st, f"xt{i}", [128, 512]) for i in range(2)]
                pys = [f.ptile(st, f"py{i}", [128, 512], F32) for i in range(4)]
                pos_ = [f.ptile(st, f"po{i}", [128, 512], F32) for i in range(2)]
                ptp = [f.ptile(st, f"ptp{i}", [128, 8, 128], BF16) for i in range(2)]
                for sb in range(NSB):
                    pt_ = ptp[sb % 2]
                    for tb in range(NQB):
                        f.op(P, lambda: nc.tensor.transpose(out=pt_[:, tb, :], in_=Perm[:, tb, sb * 128:(sb + 1) * 128], identity=ident[:]),
                             reads=[Perm, ident], writes=[pt_], acc=True)
                    f.op(V, lambda: nc.vector.tensor_copy(out=PermT[:, sb, :], in_=pt_[:].rearrange("p a b -> p (a b)")), reads=[pt_], writes=[PermT], merge=True)
                wi = 0
                xi = 0
                for dp in range(8):
                    for g in range(8):
                        py = pys[(g % 2) * SBG:(g % 2) * SBG + SBG]
                        for el in range(8):
                            e = g * 8 + el
                            wd, at = wds[wi % 3], ats[wi % 3]
                            wi += 1
                            load_panel(wd, w_ed, e * 512, 4, dp * 512, 512)
                            f.dma(SP, at[:], actT_d[e].rearrange("c p t -> p c t"), writes=[at], sb=at)
                            for sl in range(SBG):
                                for k in range(4):
                                    f.op(P, lambda: nc.tensor.matmul(py[sl][:, :], lhsT=at[:, k, sl * 128:(sl + 1) * 128], rhs=wd[:, k, :],
                                                                      start=(el == 0 and k == 0), stop=(el == 7 and k == 3)),
                                         reads=[at, wd], writes=[py[sl]], acc=True)
                        for sl in range(SBG):
                            sb = g * SBG + sl
                            f.op(A, lambda: nc.scalar.copy(out=ysh[:, sb, :], in_=py[sl][:]), reads=[py[sl]], writes=[ysh], merge=True)
                            f.op(V, lambda: nc.vector.tensor_tensor(out=ysr[:], in0=py[sl][:], in1=ysh[:, sb, :], op=ALU.subtract), reads=[py[sl], ysh], writes=[ysr])
                            f.op(V, lambda: nc.vector.tensor_copy(out=ysl[:, sb, :], in_=ysr[:]), reads=[ysr], writes=[ysl], merge=True)
                    for tb in range(NQB):
                        po, xt = pos_[xi % 2], xts[xi % 2]
                        xi += 1
                        f.dma(SP, xt[:], x1s[tb * 128:(tb + 1) * 128, dp * 512:(dp + 1) * 512], writes=[xt], sb=xt)
                        n_ = 0
                        for part in (ysh, ysl):
                            for sb in range(NSB):
                                f.op(P, lambda: nc.tensor.matmul(po[:, :], lhsT=PermT[:, sb, tb * 128:(tb + 1) * 128], rhs=part[:, sb, :],
                                                                  start=(n_ == 0), stop=(n_ == 2 * NSB - 1)), reads=[PermT, part], writes=[po], acc=True)
                                n_ += 1
                        f.op(V, lambda: nc.vector.tensor_tensor(out=xt[:], in0=po[:], in1=xt[:], op=ALU.add), reads=[po, xt], writes=[xt])
                        f.dma(SP, out[tb * 128:(tb + 1) * 128, dp * 512:(dp + 1) * 512], xt[:], reads=[xt], writes=[dram["out"]], sb=xt, merge=True)
                f.barrier()
        return finish(nc, f, dram)

    return nc


def finish(nc, f, dram):
    deps = {}
    for b in f.dma_bufs:
        if b.dcount > 0:
            deps[id(b.dsem)] = (b.dsem, b.dcount)
    for (s_, c_) in f.sem_pool + f.retired:
        deps[id(s_)] = (s_, c_)
    for e in f.engs:
        if e.count > 0:
            deps[id(e.sem)] = (e.sem, e.count)
    f._wait(f.sp, deps)
    return nc


def host_consts():
    ident = np.eye(128, dtype=np.float32)
    rot = np.zeros((64, 64), np.float32)
    for m in range(32):
        rot[m + 32, m] = -1.0
    for m in range(32, 64):
        rot[m - 32, m] = 1.0
    tri = np.triu(np.ones((128, 128), np.float32), 1)
    iota = np.broadcast_to(np.arange(NSLOT, dtype=np.float32), (128, NSLOT)).copy()
    half = 32
    inv = (np.float32(10000.0) ** (-np.arange(half, dtype=np.float32) / np.float32(half))).astype(np.float32)
    invf = np.concatenate([inv, inv]).reshape(64, 1).astype(np.float32)
    goff = np.broadcast_to(np.arange(8, dtype=np.float32) * R_SLOT, (128, 8)).copy()
    return {"c_goff": goff, "c_ident": ident, "c_rot": rot, "c_tri": tri, "c_iota": iota, "c_invf": invf}


def chunked(v, nch):
    return np.ascontiguousarray(np.asarray(v, np.float32).reshape(nch, 128).T)


def make_in_maps(inp, cores):
    a = {k: np.asarray(v) for k, v in inp.items()}
    x = a["x"][0]
    pos = a["positions"][0].astype(np.int32)
    shared = {
        "x_full": x,
        "mem": a["mem"][0],
        "pos_full": pos.reshape(1, T),
        "rel_bias": a["rel_bias"].reshape(1, 32 * 12),
        "mix_norm_g": chunked(a["mix_norm_g"][0], KC),
        "w_in": a["w_in"][0],
        "diff_q_norm_g": a["diff_q_norm_g"][0].reshape(128, 1),
        "diff_k_norm_g": a["diff_k_norm_g"][0].reshape(128, 1),
        "lam_v": np.concatenate([a["diff_lambda_q1"][0], a["diff_lambda_k1"][0], a["diff_lambda_q2"][0],
                                 a["diff_lambda_k2"][0]]).reshape(1, 512),
        "diff_subln_g": a["diff_subln_g"][0].reshape(1, 256),
        "mla_cq_norm_g": chunked(a["mla_cq_norm_g"][0], 12),
        "mla_ckv_norm_g": chunked(a["mla_ckv_norm_g"][0], 4),
        "mla_w_uq": a["mla_w_uq"][0],
        "mla_w_ukv": a["mla_w_ukv"][0],
        "mla_q_norm_g": a["mla_q_norm_g"][0].reshape(192, 1),
        "mla_k_norm_g": a["mla_k_norm_g"][0].reshape(192, 1),
        "mem_norm_g": chunked(a["mem_norm_g"][0], KC),
        "mem_w_kv": a["mem_w_kv"][0],
        "mem_q_norm_g": chunked(a["mem_q_norm_g"][0], 2),
        "mem_k_norm_g": chunked(a["mem_k_norm_g"][0], 2),
        "w_o_diff": a["w_o_diff"][0],
        "w_o_mla": a["w_o_mla"][0],
        "w_o_mem": a["w_o_mem"][0],
        "w_out": a["w_out"][0],
        "ffn_norm_g": chunked(a["ffn_norm_g"][0], KC),
        "w_rt": np.ascontiguousarray(np.concatenate([a["w_route_group"][0], a["w_route_expert"][0]], axis=1)),
        "b_rt": np.concatenate([a["b_route_group"][0], a["b_route_expert"][0]]).reshape(1, 72),
        "w_exp_gate": a["w_exp_gate"][0],
        "w_exp_up": a["w_exp_up"][0],
        "w_exp_down": a["w_exp_down"][0].reshape(64 * 512, D),
    }
    shared.update(host_consts())
    maps = []
    xb = x.reshape(T // 128, 128, D)
    pb = pos.reshape(T // 128, 128)
    for c in cores:
        m = dict(shared)
        m["x_own"] = np.ascontiguousarray(xb[c::NCORES].reshape(TL, D))
        m["pos_own"] = np.ascontiguousarray(pb[c::NCORES].reshape(1, TL))
        maps.append(m)
    return maps


def kernel(**inputs):
    nc = build()
    cores = list(range(NCORES))
    in_maps = make_in_maps(inputs, cores)
    res = run_bass_kernel_spmd(nc, in_maps, core_ids=cores)
    outb = np.zeros((T // 128, 128, D), np.float32)
    for c in cores:
        outb[c::NCORES] = np.asarray(res.results[c]["out"]).reshape(TL // 128, 128, D)
    return outb.reshape(1, T, D)
```

```python
import math
from contextlib import ExitStack
import numpy as np
import concourse.bass as bass
import concourse.mybir as mybir
from concourse.bass_utils import run_bass_kernel_spmd

F32 = mybir.dt.float32
BF16 = mybir.dt.bfloat16
I32 = mybir.dt.int32
AF = mybir.ActivationFunctionType
ALU = mybir.AluOpType
AX = mybir.AxisListType

NCORES = 8
T = 8192
TL = 1024
D = 4096
KC = 32
NMEM = 256
EPS = 1e-6
IN_COLS = 20032
C_DQ, C_DK, C_DV, C_CQ, C_CKV, C_KR, C_MQ, C_G = 0, 1536, 3072, 4608, 6144, 6656, 6720, 7744
R_SLOT = 256
NSLOT = 8 * R_SLOT
SEM_CAP = 24000


class Buf:
    __slots__ = ("name", "writes", "reads", "dsem", "dcount")

    def __init__(self, name):
        self.name = name
        self.writes = {}
        self.reads = {}
        self.dsem = None
        self.dcount = 0


class TT:
    def __init__(self, t, name):
        self.t = t
        self.b = Buf(name)
        self.psum = False

    def __getitem__(self, k):
        return self.t[k]


class Eng:
    def __init__(self, fw, name, h):
        self.fw, self.name, self.h = fw, name, h
        self.sem = None
        self.count = 0
        self.waited = {}
        self.allsems = []

    def new_sem(self):
        self.sem = self.fw.alloc_sem(self.name)
        self.count = 0
        self.allsems.append(self.sem)


class FW:
    def __init__(self, nc, stack):
        self.nc = nc
        self.stack = stack
        self.nsem = 0
        self.pe = Eng(self, "pe", nc.tensor)
        self.act = Eng(self, "act", nc.scalar)
        self.dve = Eng(self, "dve", nc.vector)
        self.pool = Eng(self, "pool", nc.gpsimd)
        self.sp = Eng(self, "sp", nc.sync)
        self.engs = [self.pe, self.act, self.dve, self.pool, self.sp]
        for e in self.engs:
            e.new_sem()
        self.ninst = 0
        self.dma_bufs = []
        self.sem_pool = []
        self.retired = []
        self.uid = 0

    def alloc_sem(self, name):
        self.nsem += 1
        return self.stack.enter_context(self.nc.semaphore(f"s{self.nsem}_{name}"))

    def tile(self, st, name, shape, dt):
        self.uid += 1
        nm = f"{name}_{self.uid}"
        tt = TT(st.enter_context(self.nc.sbuf_tensor(nm, shape, dt)), nm)
        if st is not self.stack:
            st.callback(self.release, tt)
        return tt

    def release(self, tt):
        b = tt.b
        if b.dsem is not None:
            if b in self.dma_bufs:
                self.dma_bufs.remove(b)
            if b.dcount < SEM_CAP - 4000:
                self.sem_pool.append((b.dsem, b.dcount))
            else:
                self.retired.append((b.dsem, b.dcount))
            b.dsem = None

    def ptile(self, st, name, shape, dt):
        self.uid += 1
        nm = f"{name}_{self.uid}"
        tt = TT(st.enter_context(self.nc.psum_tensor(nm, shape, dt)), nm)
        tt.psum = True
        return tt

    def _wait(self, eng, deps):
        for key, (sem, val) in deps.items():
            if eng.waited.get(key, 0) < val:
                eng.h.wait_ge(sem, val)
                eng.waited[key] = val

    def _deps(self, reads, writes, acc):
        deps = {}

        def add(d):
            for k, sv in d.items():
                if k not in deps or deps[k][1] < sv[1]:
                    deps[k] = sv
        for b in reads:
            add(b.b.writes)
            if b.psum:
                add(b.b.reads)
        for b in writes:
            add(b.b.reads)
            add(b.b.writes)
        return deps

    def op(self, eng, fn, reads=(), writes=(), acc=False, merge=False):
        deps = self._deps(reads, writes, acc)
        if acc:
            deps.pop(id(eng.sem), None)
        self._wait(eng, deps)
        if eng.count >= SEM_CAP:
            eng.new_sem()
        inst = fn()
        inst.then_inc(eng.sem, 1)
        eng.count += 1
        self.ninst += 1
        ev = (eng.sem, eng.count)
        key = id(eng.sem)
        for w in writes:
            if merge or acc:
                w.b.writes[key] = ev
            else:
                w.b.writes = {key: ev}
                w.b.reads = {}
        for r in reads:
            r.b.reads[key] = ev
        return inst

    def dma(self, eng, out, in_, reads=(), writes=(), sb=None, merge=False, **kw):
        deps = self._deps(reads, writes, False)
        self._wait(eng, deps)
        b = sb.b
        if b.dsem is not None and b.dcount >= SEM_CAP:
            self.retired.append((b.dsem, b.dcount))
            self.dma_bufs.remove(b)
            b.dsem = None
        if b.dsem is None:
            if self.sem_pool:
                b.dsem, b.dcount = self.sem_pool.pop()
            else:
                b.dsem = self.alloc_sem("d")
                b.dcount = 0
            self.dma_bufs.append(b)
        inst = eng.h.dma_start(out=out, in_=in_, **kw)
        inst.then_inc(b.dsem, 16)
        b.dcount += 16
        self.ninst += 1
        ev = (b.dsem, b.dcount)
        key = id(b.dsem)
        for w in writes:
            if merge:
                w.b.writes[key] = ev
            else:
                w.b.writes = {key: ev}
                w.b.reads = {}
        for r in reads:
            r.b.reads[key] = ev
        return inst

    def barrier(self):
        deps = {}
        for e in self.engs:
            if e.count > 0:
                deps[id(e.sem)] = (e.sem, e.count)
        for b in self.dma_bufs:
            if b.dcount > 0:
                deps[id(b.dsem)] = (b.dsem, b.dcount)
        for (s_, c_) in self.retired:
            deps[id(s_)] = (s_, c_)
        for e in self.engs:
            self._wait(e, deps)


def t5_thresholds():
    n = np.arange(0, 4096, dtype=np.int32)
    max_exact = 16
    nf = np.maximum(n, 1).astype(np.float32)
    large = max_exact + (np.log(nf / np.float32(max_exact)) / np.float32(math.log(128 / 16))
                         * np.float32(16)).astype(np.int32)
    large = np.minimum(large, 31)
    bucket = np.where(n < max_exact, n, large)
    thr = []
    for b in range(1, 32):
        thr.append(int(np.argmax(bucket >= b)))
    return thr


def build(stop_after=99, dbg=False, skip=()):
    nc = bass.Bass("TRN2", target_bir_lowering=False)

    def din(name, shape, dt=F32):
        return nc.dram_tensor(name, list(shape), dt, kind="ExternalInput").ap()

    def dscr(name, shape, dt):
        kind = "ExternalOutput" if dbg else "Internal"
        return nc.dram_tensor(name, list(shape), dt, kind=kind).ap()

    x_full = din("x_full", [T, D])
    x_own = din("x_own", [TL, D])
    mem = din("mem", [NMEM, D])
    pos_full = din("pos_full", [1, T], I32)
    pos_own = din("pos_own", [1, TL], I32)
    rel_bias = din("rel_bias", [1, 32 * 12])
    mix_g = din("mix_norm_g", [128, KC])
    w_in = din("w_in", [D, IN_COLS])
    dq_g = din("diff_q_norm_g", [128, 1])
    dk_g = din("diff_k_norm_g", [128, 1])
    lam_v = din("lam_v", [1, 4 * 128])
    subln_g = din("diff_subln_g", [1, 256])
    cq_g = din("mla_cq_norm_g", [128, 12])
    ckv_g = din("mla_ckv_norm_g", [128, 4])
    w_uq = din("mla_w_uq", [1536, 2304])
    w_ukv = din("mla_w_ukv", [512, 3072])
    mq_g = din("mla_q_norm_g", [192, 1])
    mk_g = din("mla_k_norm_g", [192, 1])
    mem_g = din("mem_norm_g", [128, KC])
    mem_wkv = din("mem_w_kv", [D, 2048])
    memq_g = din("mem_q_norm_g", [128, 2])
    memk_g = din("mem_k_norm_g", [128, 2])
    w_o_diff = din("w_o_diff", [1536, D])
    w_o_mla = din("w_o_mla", [1536, D])
    w_o_mem = din("w_o_mem", [1024, D])
    w_out = din("w_out", [D, D])
    ffn_g = din("ffn_norm_g", [128, KC])
    w_rt = din("w_rt", [D, 72])
    b_rt = din("b_rt", [1, 72])
    if stop_after >= 6:
        w_eg = din("w_exp_gate", [64, D, 512])
        w_eu = din("w_exp_up", [64, D, 512])
        w_ed = din("w_exp_down", [64 * 512, D])
    c_ident = din("c_ident", [128, 128])
    c_rot = din("c_rot", [64, 64])
    c_tri = din("c_tri", [128, 128])
    c_iota = din("c_iota", [128, NSLOT])
    c_invf = din("c_invf", [64, 1])
    c_goff = din("c_goff", [128, 8])
    out = nc.dram_tensor("out", [TL, D], F32, kind="ExternalOutput").ap()

    hT_all = dscr("hT_all", [KC, 128, T], BF16)
    QdT = dscr("QdT", [12, 128, TL], BF16)
    KdT = dscr("KdT", [12, 128, T], BF16)
    Vd1 = dscr("Vd1", [6, T, 257], BF16)
    QmT = dscr("QmT", [12, 192, TL], BF16)
    KmT = dscr("KmT", [12, 192, T], BF16)
    Vm1 = dscr("Vm1", [12, T, 129], BF16)
    QeT = dscr("QeT", [4, 256, TL], BF16)
    gT = dscr("gT", [96, 128, TL], F32)
    x1s = dscr("x1s", [TL, D], F32)
    ys = dscr("ys", [NSLOT, D], F32)

    thr = t5_thresholds()

    with ExitStack() as top:
        f = FW(nc, top)
        V, A, P, G = f.dve, f.act, f.pe, f.pool
        SP = f.sp
        dram = {n: TT(None, n) for n in ["hT_all", "QdT", "KdT", "Vd1", "QmT", "KmT", "Vm1", "QeT", "gT", "x1s", "ys", "out"]}

        def tl(st, name, shape, dt=F32):
            return f.tile(st, name, shape, dt)

        ident_f = tl(top, "ident_f", [128, 128])
        ident = tl(top, "ident", [128, 128], BF16)
        ones = tl(top, "ones", [128, 128], BF16)
        rotT = tl(top, "rotT", [64, 64], BF16)
        rot_f = tl(top, "rot_f", [64, 64])
        mixg = tl(top, "mixg", [128, KC])
        dqg = tl(top, "dqg", [128, 1])
        dkg = tl(top, "dkg", [128, 1])
        cqg = tl(top, "cqg", [128, 12])
        ckvg = tl(top, "ckvg", [128, 4])
        mqgA = tl(top, "mqgA", [128, 1])
        mqgB = tl(top, "mqgB", [64, 1])
        mkgA = tl(top, "mkgA", [128, 1])
        mkgB = tl(top, "mkgB", [64, 1])
        memqg = tl(top, "memqg", [128, 2])
        invf = tl(top, "invf", [64, 1])
        memkg = tl(top, "memkg", [128, 2])
        memg = tl(top, "memg", [128, KC])
        ffng = tl(top, "ffng", [128, KC])
        for dst, src in [(ident_f, c_ident), (rot_f, c_rot), (mixg, mix_g), (dqg, dq_g), (dkg, dk_g), (cqg, cq_g),
                         (ckvg, ckv_g), (mqgA, mq_g[0:128, :]), (mqgB, mq_g[128:192, :]), (mkgA, mk_g[0:128, :]),
                         (mkgB, mk_g[128:192, :]), (memqg, memq_g), (invf, c_invf), (memkg, memk_g), (memg, mem_g), (ffng, ffn_g)]:
            f.dma(SP, dst[:], src, writes=[dst], sb=dst)
        f.op(V, lambda: nc.vector.tensor_copy(out=ident[:], in_=ident_f[:]), reads=[ident_f], writes=[ident])
        f.op(V, lambda: nc.vector.tensor_copy(out=rotT[:], in_=rot_f[:]), reads=[rot_f], writes=[rotT])
        f.op(V, lambda: nc.vector.memset(ones[:], 1.0), writes=[ones])

        NQB = TL // 128
        NTT_ALL = T // 512
        NTT_OWN = TL // 512
        hT_ownD = dscr("hT_ownD", [KC, 128, TL], BF16)
        dram["hT_ownD"] = TT(None, "hT_ownD")
        epsb = tl(top, "epsb", [128, 1])
        f.op(V, lambda: nc.vector.memset(epsb[:], EPS), writes=[epsb])

        def rstd_from(ps_ss, out_t, inv_n, np_=128, n=512):
            f.op(A, lambda: nc.scalar.activation(out=out_t[0:np_, 0:n], in_=ps_ss[0:np_, 0:n], func=AF.Sqrt,
                                                  scale=inv_n, bias=epsb[0:np_, 0:1]),
                 reads=[ps_ss, epsb], writes=[out_t])
            f.op(V, lambda: nc.vector.reciprocal(out=out_t[0:np_, 0:n], in_=out_t[0:np_, 0:n]),
                 reads=[out_t], writes=[out_t])

        def phase_norm(src, ntiles, gain_t, dstT, dkey, ss_out=None):
            with ExitStack() as st:
                xts = [tl(st, f"xt{i}", [128, D]) for i in range(2)]
                xbs = [tl(st, f"xb{i}", [128, D], BF16) for i in range(2)]
                sss = [tl(st, f"ss{i}", [128, 1]) for i in range(2)]
                junk = tl(st, "junk", [128, D], BF16)
                hts = [tl(st, f"ht{i}", [128, KC, 128], BF16) for i in range(2)]
                pts = [f.ptile(st, f"pt{i}", [128, 8, 128], BF16) for i in range(4)]
                for i in range(ntiles):
                    xt, xb, ss, ht = xts[i % 2], xbs[i % 2], sss[i % 2], hts[i % 2]
                    f.dma(SP, xt[:], src[i * 128:(i + 1) * 128, :], writes=[xt], sb=xt)
                    f.op(A, lambda: nc.scalar.activation(out=junk[:], in_=xt[:], func=AF.Square, accum_out=ss[:]),
                         reads=[xt], writes=[junk, ss])
                    f.op(A, lambda: nc.scalar.activation(out=ss[:], in_=ss[:], func=AF.Sqrt, scale=1.0 / D, bias=epsb[:, 0:1]),
                         reads=[ss, epsb], writes=[ss])
                    f.op(V, lambda: nc.vector.reciprocal(out=ss[:], in_=ss[:]), reads=[ss], writes=[ss])
                    f.op(A, lambda: nc.scalar.activation(out=xb[:], in_=xt[:], func=AF.Copy, scale=ss[:, 0:1]),
                         reads=[xt, ss], writes=[xb])
                    for q in range(4):
                        pt = pts[q]
                        for k in range(8):
                            c = q * 8 + k
                            f.op(P, lambda: nc.tensor.transpose(out=pt[:, k, :], in_=xb[:, c * 128:(c + 1) * 128], identity=ident[:]),
                                 reads=[xb, ident], writes=[pt], acc=True)
                        gb = gain_t[:, q * 8:(q + 1) * 8].unsqueeze(2).to_broadcast([128, 8, 128])
                        f.op(V, lambda: nc.vector.tensor_tensor(out=ht[:, q * 8:(q + 1) * 8, :], in0=pt[:], in1=gb, op=ALU.mult),
                             reads=[pt, gain_t], writes=[ht], merge=True)
                    f.dma(A, dstT[:, :, i * 128:(i + 1) * 128].rearrange("c p t -> p c t"), ht[:],
                          reads=[ht], writes=[dram[dkey]], sb=ht, merge=True)
                f.barrier()

        if "p1" not in skip:
            phase_norm(x_full, T // 128, mixg, hT_all, "hT_all")
            phase_norm(x_own, TL // 128, mixg, hT_ownD, "hT_ownD")
        if stop_after <= 1:
            return finish(nc, f, dram)

        pan_serial = TT(None, "pan_serial")

        def load_panel(pan, W, r0, nk, c0, ncols):
            f.dma(G, pan[:, 0:nk, 0:ncols], W[r0:r0 + nk * 128, c0:c0 + ncols].rearrange("(k p) c -> p k c", p=128),
                  writes=[pan], sb=pan)

        def load_hT(ht, srcT, t0, nk=KC, n=512, k0=0):
            f.dma(SP, ht[:, 0:nk, 0:n], srcT[k0:k0 + nk, :, t0:t0 + n].rearrange("c p t -> p c t"),
                  reads=[dram_of[id(srcT)]] if id(srcT) in dram_of else [], writes=[ht], sb=ht)

        dram_of = {id(hT_all): dram["hT_all"], id(hT_ownD): dram["hT_ownD"]}

        def mm_fm(ps, np_, pan, c0, ht, nk, n=512, first=True, last=True, extra_reads=()):
            for k in range(nk):
                f.op(P, lambda: nc.tensor.matmul(ps[0:np_, 0:n], lhsT=pan[:, k, c0:c0 + np_], rhs=ht[:, k, 0:n],
                                                  start=(first and k == 0), stop=(last and k == nk - 1)),
                     reads=[pan, ht], writes=[ps], acc=True)

        def mm_tm(ps, ht, t0, pan, c0, ncols, nk):
            for k in range(nk):
                f.op(P, lambda: nc.tensor.matmul(ps[:, 0:ncols], lhsT=ht[:, k, t0:t0 + 128], rhs=pan[:, k, c0:c0 + ncols],
                                                  start=(k == 0), stop=(k == nk - 1)),
                     reads=[pan, ht], writes=[ps], acc=True)

        def fm_rms(st_tiles, chunks, dim, gains, outs, pre_ss=None, n=512):
            sq, pss, rstd = st_tiles
            nchk = len(chunks)
            for i, (ps, np_) in enumerate(chunks):
                f.op(A, lambda: nc.scalar.activation(out=sq[0:np_, 0:n], in_=ps[0:np_, 0:n], func=AF.Square),
                     reads=[ps], writes=[sq])
                f.op(P, lambda: nc.tensor.matmul(pss[:, 0:n], lhsT=ones[0:np_, :], rhs=sq[0:np_, 0:n],
                                                  start=(i == 0 and pre_ss is None), stop=(i == nchk - 1)),
                     reads=[ones, sq], writes=[pss], acc=True)
            rstd_from(pss, rstd, 1.0 / dim, n=n)
            for (ps, np_), g, (o, _) in zip(chunks, gains, outs):
                f.op(V, lambda: nc.vector.scalar_tensor_tensor(out=o[0:np_, 0:n], in0=ps[0:np_, 0:n], scalar=g, in1=rstd[0:np_, 0:n],
                                                                op0=ALU.mult, op1=ALU.mult),
                     reads=[ps, rstd], writes=[o])
            return rstd

        def pass_fm_rms(srcT, ntt, c0, nmaps, grp, dim, gain_fn, dstT, dkey, row_fn, W=None, n=512):
            W = w_in if W is None else W
            with ExitStack() as st:
                pans = [tl(st, f"pan{i}", [128, KC, 512], BF16) for i in range(2)]
                hts = [tl(st, f"ht{i}", [128, KC, 512], BF16) for i in range(2)]
                sq = tl(st, "sq", [128, 512], BF16)
                rstd = tl(st, "rstd", [128, 512])
                obs = [tl(st, f"ob{i}", [128, 512], BF16) for i in range(4)]
                pss = f.ptile(st, "pss", [128, 512], F32)
                psc = [f.ptile(st, f"psc{i}", [128, 512], F32) for i in range(4)]
                npan = (nmaps + 3) // 4
                it = 0
                for pi in range(npan):
                    pan = pans[pi % 2]
                    ncol = min(512, (nmaps - pi * 4) * 128)
                    load_panel(pan, W, 0, KC, c0 + pi * 512, ncol)
                    for tt in range(ntt):
                        ht = hts[it % 2]
                        it += 1
                        load_hT(ht, srcT, tt * n, n=n)
                        for sg in range(0, ncol // 128, grp):
                            chunks = []
                            for s_ in range(grp):
                                ps = psc[(sg + s_) % 4]
                                mm_fm(ps, 128, pan, (sg + s_) * 128, ht, KC, n=n)
                                chunks.append((ps, 128))
                            outs = [(obs[(sg + s_) % 4], 128) for s_ in range(grp)]
                            fm_rms((sq, pss, rstd), chunks, dim, [gain_fn(s_) for s_ in range(grp)], outs, n=n)
                            for s_ in range(grp):
                                m = pi * 4 + sg + s_
                                f.dma(A, row_fn(m)[:, tt * n:(tt + 1) * n], outs[s_][0][:, 0:n],
                                      reads=[outs[s_][0]], writes=[dram[dkey]], sb=outs[s_][0], merge=True)
                f.barrier()

        if "p2" not in skip:
            pass_fm_rms(hT_ownD, NTT_OWN, C_DQ, 12, 1, 128, lambda s_: dqg[:, 0:1], QdT, "QdT", lambda m: QdT[m])
            pass_fm_rms(hT_all, NTT_ALL, C_DK, 12, 1, 128, lambda s_: dkg[:, 0:1], KdT, "KdT", lambda m: KdT[m])
            pass_fm_rms(hT_ownD, NTT_OWN, C_MQ, 8, 2, 256, lambda s_: memqg[:, s_:s_ + 1], QeT, "QeT",
                        lambda m: QeT[m // 2, (m % 2) * 128:(m % 2) * 128 + 128, :])
        if stop_after <= 2:
            return finish(nc, f, dram)

        if "p3" not in skip:
            with ExitStack() as st:
                pans = [tl(st, f"pan{i}", [128, KC, 512], BF16) for i in range(2)]
                hts = [tl(st, f"ht{i}", [128, KC, 512], BF16) for i in range(2)]
                gos = [tl(st, f"go{i}", [128, 512]) for i in range(4)]
                v1s = [tl(st, f"v1{i}", [128, 2, 257], BF16) for i in range(2)]
                psc = [f.ptile(st, f"psc{i}", [128, 512], F32) for i in range(4)]
                for v1 in v1s:
                    f.op(V, lambda: nc.vector.memset(v1[:], 1.0), writes=[v1])
                it = 0
                for pi in range(24):
                    pan = pans[pi % 2]
                    load_panel(pan, w_in, 0, KC, C_G + pi * 512, 512)
                    for tt in range(NTT_OWN):
                        ht = hts[it % 2]
                        it += 1
                        load_hT(ht, hT_ownD, tt * 512)
                        for sc in range(4):
                            ps, go = psc[sc], gos[sc]
                            mm_fm(ps, 128, pan, sc * 128, ht, KC)
                            f.op(A, lambda: nc.scalar.activation(out=go[:], in_=ps[:], func=AF.Sigmoid), reads=[ps], writes=[go])
                            f.dma(A, gT[pi * 4 + sc, :, tt * 512:(tt + 1) * 512], go[:], reads=[go], writes=[dram["gT"]], sb=go, merge=True)
                for pi in range(3):
                    pan = pans[pi % 2]
                    load_panel(pan, w_in, 0, KC, C_DV + pi * 512, 512)
                    for tt in range(NTT_ALL):
                        ht = hts[it % 2]
                        it += 1
                        load_hT(ht, hT_all, tt * 512)
                        for tb in range(4):
                            ps, v1 = psc[tb], v1s[tb % 2]
                            mm_tm(ps, ht, tb * 128, pan, 0, 512, KC)
                            f.op(V, lambda: nc.vector.tensor_copy(out=v1[:, :, 0:256], in_=ps[:].rearrange("p (h c) -> p h c", h=2)),
                                 reads=[ps], writes=[v1])
                            r0 = tt * 512 + tb * 128
                            f.dma(A, Vd1[pi * 2:pi * 2 + 2, r0:r0 + 128, :].rearrange("h t c -> t h c"), v1[:],
                                  reads=[v1], writes=[dram["Vd1"]], sb=v1, merge=True)
                f.barrier()
        if stop_after <= 3:
            return finish(nc, f, dram)

        NCg = NCORES
        NEM = NCg + 1
        OT_d = dscr("OT_d", [KC, 128, TL], BF16)
        dram["OT_d"] = TT(None, "OT_d")
        att_st = ExitStack()
        EM = tl(att_st, "EM", [128, 12, NEM * 128], BF16)
        MK = tl(att_st, "MK", [128, NEM * 128], BF16)
        rb = tl(att_st, "rb", [128, 32 * 12])
        lam = tl(att_st, "lam", [128, 1])
        nlam = tl(att_st, "nlam", [128, 1])
        sublng = tl(att_st, "sublng", [128, 256])
        with ExitStack() as st:
            posq_i = tl(st, "posq_i", [128, 128], I32)
            posk_i = tl(st, "posk_i", [128, NEM], I32)
            posq = tl(st, "posq", [128, 128])
            posk = tl(st, "posk", [128, NEM])
            dist = tl(st, "dist", [128, NEM * 128])
            mask = tl(st, "mask", [128, NEM * 128])
            ind = tl(st, "ind", [128, NEM * 128])
            acc = tl(st, "acc", [128, 12, NEM * 128])
            delta = tl(st, "delta", [128, 31 * 12])
            base = tl(st, "base", [128, 12])
            lv = tl(st, "lv", [128, 512])
            lp = tl(st, "lp", [128, 256])
            ls = tl(st, "ls", [128, 2])
            f.dma(SP, posq_i[:], pos_own[0:1, 128:256].partition_broadcast(128), writes=[posq_i], sb=posq_i)
            f.dma(SP, posk_i[:], pos_full[0, (NCg - 1) * 128:(2 * NCg) * 128].rearrange("(i p) -> p i", p=128),
                  writes=[posk_i], sb=posk_i, allow_slow_non_contiguous=True)
            f.dma(SP, rb[:], rel_bias[0:1, :].partition_broadcast(128), writes=[rb], sb=rb)
            f.dma(SP, lv[:], lam_v[0:1, :].partition_broadcast(128), writes=[lv], sb=lv)
            f.dma(SP, sublng[:], subln_g[0:1, :].partition_broadcast(128), writes=[sublng], sb=sublng)
            f.op(V, lambda: nc.vector.tensor_copy(out=posq[:], in_=posq_i[:]), reads=[posq_i], writes=[posq])
            f.op(V, lambda: nc.vector.tensor_copy(out=posk[:], in_=posk_i[:]), reads=[posk_i], writes=[posk])
            for i in range(NEM):
                f.op(V, lambda: nc.vector.tensor_scalar(out=dist[:, i * 128:(i + 1) * 128], in0=posq[:], scalar1=posk[:, i:i + 1],
                                                          scalar2=None, op0=ALU.subtract), reads=[posq, posk], writes=[dist], merge=True)
            f.op(V, lambda: nc.vector.tensor_single_scalar(out=mask[:], in_=dist[:], scalar=0.0, op=ALU.is_ge), reads=[dist], writes=[mask])
            f.op(V, lambda: nc.vector.tensor_copy(out=MK[:], in_=mask[:]), reads=[mask], writes=[MK])
            f.op(V, lambda: nc.vector.tensor_tensor(out=delta[:], in0=rb[:, 12:32 * 12], in1=rb[:, 0:31 * 12], op=ALU.subtract),
                 reads=[rb], writes=[delta])
            f.op(V, lambda: nc.vector.tensor_tensor(out=base[:], in0=rb[:, 0:12], in1=rb[:, 31 * 12:32 * 12], op=ALU.subtract),
                 reads=[rb], writes=[base])
            f.op(G, lambda: nc.gpsimd.memset(acc[:], 0.0), writes=[acc])
            for b in range(1, 32):
                f.op(V, lambda: nc.vector.tensor_single_scalar(out=ind[:], in_=dist[:], scalar=float(thr[b - 1]), op=ALU.is_ge),
                     reads=[dist], writes=[ind])
                for m in range(12):
                    eng = V
                    h_ = nc.vector
                    f.op(eng, lambda: h_.scalar_tensor_tensor(out=acc[:, m, :], in0=ind[:], scalar=delta[:, (b - 1) * 12 + m:(b - 1) * 12 + m + 1],
                                                               in1=acc[:, m, :], op0=ALU.mult, op1=ALU.add),
                         reads=[ind, delta, acc], writes=[acc], merge=True)
            for m in range(12):
                f.op(A, lambda: nc.scalar.activation(out=acc[:, m, :], in_=acc[:, m, :], func=AF.Exp, bias=base[:, m:m + 1]),
                     reads=[acc, base], writes=[acc], merge=True)
                f.op(V, lambda: nc.vector.tensor_tensor(out=EM[:, m, :], in0=acc[:, m, :], in1=mask[:], op=ALU.mult),
                     reads=[acc, mask], writes=[EM], merge=True)
            f.op(V, lambda: nc.vector.tensor_tensor(out=lp[:].rearrange("p (a c) -> p a c", a=2),
                                                      in0=lv[:].rearrange("p (a b c) -> p a b c", a=2, b=2)[:, :, 0, :],
                                                      in1=lv[:].rearrange("p (a b c) -> p a b c", a=2, b=2)[:, :, 1, :], op=ALU.mult),
                 reads=[lv], writes=[lp])
            f.op(V, lambda: nc.vector.reduce_sum(out=ls[:], in_=lp[:].rearrange("p (a c) -> p a c", a=2), axis=AX.X),
                 reads=[lp], writes=[ls])
            f.op(A, lambda: nc.scalar.activation(out=ls[:], in_=ls[:], func=AF.Exp), reads=[ls], writes=[ls])
            f.op(V, lambda: nc.vector.tensor_tensor(out=lam[:], in0=ls[:, 0:1], in1=ls[:, 1:2], op=ALU.subtract), reads=[ls], writes=[lam])
            f.op(V, lambda: nc.vector.tensor_scalar(out=nlam[:], in0=lam[:], scalar1=0.2, scalar2=-1.0, op0=ALU.add, op1=ALU.mult),
                 reads=[lam], writes=[nlam])
            f.op(V, lambda: nc.vector.tensor_scalar(out=sublng[:], in0=sublng[:], scalar1=0.8, scalar2=None, op0=ALU.mult),
                 reads=[sublng], writes=[sublng])
            f.barrier()

        def attention(st, QT_list, KT_list, V1, dv, nkb_fn, scale, bias_ap_fn, em_fn, units, epilogue):
            sps = [f.ptile(st, f"sps{i}", [128, 4, 128], F32) for i in range(2)]
            ops_ = [f.ptile(st, f"ops{i}", [128, 512], F32) for i in range(4)]
            pTs = [tl(st, f"pT{i}", [128, 4, 128], BF16) for i in range(3)]
            gi = 0
            for j in range(NQB):
                nkb = nkb_fn(j)
                for u in range(units):
                    ops = ops_[(j * units + u) % 4]
                    for g0 in range(0, nkb, 4):
                        sp, pT = sps[gi % 2], pTs[gi % 3]
                        gi += 1
                        nb = min(4, nkb - g0)
                        for i in range(nb):
                            kb = g0 + i
                            nch = len(QT_list[u])
                            for ci in range(nch):
                                qt, np_ = QT_list[u][ci]
                                kt, _ = KT_list[u][ci]
                                f.op(P, lambda: nc.tensor.matmul(sp[:, i, :], lhsT=kt[0:np_, kb * 128:(kb + 1) * 128],
                                                                  rhs=qt[0:np_, j * 128:(j + 1) * 128], start=(ci == 0), stop=(ci == nch - 1)),
                                     reads=[kt, qt], writes=[sp], acc=True)
                        bap = bias_ap_fn(u)
                        f.op(A, lambda: nc.scalar.activation(out=pT[:, 0:nb, :], in_=sp[:, 0:nb, :], func=AF.Exp, scale=scale,
                                                              **({"bias": bap} if bap is not None else {})),
                             reads=[sp], writes=[pT])
                        for i in range(nb):
                            kb = g0 + i
                            em = em_fn(u, j, kb)
                            if em is not None:
                                f.op(V, lambda: nc.vector.tensor_tensor(out=pT[:, i, :], in0=pT[:, i, :], in1=em, op=ALU.mult),
                                     reads=[pT, EM, MK], writes=[pT], merge=True)
                        for i in range(nb):
                            kb = g0 + i
                            f.op(P, lambda: nc.tensor.matmul(ops[:, 0:dv + 1], lhsT=pT[:, i, :], rhs=V1[:, kb, :],
                                                              start=(kb == 0), stop=(kb == nkb - 1)),
                                 reads=[pT, V1], writes=[ops], acc=True)
                    epilogue(j, u, ops)

        def run_diff():
            for h in range(6):
                with ExitStack() as st:
                    KT = [tl(st, f"KT{i}", [128, T], BF16) for i in range(2)]
                    QT = [tl(st, f"QT{i}", [128, TL], BF16) for i in range(2)]
                    V1 = tl(st, "V1", [128, T // 128, 257], BF16)
                    o1 = tl(st, "o1", [128, 256])
                    ob = tl(st, "ob", [128, 256], BF16)
                    rr = tl(st, "rr", [128, 4])
                    jk = tl(st, "jk", [128, 256])
                    otb = tl(st, "otb", [128, 2, 128], BF16)
                    tps = f.ptile(st, "tps", [128, 2, 128], BF16)
                    for i, m in enumerate((h, h + 6)):
                        f.dma(SP, KT[i][:], KdT[m], reads=[dram["KdT"]], writes=[KT[i]], sb=KT[i])
                        f.dma(SP, QT[i][:], QdT[m], reads=[dram["QdT"]], writes=[QT[i]], sb=QT[i])
                    f.dma(SP, V1[:], Vd1[h].rearrange("(b p) c -> p b c", p=128), reads=[dram["Vd1"]], writes=[V1], sb=V1)
                    state = {}

                    def epi(j, u, ops):
                        state[u] = ops
                        if u == 0:
                            return
                        O1, O2 = state[0], state[1]
                        f.op(V, lambda: nc.vector.reciprocal(out=rr[:, 0:1], in_=O1[:, 256:257]), reads=[O1], writes=[rr])
                        f.op(V, lambda: nc.vector.reciprocal(out=rr[:, 1:2], in_=O2[:, 256:257]), reads=[O2], writes=[rr])
                        f.op(V, lambda: nc.vector.tensor_tensor(out=rr[:, 1:2], in0=rr[:, 1:2], in1=nlam[:], op=ALU.mult), reads=[rr, nlam], writes=[rr])
                        f.op(V, lambda: nc.vector.tensor_scalar(out=o1[:], in0=O1[:, 0:256], scalar1=rr[:, 0:1], scalar2=None, op0=ALU.mult),
                             reads=[O1, rr], writes=[o1])
                        f.op(V, lambda: nc.vector.scalar_tensor_tensor(out=o1[:], in0=O2[:, 0:256], scalar=rr[:, 1:2], in1=o1[:],
                                                                        op0=ALU.mult, op1=ALU.add), reads=[O2, rr, o1], writes=[o1])
                        f.op(A, lambda: nc.scalar.activation(out=jk[:], in_=o1[:], func=AF.Square, accum_out=rr[:, 2:3]), reads=[o1], writes=[jk, rr])
                        f.op(A, lambda: nc.scalar.activation(out=rr[:, 2:3], in_=rr[:, 2:3], func=AF.Sqrt, scale=1.0 / 256, bias=epsb[:, 0:1]),
                             reads=[rr, epsb], writes=[rr])
                        f.op(V, lambda: nc.vector.reciprocal(out=rr[:, 3:4], in_=rr[:, 2:3]), reads=[rr], writes=[rr])
                        f.op(V, lambda: nc.vector.scalar_tensor_tensor(out=ob[:], in0=o1[:], scalar=rr[:, 3:4], in1=sublng[:],
                                                                        op0=ALU.mult, op1=ALU.mult), reads=[o1, rr, sublng], writes=[ob])
                        for c2 in range(2):
                            f.op(P, lambda: nc.tensor.transpose(out=tps[:, c2, :], in_=ob[:, c2 * 128:(c2 + 1) * 128], identity=ident[:]),
                                 reads=[ob, ident], writes=[tps], acc=True)
                        f.op(V, lambda: nc.vector.tensor_copy(out=otb[:], in_=tps[:]), reads=[tps], writes=[otb])
                        f.dma(A, OT_d[2 * h:2 * h + 2, :, j * 128:(j + 1) * 128].rearrange("c p t -> p c t"), otb[:],
                              reads=[otb], writes=[dram["OT_d"]], sb=otb, merge=True)

                    def em_fn(u, j, kb):
                        i = kb - NCg * j
                        if i < -1:
                            return None
                        m = h + 6 * u
                        return EM[:, m, (i + 1) * 128:(i + 2) * 128]

                    attention(st, [[(QT[0], 128)], [(QT[1], 128)]], [[(KT[0], 128)], [(KT[1], 128)]], V1, 256,
                              lambda j: NCg * j + NCg, 128 ** -0.5, lambda u: rb[:, 31 * 12 + h + 6 * u:31 * 12 + h + 6 * u + 1],
                              em_fn, 2, epi)
                    f.barrier()

        if "diff" not in skip:
            run_diff()
        if stop_after <= 4:
            return finish(nc, f, dram)

        TWO_PI = 2.0 * math.pi
        CW1 = 6.28125
        CW2 = TWO_PI - CW1

        def trig(st_t, pos_ap, t0, cosT, sinT):
            pi_, pf, ang, kk, ki, mm = st_t
            f.dma(SP, pi_[:], pos_ap[0:1, t0:t0 + 512].partition_broadcast(64), writes=[pi_], sb=pi_)
            f.op(V, lambda: nc.vector.tensor_copy(out=pf[:], in_=pi_[:]), reads=[pi_], writes=[pf])
            for which, outT in ((0, sinT), (1, cosT)):
                f.op(V, lambda: nc.vector.tensor_scalar(out=ang[:], in0=pf[:], scalar1=invf[:, 0:1], scalar2=(0.5 * math.pi if which else 0.0),
                                                          op0=ALU.mult, op1=ALU.add), reads=[pf, invf], writes=[ang])
                f.op(V, lambda: nc.vector.tensor_scalar(out=kk[:], in0=ang[:], scalar1=1.0 / TWO_PI, scalar2=None, op0=ALU.mult),
                     reads=[ang], writes=[kk])
                f.op(V, lambda: nc.vector.tensor_copy(out=ki[:], in_=kk[:]), reads=[kk], writes=[ki])
                f.op(V, lambda: nc.vector.tensor_copy(out=kk[:], in_=ki[:]), reads=[ki], writes=[kk])
                f.op(V, lambda: nc.vector.scalar_tensor_tensor(out=ang[:], in0=kk[:], scalar=-CW1, in1=ang[:], op0=ALU.mult, op1=ALU.add),
                     reads=[kk, ang], writes=[ang])
                f.op(V, lambda: nc.vector.scalar_tensor_tensor(out=ang[:], in0=kk[:], scalar=-CW2, in1=ang[:], op0=ALU.mult, op1=ALU.add),
                     reads=[kk, ang], writes=[ang])
                f.op(V, lambda: nc.vector.tensor_single_scalar(out=mm[:], in_=ang[:], scalar=math.pi, op=ALU.is_gt), reads=[ang], writes=[mm])
                f.op(V, lambda: nc.vector.scalar_tensor_tensor(out=ang[:], in0=mm[:], scalar=-TWO_PI, in1=ang[:], op0=ALU.mult, op1=ALU.add),
                     reads=[mm, ang], writes=[ang])
                f.op(V, lambda: nc.vector.tensor_single_scalar(out=mm[:], in_=ang[:], scalar=-math.pi, op=ALU.is_lt), reads=[ang], writes=[mm])
                f.op(V, lambda: nc.vector.scalar_tensor_tensor(out=ang[:], in0=mm[:], scalar=TWO_PI, in1=ang[:], op0=ALU.mult, op1=ALU.add),
                     reads=[mm, ang], writes=[ang])
                f.op(V, lambda: nc.vector.tensor_scalar(out=ang[:], in0=ang[:], scalar1=3.14159, scalar2=-3.14159, op0=ALU.min, op1=ALU.max),
                     reads=[ang], writes=[ang])
                f.op(A, lambda: nc.scalar.activation(out=outT[:], in_=ang[:], func=AF.Sin), reads=[ang], writes=[outT])

        def trig_tiles(st):
            return (tl(st, "pi_", [64, 512], I32), tl(st, "pf", [64, 512]), tl(st, "ang", [64, 512]), tl(st, "kk", [64, 512]),
                    tl(st, "ki", [64, 512], I32), tl(st, "mm", [64, 512]))

        def rotary(xf, xb, psR, cosT, sinT, t1, outb):
            f.op(A, lambda: nc.scalar.copy(out=xb[:], in_=xf[0:64, :]), reads=[xf], writes=[xb])
            f.op(P, lambda: nc.tensor.matmul(psR[0:64, :], lhsT=rotT[:], rhs=xb[:], start=True, stop=True), reads=[rotT, xb], writes=[psR], acc=True)
            f.op(V, lambda: nc.vector.tensor_tensor(out=t1[:], in0=xf[0:64, :], in1=cosT[:], op=ALU.mult), reads=[xf, cosT], writes=[t1])
            f.op(V, lambda: nc.vector.tensor_tensor(out=xf[0:64, :], in0=psR[0:64, :], in1=sinT[:], op=ALU.mult), reads=[psR, sinT], writes=[xf])
            f.op(V, lambda: nc.vector.tensor_tensor(out=outb[0:64, :], in0=t1[:], in1=xf[0:64, :], op=ALU.add), reads=[t1, xf], writes=[outb])

        def lat_norm(pb, pss, pans, ht, nch, gain_t, raw, sq, rstd, outb):
            for cch in range(nch):
                ps = pb[cch % 4]
                mm_fm(ps, 128, pans[cch // 4], (cch % 4) * 128, ht, KC)
                f.op(A, lambda: nc.scalar.activation(out=sq[:], in_=ps[:], func=AF.Square), reads=[ps], writes=[sq])
                f.op(P, lambda: nc.tensor.matmul(pss[:, :], lhsT=ones[:, :], rhs=sq[:], start=(cch == 0), stop=(cch == nch - 1)),
                     reads=[ones, sq], writes=[pss], acc=True)
                f.op(V, lambda: nc.vector.tensor_scalar(out=raw[:, cch, :], in0=ps[:], scalar1=gain_t[:, cch:cch + 1], scalar2=None, op0=ALU.mult),
                     reads=[ps, gain_t], writes=[raw], merge=True)
            rstd_from(pss, rstd, 1.0 / (nch * 128))
            f.op(V, lambda: nc.vector.tensor_tensor(out=outb[:, 0:nch, :], in0=raw[:, 0:nch, :],
                                                      in1=rstd[:].unsqueeze(1).to_broadcast([128, nch, 512]), op=ALU.mult),
                 reads=[raw, rstd], writes=[outb])

        cqnT_d = dscr("cqnT_d", [12, 128, TL], BF16)
        dram["cqnT_d"] = TT(None, "cqnT_d")
        dram_of[id(cqnT_d)] = dram["cqnT_d"]
        with ExitStack() as st:
            pans = [tl(st, f"pan{i}", [128, KC, 512], BF16) for i in range(3)]
            ht = tl(st, "ht", [128, KC, 512], BF16)
            raw = tl(st, "raw", [128, 12, 512])
            cqns = [tl(st, f"cqn{i}", [128, 12, 512], BF16) for i in range(1)] * 2
            sq = tl(st, "sq", [128, 512], BF16)
            rstd = tl(st, "rstd", [128, 512])
            pb = [f.ptile(st, f"pb{i}", [128, 512], F32) for i in range(6)]
            for i in range(3):
                load_panel(pans[i], w_in, 0, KC, C_CQ + i * 512, 512)
            if stop_after <= 4.25:
                f.barrier()
                return finish(nc, f, dram)
            for tt in range(NTT_OWN):
                cqn = cqns[tt % 2]
                load_hT(ht, hT_ownD, tt * 512)
                if stop_after <= 4.27:
                    f.barrier()
                    return finish(nc, f, dram)
                lat_norm(pb, pb[5], pans, ht, 12, cqg, raw, sq, rstd, cqn)
                if stop_after <= 4.29:
                    f.barrier()
                    return finish(nc, f, dram)
                f.dma(A, cqnT_d[:, :, tt * 512:(tt + 1) * 512].rearrange("c p t -> p c t"), cqn[:],
                      reads=[cqn], writes=[dram["cqnT_d"]], sb=cqn, merge=True)
            f.barrier()
        if stop_after <= 4.3:
            return finish(nc, f, dram)
        with ExitStack() as st:
            wq = tl(st, "wq", [128, 12, 2304], BF16)
            cqns = [tl(st, f"cqn{i}", [128, 12, 512], BF16) for i in range(2)]
            sq = tl(st, "sq", [128, 512], BF16)
            rstd2 = tl(st, "rstd2", [128, 512])
            cosT, sinT = tl(st, "cosT", [64, 512]), tl(st, "sinT", [64, 512])
            tt_ = trig_tiles(st)
            oAs = [tl(st, f"oA{i}", [128, 512], BF16) for i in range(2)]
            oBf = tl(st, "oBf", [64, 512])
            xb = tl(st, "xb", [64, 512], BF16)
            t1 = tl(st, "t1", [64, 512])
            oBs = [tl(st, f"oB{i}", [64, 512], BF16) for i in range(2)]
            pb = [f.ptile(st, f"pb{i}", [128, 512], F32) for i in range(8)]
            load_panel(wq, w_uq, 0, 12, 0, 2304)
            for tt in range(NTT_OWN):
                cqn = cqns[tt % 2]
                load_hT(cqn, cqnT_d, tt * 512, nk=12)
                trig(tt_, pos_own, tt * 512, cosT, sinT)
                for h in range(12):
                    psA, psB = pb[h % 2], pb[2 + h % 2]
                    oA, oB = oAs[h % 2], oBs[h % 2]
                    mm_fm(psA, 128, wq, h * 192, cqn, 12)
                    mm_fm(psB, 64, wq, h * 192 + 128, cqn, 12)
                    fm_rms((sq, pb[5], rstd2), [(psA, 128), (psB, 64)], 192, [mqgA[:, 0:1], mqgB[:, 0:1]], [(oA, 128), (oBf, 64)])
                    rotary(oBf, xb, pb[6], cosT, sinT, t1, oB)
                    f.dma(A, QmT[h, 0:128, tt * 512:(tt + 1) * 512], oA[:], reads=[oA], writes=[dram["QmT"]], sb=oA, merge=True)
                    f.dma(A, QmT[h, 128:192, tt * 512:(tt + 1) * 512], oB[:], reads=[oB], writes=[dram["QmT"]], sb=oB, merge=True)
            f.barrier()

        if stop_after <= 4.6:
            return finish(nc, f, dram)
        with ExitStack() as st:
            pan = tl(st, "pan", [128, KC, 576], BF16)
            hts = [tl(st, f"ht{i}", [128, KC, 512], BF16) for i in range(2)]
            wkv = tl(st, "wkv", [128, 4, 3072], BF16)
            raw = tl(st, "raw", [128, 4, 512])
            ckn = tl(st, "ckn", [128, 4, 512], BF16)
            sq = tl(st, "sq", [128, 512], BF16)
            sqk = tl(st, "sqk", [64, 512], BF16)
            rstd = tl(st, "rstd", [128, 512])
            rstd2 = tl(st, "rstd2", [128, 512])
            cosT, sinT = tl(st, "cosT", [64, 512]), tl(st, "sinT", [64, 512])
            tt_ = trig_tiles(st)
            krf = tl(st, "krf", [64, 512])
            krot = tl(st, "krot", [64, 512])
            krb = tl(st, "krb", [64, 512], BF16)
            xb = tl(st, "xb", [64, 512], BF16)
            t1 = tl(st, "t1", [64, 512])
            oAs = [tl(st, f"oA{i}", [128, 512], BF16) for i in range(2)]
            oBs = [tl(st, f"oB{i}", [64, 512], BF16) for i in range(2)]
            v1s = [tl(st, f"v1{i}", [128, 4, 129], BF16) for i in range(2)]
            pb = [f.ptile(st, f"pb{i}", [128, 512], F32) for i in range(8)]
            for v1 in v1s:
                f.op(V, lambda: nc.vector.memset(v1[:], 1.0), writes=[v1])
            load_panel(pan, w_in, 0, KC, C_CKV, 576)
            load_panel(wkv, w_ukv, 0, 4, 0, 3072)
            wkv_v = wkv[:].rearrange("p k (h c) -> p k h c", c=256)
            vi = 0
            for tt in range(NTT_ALL):
                ht = hts[tt % 2]
                load_hT(ht, hT_all, tt * 512)
                trig(tt_, pos_full, tt * 512, cosT, sinT)
                lat_norm(pb, pb[5], [pan], ht, 4, ckvg, raw, sq, rstd, ckn)
                mm_fm(pb[4], 64, pan, 512, ht, KC)
                f.op(A, lambda: nc.scalar.activation(out=sqk[:], in_=pb[4][0:64, :], func=AF.Square), reads=[pb[4]], writes=[sqk])
                f.op(V, lambda: nc.vector.tensor_scalar(out=krf[:], in0=pb[4][0:64, :], scalar1=mkgB[:, 0:1], scalar2=None, op0=ALU.mult),
                     reads=[pb[4], mkgB], writes=[krf])
                rotary(krf, xb, pb[6], cosT, sinT, t1, krb)
                f.op(V, lambda: nc.vector.tensor_copy(out=krot[:], in_=krb[:]), reads=[krb], writes=[krot])
                for h in range(12):
                    psN = pb[h % 2]
                    oA, oB = oAs[h % 2], oBs[h % 2]
                    mm_fm(psN, 128, wkv, h * 256, ckn, 4)
                    f.op(A, lambda: nc.scalar.activation(out=sq[:], in_=psN[:], func=AF.Square), reads=[psN], writes=[sq])
                    f.op(P, lambda: nc.tensor.matmul(pb[5][:, :], lhsT=ones[:, :], rhs=sq[:], start=True, stop=False),
                         reads=[ones, sq], writes=[pb[5]], acc=True)
                    f.op(P, lambda: nc.tensor.matmul(pb[5][:, :], lhsT=ones[0:64, :], rhs=sqk[:], start=False, stop=True),
                         reads=[ones, sqk], writes=[pb[5]], acc=True)
                    rstd_from(pb[5], rstd2, 1.0 / 192)
                    f.op(V, lambda: nc.vector.scalar_tensor_tensor(out=oA[:], in0=psN[:], scalar=mkgA[:, 0:1], in1=rstd2[:], op0=ALU.mult, op1=ALU.mult),
                         reads=[psN, mkgA, rstd2], writes=[oA])
                    f.op(V, lambda: nc.vector.tensor_tensor(out=oB[:], in0=krot[:], in1=rstd2[0:64, :], op=ALU.mult), reads=[krot, rstd2], writes=[oB])
                    f.dma(A, KmT[h, 0:128, tt * 512:(tt + 1) * 512], oA[:], reads=[oA], writes=[dram["KmT"]], sb=oA, merge=True)
                    f.dma(A, KmT[h, 128:192, tt * 512:(tt + 1) * 512], oB[:], reads=[oB], writes=[dram["KmT"]], sb=oB, merge=True)
                for tb in range(4):
                    for hg in range(3):
                        psV, v1 = pb[2 + (vi % 2)], v1s[vi % 2]
                        vi += 1
                        for k in range(4):
                            f.op(P, lambda: nc.tensor.matmul(psV[:, :].rearrange("p (h c) -> p h c", c=128), lhsT=ckn[:, k, tb * 128:(tb + 1) * 128],
                                                              rhs=wkv_v[:, k, hg * 4:(hg + 1) * 4, 128:256], start=(k == 0), stop=(k == 3)),
                                 reads=[ckn, wkv], writes=[psV], acc=True)
                        f.op(V, lambda: nc.vector.tensor_copy(out=v1[:, :, 0:128], in_=psV[:, :].rearrange("p (h c) -> p h c", c=128)),
                             reads=[psV], writes=[v1])
                        r0 = tt * 512 + tb * 128
                        f.dma(A, Vm1[hg * 4:(hg + 1) * 4, r0:r0 + 128, :].rearrange("h t c -> t h c"), v1[:],
                              reads=[v1], writes=[dram["Vm1"]], sb=v1, merge=True)
            f.barrier()
        if stop_after <= 5:
            return finish(nc, f, dram)

        def run_mla():
            for h in range(12):
                with ExitStack() as st:
                    KnT = tl(st, "KnT", [128, T], BF16)
                    KrT = tl(st, "KrT", [64, T], BF16)
                    QnT = tl(st, "QnT", [128, TL], BF16)
                    QrT = tl(st, "QrT", [64, TL], BF16)
                    V1 = tl(st, "V1", [128, T // 128, 129], BF16)
                    rr = tl(st, "rr", [128, 1])
                    ob = tl(st, "ob", [128, 128], BF16)
                    otb = tl(st, "otb", [128, 128], BF16)
                    tps = f.ptile(st, "tps", [128, 128], BF16)
                    f.dma(SP, KnT[:], KmT[h, 0:128, :], reads=[dram["KmT"]], writes=[KnT], sb=KnT)
                    f.dma(SP, KrT[:], KmT[h, 128:192, :], reads=[dram["KmT"]], writes=[KrT], sb=KrT)
                    f.dma(SP, QnT[:], QmT[h, 0:128, :], reads=[dram["QmT"]], writes=[QnT], sb=QnT)
                    f.dma(SP, QrT[:], QmT[h, 128:192, :], reads=[dram["QmT"]], writes=[QrT], sb=QrT)
                    f.dma(SP, V1[:], Vm1[h].rearrange("(b p) c -> p b c", p=128), reads=[dram["Vm1"]], writes=[V1], sb=V1)

                    def epi(j, u, O):
                        f.op(V, lambda: nc.vector.reciprocal(out=rr[:], in_=O[:, 128:129]), reads=[O], writes=[rr])
                        f.op(V, lambda: nc.vector.tensor_scalar(out=ob[:], in0=O[:, 0:128], scalar1=rr[:, 0:1], scalar2=None, op0=ALU.mult),
                             reads=[O, rr], writes=[ob])
                        f.op(P, lambda: nc.tensor.transpose(out=tps[:], in_=ob[:], identity=ident[:]), reads=[ob, ident], writes=[tps], acc=True)
                        f.op(V, lambda: nc.vector.tensor_copy(out=otb[:], in_=tps[:]), reads=[tps], writes=[otb])
                        f.dma(A, OT_d[12 + h, :, j * 128:(j + 1) * 128], otb[:], reads=[otb], writes=[dram["OT_d"]], sb=otb, merge=True)

                    def em_fn(u, j, kb):
                        i = kb - NCg * j
                        if i < 0:
                            return None
                        return MK[:, (i + 1) * 128:(i + 2) * 128]

                    attention(st, [[(QnT, 128), (QrT, 64)]], [[(KnT, 128), (KrT, 64)]], V1, 128,
                              lambda j: NCg * j + NCg, 192 ** -0.5, lambda u: None, em_fn, 1, epi)
                    f.barrier()

        if "mla" not in skip:
            run_mla()

        hT_memD = dscr("hT_memD", [KC, 128, NMEM], BF16)
        KeT = dscr("KeT", [4, 256, NMEM], BF16)
        Ve1 = dscr("Ve1", [4, NMEM, 257], BF16)
        for nm, ap in (("hT_memD", hT_memD), ("KeT", KeT), ("Ve1", Ve1)):
            dram[nm] = TT(None, nm)
            dram_of[id(ap)] = dram[nm]
        phase_norm(mem, NMEM // 128, memg, hT_memD, "hT_memD")
        pass_fm_rms(hT_memD, 1, 0, 8, 2, 256, lambda s_: memkg[:, s_:s_ + 1], KeT, "KeT",
                    lambda m: KeT[m // 2, (m % 2) * 128:(m % 2) * 128 + 128, :], W=mem_wkv, n=NMEM)
        with ExitStack() as st:
            pans = [tl(st, f"pan{i}", [128, KC, 512], BF16) for i in range(2)]
            ht = tl(st, "ht", [128, KC, NMEM], BF16)
            v1s = [tl(st, f"v1{i}", [128, 2, 257], BF16) for i in range(2)]
            psc = [f.ptile(st, f"psc{i}", [128, 512], F32) for i in range(2)]
            for v1 in v1s:
                f.op(V, lambda: nc.vector.memset(v1[:], 1.0), writes=[v1])
            load_hT(ht, hT_memD, 0, n=NMEM)
            vi = 0
            for pi in range(2):
                load_panel(pans[pi], mem_wkv, 0, KC, 1024 + pi * 512, 512)
                for tb in range(NMEM // 128):
                    ps, v1 = psc[vi % 2], v1s[vi % 2]
                    vi += 1
                    mm_tm(ps, ht, tb * 128, pans[pi], 0, 512, KC)
                    f.op(V, lambda: nc.vector.tensor_copy(out=v1[:, :, 0:256], in_=ps[:].rearrange("p (h c) -> p h c", h=2)),
                         reads=[ps], writes=[v1])
                    f.dma(A, Ve1[pi * 2:pi * 2 + 2, tb * 128:(tb + 1) * 128, :].rearrange("h t c -> t h c"), v1[:],
                          reads=[v1], writes=[dram["Ve1"]], sb=v1, merge=True)
            f.barrier()
        for e in range(4):
            with ExitStack() as st:
                KT = [tl(st, f"KT{i}", [128, NMEM], BF16) for i in range(2)]
                QT = [tl(st, f"QT{i}", [128, TL], BF16) for i in range(2)]
                V1 = tl(st, "V1", [128, NMEM // 128, 257], BF16)
                rr = tl(st, "rr", [128, 1])
                ob = tl(st, "ob", [128, 256], BF16)
                otb = tl(st, "otb", [128, 2, 128], BF16)
                tps = f.ptile(st, "tps", [128, 2, 128], BF16)
                for i in range(2):
                    f.dma(SP, KT[i][:], KeT[e, i * 128:(i + 1) * 128, :], reads=[dram["KeT"]], writes=[KT[i]], sb=KT[i])
                    f.dma(SP, QT[i][:], QeT[e, i * 128:(i + 1) * 128, :], reads=[dram["QeT"]], writes=[QT[i]], sb=QT[i])
                f.dma(SP, V1[:], Ve1[e].rearrange("(b p) c -> p b c", p=128), reads=[dram["Ve1"]], writes=[V1], sb=V1)

                def epi(j, u, O):
                    f.op(V, lambda: nc.vector.reciprocal(out=rr[:], in_=O[:, 256:257]), reads=[O], writes=[rr])
                    f.op(V, lambda: nc.vector.tensor_scalar(out=ob[:], in0=O[:, 0:256], scalar1=rr[:, 0:1], scalar2=None, op0=ALU.mult),
                         reads=[O, rr], writes=[ob])
                    for c2 in range(2):
                        f.op(P, lambda: nc.tensor.transpose(out=tps[:, c2, :], in_=ob[:, c2 * 128:(c2 + 1) * 128], identity=ident[:]),
                             reads=[ob, ident], writes=[tps], acc=True)
                    f.op(V, lambda: nc.vector.tensor_copy(out=otb[:], in_=tps[:]), reads=[tps], writes=[otb])
                    f.dma(A, OT_d[24 + 2 * e:26 + 2 * e, :, j * 128:(j + 1) * 128].rearrange("c p t -> p c t"), otb[:],
                          reads=[otb], writes=[dram["OT_d"]], sb=otb, merge=True)

                attention(st, [[(QT[0], 128), (QT[1], 128)]], [[(KT[0], 128), (KT[1], 128)]], V1, 256,
                          lambda j: NMEM // 128, 256 ** -0.5, lambda u: None, lambda u, j, kb: None, 1, epi)
                f.barrier()
        f.barrier()
        att_st.close()
        if stop_after <= 6:
            return finish(nc, f, dram)

        mixT_d = dscr("mixT_d", [KC, 128, TL], BF16)
        dram["mixT_d"] = TT(None, "mixT_d")
        dram_of[id(mixT_d)] = dram["mixT_d"]
        dram_of[id(OT_d)] = dram["OT_d"]
        with ExitStack() as st:
            pw = [[tl(st, f"pw{i}{b}", [128, nk_, 512], BF16) for b, nk_ in enumerate((12, 12, 8))] for i in range(2)]
            ots = [tl(st, f"ot{i}", [128, KC, 512], BF16) for i in range(2)]
            gts = [[tl(st, f"gt{i}{b}", [128, 512]) for b in range(3)] for i in range(2)]
            mx = tl(st, "mx", [128, 512])
            mbs = [tl(st, f"mb{i}", [128, 512], BF16) for i in range(2)]
            psc = [f.ptile(st, f"psc{i}", [128, 512], F32) for i in range(6)]
            it = 0
            gi = 0
            for dp in range(8):
                pws = pw[dp % 2]
                for b, (Wb, nk_) in enumerate(((w_o_diff, 12), (w_o_mla, 12), (w_o_mem, 8))):
                    load_panel(pws[b], Wb, 0, nk_, dp * 512, 512)
                for tt in range(NTT_OWN):
                    ot = ots[it % 2]
                    it += 1
                    load_hT(ot, OT_d, tt * 512)
                    for sc in range(4):
                        dc = dp * 4 + sc
                        gt = gts[gi % 2]
                        mb = mbs[gi % 2]
                        pss_ = psc[(gi % 2) * 3:(gi % 2) * 3 + 3]
                        gi += 1
                        for b, (k0, nk_) in enumerate(((0, 12), (12, 12), (24, 8))):
                            f.dma(SP, gt[b][:], gT[b * 32 + dc, :, tt * 512:(tt + 1) * 512], reads=[dram["gT"]], writes=[gt[b]], sb=gt[b])
                            for k in range(nk_):
                                f.op(P, lambda: nc.tensor.matmul(pss_[b][:, :], lhsT=pws[b][:, k, sc * 128:(sc + 1) * 128], rhs=ot[:, k0 + k, :],
                                                                  start=(k == 0), stop=(k == nk_ - 1)),
                                     reads=[pws[b], ot], writes=[pss_[b]], acc=True)
                        f.op(V, lambda: nc.vector.tensor_tensor(out=mx[:], in0=pss_[0][:], in1=gt[0][:], op=ALU.mult), reads=[pss_[0], gt[0]], writes=[mx])
                        f.op(V, lambda: nc.vector.tensor_tensor(out=gt[1][:], in0=pss_[1][:], in1=gt[1][:], op=ALU.mult), reads=[pss_[1], gt[1]], writes=[gt[1]])
                        f.op(V, lambda: nc.vector.tensor_tensor(out=gt[2][:], in0=pss_[2][:], in1=gt[2][:], op=ALU.mult), reads=[pss_[2], gt[2]], writes=[gt[2]])
                        f.op(G, lambda: nc.gpsimd.tensor_tensor(out=mx[:], in0=mx[:], in1=gt[1][:], op=ALU.add), reads=[mx, gt[1]], writes=[mx])
                        f.op(G, lambda: nc.gpsimd.tensor_tensor(out=mb[:], in0=mx[:], in1=gt[2][:], op=ALU.add), reads=[mx, gt[2]], writes=[mb])
                        f.dma(A, mixT_d[dc, :, tt * 512:(tt + 1) * 512], mb[:], reads=[mb], writes=[dram["mixT_d"]], sb=mb, merge=True)
            f.barrier()
        with ExitStack() as st:
            pans = [tl(st, f"pan{i}", [128, KC, 512], BF16) for i in range(2)]
            mts = [tl(st, f"mt{i}", [128, KC, 512], BF16) for i in range(2)]
            xts = [tl(st, f"xt{i}", [128, 512]) for i in range(2)]
            psc = [f.ptile(st, f"psc{i}", [128, 512], F32) for i in range(2)]
            it = 0
            xi = 0
            for dp in range(8):
                pan = pans[dp % 2]
                load_panel(pan, w_out, 0, KC, dp * 512, 512)
                for tt in range(NTT_OWN):
                    mt = mts[it % 2]
                    it += 1
                    load_hT(mt, mixT_d, tt * 512)
                    for tb in range(4):
                        ps, xt = psc[xi % 2], xts[xi % 2]
                        xi += 1
                        r0 = tt * 512 + tb * 128
                        f.dma(SP, xt[:], x_own[r0:r0 + 128, dp * 512:(dp + 1) * 512], writes=[xt], sb=xt)
                        mm_tm(ps, mt, tb * 128, pan, 0, 512, KC)
                        f.op(V, lambda: nc.vector.tensor_tensor(out=xt[:], in0=ps[:], in1=xt[:], op=ALU.add), reads=[ps, xt], writes=[xt])
                        f.dma(A, x1s[r0:r0 + 128, dp * 512:(dp + 1) * 512], xt[:], reads=[xt], writes=[dram["x1s"]], sb=xt, merge=True)
            f.barrier()
        if stop_after <= 7:
            return finish(nc, f, dram)

        RS = R_SLOT
        NSB = NSLOT // 128
        SBG = RS // 128
        h2T_d = dscr("h2T_d", [KC, 128, TL], BF16)
        h2gT_d = dscr("h2gT_d", [8, KC, 128, RS], BF16)
        actT_d = dscr("actT_d", [64, 4, 128, RS], BF16)
        for nm, ap in (("h2T_d", h2T_d), ("actT_d", actT_d), ("h2gT_d", h2gT_d)):
            dram[nm] = TT(None, nm)
            dram_of[id(ap)] = dram[nm]
        with ExitStack() as s0:
            Perm = tl(s0, "Perm", [128, NQB, NSLOT], BF16)
            gws = tl(s0, "gws", [128, NSB, 64])
            with ExitStack() as s1:
                xb_all = tl(s1, "xb_all", [128, NQB, D], BF16)
                with ExitStack() as st:
                    xts = [tl(st, f"xt{i}", [128, D]) for i in range(2)]
                    sss = [tl(st, f"ss{i}", [128, 1]) for i in range(2)]
                    junk = tl(st, "junk", [128, D], BF16)
                    hts = [tl(st, f"ht{i}", [128, KC, 128], BF16) for i in range(2)]
                    pts = [f.ptile(st, f"pt{i}", [128, 8, 128], BF16) for i in range(4)]
                    for i in range(NQB):
                        xt, ss, ht = xts[i % 2], sss[i % 2], hts[i % 2]
                        f.dma(SP, xt[:], x1s[i * 128:(i + 1) * 128, :], writes=[xt], sb=xt)
                        f.op(A, lambda: nc.scalar.activation(out=junk[:], in_=xt[:], func=AF.Square, accum_out=ss[:]), reads=[xt], writes=[junk, ss])
                        f.op(A, lambda: nc.scalar.activation(out=ss[:], in_=ss[:], func=AF.Sqrt, scale=1.0 / D, bias=epsb[:, 0:1]), reads=[ss, epsb], writes=[ss])
                        f.op(V, lambda: nc.vector.reciprocal(out=ss[:], in_=ss[:]), reads=[ss], writes=[ss])
                        f.op(A, lambda: nc.scalar.activation(out=xb_all[:, i, :], in_=xt[:], func=AF.Copy, scale=ss[:, 0:1]), reads=[xt, ss], writes=[xb_all], merge=True)
                        for q in range(4):
                            pt = pts[q]
                            for k in range(8):
                                c = q * 8 + k
                                f.op(P, lambda: nc.tensor.transpose(out=pt[:, k, :], in_=xb_all[:, i, c * 128:(c + 1) * 128], identity=ident[:]),
                                     reads=[xb_all, ident], writes=[pt], acc=True)
                            gb = ffng[:, q * 8:(q + 1) * 8].unsqueeze(2).to_broadcast([128, 8, 128])
                            f.op(V, lambda: nc.vector.tensor_tensor(out=ht[:, q * 8:(q + 1) * 8, :], in0=pt[:], in1=gb, op=ALU.mult),
                                 reads=[pt, ffng], writes=[ht], merge=True)
                        f.dma(A, h2T_d[:, :, i * 128:(i + 1) * 128].rearrange("c p t -> p c t"), ht[:], reads=[ht], writes=[dram["h2T_d"]], sb=ht, merge=True)
                    f.barrier()
                with ExitStack() as st:
                    wrt = tl(st, "wrt", [128, KC, 72], BF16)
                    brt = tl(st, "brt", [128, 72])
                    hbs = [tl(st, f"hb{i}", [128, KC, 128], BF16) for i in range(2)]
                    gw = tl(st, "gw", [128, NQB, 64])
                    gwh = tl(st, "gwh", [128, NQB, 64], BF16)
                    gwr = tl(st, "gwr", [128, NQB, 64])
                    gwl = tl(st, "gwl", [128, NQB, 64], BF16)
                    GmA = tl(st, "GmA", [128, NQB, 8])
                    Gmb = tl(st, "Gmb", [128, NQB, 8], BF16)
                    tri_f = tl(st, "tri_f", [128, 128])
                    tri_b = tl(st, "tri_b", [128, 128], BF16)
                    goff = tl(st, "goff", [128, 8])
                    iota = tl(st, "iota", [128, NSLOT])
                    slot = tl(st, "slot", [128, NQB])
                    tmp8 = tl(st, "tmp8", [128, 8])
                    lg = tl(st, "lg", [128, 72])
                    sm = tl(st, "sm", [128, 16])
                    t8 = tl(st, "t8", [128, 8])
                    ex8 = tl(st, "ex8", [128, 8])
                    lem = tl(st, "lem", [128, 64])
                    lem2 = tl(st, "lem2", [128, 64])
                    mk1 = tl(st, "mk1", [128, 64])
                    mk2 = tl(st, "mk2", [128, 64])
                    psr = f.ptile(st, "psr", [128, 512], F32)
                    psk = f.ptile(st, "psk", [128, 512], F32)
                    psg_ = [f.ptile(st, f"psgw{i}", [128, 512], F32) for i in range(2)]
                    load_panel(wrt, w_rt, 0, KC, 0, 72)
                    f.dma(SP, brt[:], b_rt[0:1, :].partition_broadcast(128), writes=[brt], sb=brt)
                    f.dma(SP, tri_f[:], c_tri, writes=[tri_f], sb=tri_f)
                    f.dma(SP, goff[:], c_goff, writes=[goff], sb=goff)
                    f.dma(SP, iota[:], c_iota, writes=[iota], sb=iota)
                    f.op(V, lambda: nc.vector.tensor_copy(out=tri_b[:], in_=tri_f[:]), reads=[tri_f], writes=[tri_b])
                    S = lambda i: sm[:, i:i + 1]
                    for tb in range(NQB):
                        hb = hbs[tb % 2]
                        load_hT(hb, h2T_d, tb * 128, n=128)
                        mm_tm(psr, hb, 0, wrt, 0, 72, KC)
                        f.op(V, lambda: nc.vector.tensor_tensor(out=lg[:], in0=psr[:, 0:72], in1=brt[:], op=ALU.add), reads=[psr, brt], writes=[lg])
                        f.op(V, lambda: nc.vector.reduce_max(out=S(0), in_=lg[:, 0:8], axis=AX.X), reads=[lg], writes=[sm])
                        f.op(V, lambda: nc.vector.tensor_scalar(out=GmA[:, tb, :], in0=lg[:, 0:8], scalar1=S(0), scalar2=None, op0=ALU.is_equal), reads=[lg, sm], writes=[GmA], merge=True)
                        f.op(V, lambda: nc.vector.tensor_scalar(out=S(1), in0=S(0), scalar1=-1.0, scalar2=None, op0=ALU.mult), reads=[sm], writes=[sm])
                        f.op(A, lambda: nc.scalar.activation(out=ex8[:], in_=lg[:, 0:8], func=AF.Exp, bias=S(1), accum_out=S(2)), reads=[lg, sm], writes=[ex8, sm])
                        f.op(V, lambda: nc.vector.reciprocal(out=S(3), in_=S(2)), reads=[sm], writes=[sm])
                        f.op(V, lambda: nc.vector.tensor_scalar(out=t8[:], in0=GmA[:, tb, :], scalar1=-1.0, scalar2=1e30, op0=ALU.add, op1=ALU.mult), reads=[GmA], writes=[t8])
                        f.op(V, lambda: nc.vector.tensor_tensor(out=lem[:].rearrange("p (g e) -> p g e", g=8), in0=lg[:, 8:72].rearrange("p (g e) -> p g e", g=8),
                                                                  in1=t8[:].unsqueeze(2).to_broadcast([128, 8, 8]), op=ALU.add), reads=[lg, t8], writes=[lem])
                        f.op(V, lambda: nc.vector.reduce_max(out=S(4), in_=lem[:], axis=AX.X), reads=[lem], writes=[sm])
                        f.op(V, lambda: nc.vector.tensor_scalar(out=mk1[:], in0=lem[:], scalar1=S(4), scalar2=None, op0=ALU.is_equal), reads=[lem, sm], writes=[mk1])
                        f.op(V, lambda: nc.vector.scalar_tensor_tensor(out=lem2[:], in0=mk1[:], scalar=-1e30, in1=lem[:], op0=ALU.mult, op1=ALU.add), reads=[mk1, lem], writes=[lem2])
                        f.op(V, lambda: nc.vector.reduce_max(out=S(5), in_=lem2[:], axis=AX.X), reads=[lem2], writes=[sm])
                        f.op(V, lambda: nc.vector.tensor_scalar(out=mk2[:], in0=lem2[:], scalar1=S(5), scalar2=None, op0=ALU.is_equal), reads=[lem2, sm], writes=[mk2])
                        f.op(V, lambda: nc.vector.tensor_scalar(out=S(6), in0=S(4), scalar1=-1.0, scalar2=None, op0=ALU.mult), reads=[sm], writes=[sm])
                        f.op(A, lambda: nc.scalar.activation(out=S(7), in_=S(5), func=AF.Exp, bias=S(6)), reads=[sm], writes=[sm])
                        f.op(V, lambda: nc.vector.tensor_scalar(out=S(8), in0=S(7), scalar1=1.0, scalar2=None, op0=ALU.add), reads=[sm], writes=[sm])
                        f.op(V, lambda: nc.vector.reciprocal(out=S(9), in_=S(8)), reads=[sm], writes=[sm])
                        f.op(V, lambda: nc.vector.tensor_tensor(out=S(10), in0=S(3), in1=S(9), op=ALU.mult), reads=[sm], writes=[sm])
                        f.op(V, lambda: nc.vector.tensor_tensor(out=S(11), in0=S(10), in1=S(7), op=ALU.mult), reads=[sm], writes=[sm])
                        f.op(V, lambda: nc.vector.tensor_scalar(out=gw[:, tb, :], in0=mk1[:], scalar1=S(10), scalar2=None, op0=ALU.mult), reads=[mk1, sm], writes=[gw], merge=True)
                        f.op(V, lambda: nc.vector.scalar_tensor_tensor(out=gw[:, tb, :], in0=mk2[:], scalar=S(11), in1=gw[:, tb, :], op0=ALU.mult, op1=ALU.add),
                             reads=[mk2, sm, gw], writes=[gw], merge=True)
                    f.op(V, lambda: nc.vector.tensor_copy(out=gwh[:], in_=gw[:]), reads=[gw], writes=[gwh])
                    f.op(V, lambda: nc.vector.tensor_tensor(out=gwr[:], in0=gw[:], in1=gwh[:], op=ALU.subtract), reads=[gw, gwh], writes=[gwr])
                    f.op(V, lambda: nc.vector.tensor_copy(out=gwl[:], in_=gwr[:]), reads=[gwr], writes=[gwl])
                    f.op(V, lambda: nc.vector.tensor_copy(out=Gmb[:], in_=GmA[:]), reads=[GmA], writes=[Gmb])
                    for tb in range(NQB):
                        f.op(P, lambda: nc.tensor.matmul(psk[:, 0:8], lhsT=tri_b[:], rhs=Gmb[:, tb, :], start=True, stop=(tb == 0)),
                             reads=[tri_b, Gmb], writes=[psk], acc=True)
                        for t2 in range(tb):
                            f.op(P, lambda: nc.tensor.matmul(psk[:, 0:8], lhsT=ones[:], rhs=Gmb[:, t2, :], start=False, stop=(t2 == tb - 1)),
                                 reads=[ones, Gmb], writes=[psk], acc=True)
                        f.op(V, lambda: nc.vector.tensor_tensor(out=tmp8[:], in0=psk[:, 0:8], in1=goff[:], op=ALU.add), reads=[psk, goff], writes=[tmp8])
                        f.op(V, lambda: nc.vector.tensor_tensor(out=tmp8[:], in0=tmp8[:], in1=GmA[:, tb, :], op=ALU.mult), reads=[tmp8, GmA], writes=[tmp8])
                        f.op(V, lambda: nc.vector.reduce_sum(out=slot[:, tb:tb + 1], in_=tmp8[:], axis=AX.X), reads=[tmp8], writes=[slot], merge=True)
                        f.op(V, lambda: nc.vector.tensor_scalar(out=Perm[:, tb, :], in0=iota[:], scalar1=slot[:, tb:tb + 1], scalar2=None, op0=ALU.is_equal),
                             reads=[iota, slot], writes=[Perm], merge=True)
                    for sb in range(NSB):
                        pg2 = psg_[sb % 2]
                        n_ = 0
                        for part in (gwh, gwl):
                            for tb in range(NQB):
                                f.op(P, lambda: nc.tensor.matmul(pg2[:, 0:64], lhsT=Perm[:, tb, sb * 128:(sb + 1) * 128], rhs=part[:, tb, :],
                                                                  start=(n_ == 0), stop=(n_ == 2 * NQB - 1)), reads=[Perm, part], writes=[pg2], acc=True)
                                n_ += 1
                        f.op(V, lambda: nc.vector.tensor_copy(out=gws[:, sb, :], in_=pg2[:, 0:64]), reads=[pg2], writes=[gws], merge=True)
                    f.barrier()
                with ExitStack() as st:
                    hgs = [tl(st, f"hg{i}", [128, 4, RS], BF16) for i in range(2)]
                    pps = [f.ptile(st, f"pp{i}", [128, 512], F32) for i in range(4)]
                    hi = 0
                    for g in range(8):
                        for c4 in range(0, KC, 4):
                            hg = hgs[hi % 2]
                            hi += 1
                            for cc in range(4):
                                c = c4 + cc
                                pp = pps[cc]
                                for tb in range(NQB):
                                    f.op(P, lambda: nc.tensor.matmul(pp[:, 0:RS], lhsT=xb_all[:, tb, c * 128:(c + 1) * 128], rhs=Perm[:, tb, g * RS:(g + 1) * RS],
                                                                      start=(tb == 0), stop=(tb == NQB - 1)), reads=[xb_all, Perm], writes=[pp], acc=True)
                                f.op(V, lambda: nc.vector.tensor_scalar(out=hg[:, cc, :], in0=pp[:, 0:RS], scalar1=ffng[:, c:c + 1], scalar2=None, op0=ALU.mult),
                                     reads=[pp, ffng], writes=[hg], merge=True)
                            f.dma(A, h2gT_d[g, c4:c4 + 4, :, :].rearrange("c p t -> p c t"), hg[:], reads=[hg], writes=[dram["h2gT_d"]], sb=hg, merge=True)
                    f.barrier()
            with ExitStack() as st:
                pans = [tl(st, f"pan{i}", [128, KC, 512], BF16) for i in range(3)]
                h2gs = [tl(st, f"h2g{i}", [128, KC, RS], BF16) for i in range(2)]
                sgs = [tl(st, f"sg{i}", [128, 512]) for i in range(2)]
                acts = [tl(st, f"act{i}", [128, 512], BF16) for i in range(2)]
                atbs = [tl(st, f"atb{i}", [128, 4, 128], BF16) for i in range(2)]
                psg = [f.ptile(st, f"psg{i}", [128, 512], F32) for i in range(2)]
                psu = [f.ptile(st, f"psu{i}", [128, 512], F32) for i in range(2)]
                tpss = [f.ptile(st, f"tpa{i}", [128, 4, 128], BF16) for i in range(2)]
                pi_ = 0
                ci = 0
                for g in range(8):
                    h2g = h2gs[g % 2]
                    f.dma(SP, h2g[:], h2gT_d[g].rearrange("c p t -> p c t"), writes=[h2g], sb=h2g)
                    for el in range(8):
                        e = g * 8 + el
                        pg_, pu_ = pans[pi_ % 3], pans[(pi_ + 1) % 3]
                        pi_ += 2
                        load_panel(pg_, w_eg[e], 0, KC, 0, 512)
                        load_panel(pu_, w_eu[e], 0, KC, 0, 512)
                        for sl in range(SBG):
                            sb = g * SBG + sl
                            pG, pU, sg, act, atb, tpa = psg[ci % 2], psu[ci % 2], sgs[ci % 2], acts[ci % 2], atbs[ci % 2], tpss[ci % 2]
                            ci += 1
                            mm_tm(pG, h2g, sl * 128, pg_, 0, 512, KC)
                            mm_tm(pU, h2g, sl * 128, pu_, 0, 512, KC)
                            f.op(A, lambda: nc.scalar.activation(out=sg[:], in_=pG[:], func=AF.Silu), reads=[pG], writes=[sg])
                            f.op(V, lambda: nc.vector.scalar_tensor_tensor(out=act[:], in0=pU[:], scalar=gws[:, sb, e:e + 1], in1=sg[:], op0=ALU.mult, op1=ALU.mult),
                                 reads=[pU, gws, sg], writes=[act])
                            for k in range(4):
                                f.op(P, lambda: nc.tensor.transpose(out=tpa[:, k, :], in_=act[:, k * 128:(k + 1) * 128], identity=ident[:]),
                                     reads=[act, ident], writes=[tpa], acc=True)
                            f.op(V, lambda: nc.vector.tensor_copy(out=atb[:], in_=tpa[:]), reads=[tpa], writes=[atb])
                            f.dma(A, actT_d[e, :, :, sl * 128:(sl + 1) * 128].rearrange("c p t -> p c t"), atb[:],
                                  reads=[atb], writes=[dram["actT_d"]], sb=atb, merge=True)
                f.barrier()
            if stop_after <= 8:
                return finish(nc, f, dram)
            with ExitStack() as st:
                PermT = tl(st, "PermT", [128, NSB, TL], BF16)
                ysh = tl(st, "ysh", [128, NSB, 512], BF16)
                ysl = tl(st, "ysl", [128, NSB, 512], BF16)
                ysr = tl(st, "ysr", [128, 512])
                wds = [tl(st, f"wd{i}", [128, 4, 512], BF16) for i in range(3)]
                ats = [tl(st, f"at{i}", [128, 4, RS], BF16) for i in range(3)]
                xts = [tl(st, f"xt{i}", [128, 512]) for i in range(2)]
                pys = [f.ptile(st, f"py{i}", [128, 512], F32) for i in range(4)]
                pos_ = [f.ptile(st, f"po{i}", [128, 512], F32) for i in range(2)]
                ptp = [f.ptile(st, f"ptp{i}", [128, 8, 128], BF16) for i in range(2)]
                for sb in range(NSB):
                    pt_ = ptp[sb % 2]
                    for tb in range(NQB):
                        f.op(P, lambda: nc.tensor.transpose(out=pt_[:, tb, :], in_=Perm[:, tb, sb * 128:(sb + 1) * 128], identity=ident[:]),
                             reads=[Perm, ident], writes=[pt_], acc=True)
                    f.op(V, lambda: nc.vector.tensor_copy(out=PermT[:, sb, :], in_=pt_[:].rearrange("p a b -> p (a b)")), reads=[pt_], writes=[PermT], merge=True)
                wi = 0
                xi = 0
                for dp in range(8):
                    for g in range(8):
                        py = pys[(g % 2) * SBG:(g % 2) * SBG + SBG]
                        for el in range(8):
                            e = g * 8 + el
                            wd, at = wds[wi % 3], ats[wi % 3]
                            wi += 1
                            load_panel(wd, w_ed, e * 512, 4, dp * 512, 512)
                            f.dma(SP, at[:], actT_d[e].rearrange("c p t -> p c t"), writes=[at], sb=at)
                            for sl in range(SBG):
                                for k in range(4):
                                    f.op(P, lambda: nc.tensor.matmul(py[sl][:, :], lhsT=at[:, k, sl * 128:(sl + 1) * 128], rhs=wd[:, k, :],
                                                                      start=(el == 0 and k == 0), stop=(el == 7 and k == 3)),
                                         reads=[at, wd], writes=[py[sl]], acc=True)
                        for sl in range(SBG):
                            sb = g * SBG + sl
                            f.op(A, lambda: nc.scalar.copy(out=ysh[:, sb, :], in_=py[sl][:]), reads=[py[sl]], writes=[ysh], merge=True)
                            f.op(V, lambda: nc.vector.tensor_tensor(out=ysr[:], in0=py[sl][:], in1=ysh[:, sb, :], op=ALU.subtract), reads=[py[sl], ysh], writes=[ysr])
                            f.op(V, lambda: nc.vector.tensor_copy(out=ysl[:, sb, :], in_=ysr[:]), reads=[ysr], writes=[ysl], merge=True)
                    for tb in range(NQB):
                        po, xt = pos_[xi % 2], xts[xi % 2]
                        xi += 1
                        f.dma(SP, xt[:], x1s[tb * 128:(tb + 1) * 128, dp * 512:(dp + 1) * 512], writes=[xt], sb=xt)
                        n_ = 0
                        for part in (ysh, ysl):
                            for sb in range(NSB):
                                f.op(P, lambda: nc.tensor.matmul(po[:, :], lhsT=PermT[:, sb, tb * 128:(tb + 1) * 128], rhs=part[:, sb, :],
                                                                  start=(n_ == 0), stop=(n_ == 2 * NSB - 1)), reads=[PermT, part], writes=[po], acc=True)
                                n_ += 1
                        f.op(V, lambda: nc.vector.tensor_tensor(out=xt[:], in0=po[:], in1=xt[:], op=ALU.add), reads=[po, xt], writes=[xt])
                        f.dma(A, out[tb * 128:(tb + 1) * 128, dp * 512:(dp + 1) * 512], xt[:], reads=[xt], writes=[dram["out"]], sb=xt, merge=True)
                f.barrier()
        return finish(nc, f, dram)

    return nc


def finish(nc, f, dram):
    deps = {}
    for b in f.dma_bufs:
        if b.dcount > 0:
            deps[id(b.dsem)] = (b.dsem, b.dcount)
    for (s_, c_) in f.sem_pool + f.retired:
        deps[id(s_)] = (s_, c_)
    for e in f.engs:
        if e.count > 0:
            deps[id(e.sem)] = (e.sem, e.count)
    f._wait(f.sp, deps)
    return nc


def host_consts():
    ident = np.eye(128, dtype=np.float32)
    rot = np.zeros((64, 64), np.float32)
    for m in range(32):
        rot[m + 32, m] = -1.0
    for m in range(32, 64):
        rot[m - 32, m] = 1.0
    tri = np.triu(np.ones((128, 128), np.float32), 1)
    iota = np.broadcast_to(np.arange(NSLOT, dtype=np.float32), (128, NSLOT)).copy()
    half = 32
    inv = (np.float32(10000.0) ** (-np.arange(half, dtype=np.float32) / np.float32(half))).astype(np.float32)
    invf = np.concatenate([inv, inv]).reshape(64, 1).astype(np.float32)
    goff = np.broadcast_to(np.arange(8, dtype=np.float32) * R_SLOT, (128, 8)).copy()
    return {"c_goff": goff, "c_ident": ident, "c_rot": rot, "c_tri": tri, "c_iota": iota, "c_invf": invf}


def chunked(v, nch):
    return np.ascontiguousarray(np.asarray(v, np.float32).reshape(nch, 128).T)


def make_in_maps(inp, cores):
    a = {k: np.asarray(v) for k, v in inp.items()}
    x = a["x"][0]
    pos = a["positions"][0].astype(np.int32)
    shared = {
        "x_full": x,
        "mem": a["mem"][0],
        "pos_full": pos.reshape(1, T),
        "rel_bias": a["rel_bias"].reshape(1, 32 * 12),
        "mix_norm_g": chunked(a["mix_norm_g"][0], KC),
        "w_in": a["w_in"][0],
        "diff_q_norm_g": a["diff_q_norm_g"][0].reshape(128, 1),
        "diff_k_norm_g": a["diff_k_norm_g"][0].reshape(128, 1),
        "lam_v": np.concatenate([a["diff_lambda_q1"][0], a["diff_lambda_k1"][0], a["diff_lambda_q2"][0],
                                 a["diff_lambda_k2"][0]]).reshape(1, 512),
        "diff_subln_g": a["diff_subln_g"][0].reshape(1, 256),
        "mla_cq_norm_g": chunked(a["mla_cq_norm_g"][0], 12),
        "mla_ckv_norm_g": chunked(a["mla_ckv_norm_g"][0], 4),
        "mla_w_uq": a["mla_w_uq"][0],
        "mla_w_ukv": a["mla_w_ukv"][0],
        "mla_q_norm_g": a["mla_q_norm_g"][0].reshape(192, 1),
        "mla_k_norm_g": a["mla_k_norm_g"][0].reshape(192, 1),
        "mem_norm_g": chunked(a["mem_norm_g"][0], KC),
        "mem_w_kv": a["mem_w_kv"][0],
        "mem_q_norm_g": chunked(a["mem_q_norm_g"][0], 2),
        "mem_k_norm_g": chunked(a["mem_k_norm_g"][0], 2),
        "w_o_diff": a["w_o_diff"][0],
        "w_o_mla": a["w_o_mla"][0],
        "w_o_mem": a["w_o_mem"][0],
        "w_out": a["w_out"][0],
        "ffn_norm_g": chunked(a["ffn_norm_g"][0], KC),
        "w_rt": np.ascontiguousarray(np.concatenate([a["w_route_group"][0], a["w_route_expert"][0]], axis=1)),
        "b_rt": np.concatenate([a["b_route_group"][0], a["b_route_expert"][0]]).reshape(1, 72),
        "w_exp_gate": a["w_exp_gate"][0],
        "w_exp_up": a["w_exp_up"][0],
        "w_exp_down": a["w_exp_down"][0].reshape(64 * 512, D),
    }
    shared.update(host_consts())
    maps = []
    xb = x.reshape(T // 128, 128, D)
    pb = pos.reshape(T // 128, 128)
    for c in cores:
        m = dict(shared)
        m["x_own"] = np.ascontiguousarray(xb[c::NCORES].reshape(TL, D))
        m["pos_own"] = np.ascontiguousarray(pb[c::NCORES].reshape(1, TL))
        maps.append(m)
    return maps


def kernel(**inputs):
    nc = build()
    cores = list(range(NCORES))
    in_maps = make_in_maps(inputs, cores)
    res = run_bass_kernel_spmd(nc, in_maps, core_ids=cores)
    outb = np.zeros((T // 128, 128, D), np.float32)
    for c in cores:
        outb[c::NCORES] = np.asarray(res.results[c]["out"]).reshape(TL // 128, 128, D)
    return outb.reshape(1, T, D)
```

```python
import math
from contextlib import ExitStack
import numpy as np
import concourse.bass as bass
import concourse.mybir as mybir
from concourse.bass_utils import run_bass_kernel_spmd

F32 = mybir.dt.float32
BF16 = mybir.dt.bfloat16
I32 = mybir.dt.int32
AF = mybir.ActivationFunctionType
ALU = mybir.AluOpType
AX = mybir.AxisListType

NCORES = 8
T = 8192
TL = 1024
D = 4096
KC = 32
NMEM = 256
EPS = 1e-6
IN_COLS = 20032
C_DQ, C_DK, C_DV, C_CQ, C_CKV, C_KR, C_MQ, C_G = 0, 1536, 3072, 4608, 6144, 6656, 6720, 7744
R_SLOT = 256
NSLOT = 8 * R_SLOT
SEM_CAP = 24000


class Buf:
    __slots__ = ("name", "writes", "reads", "dsem", "dcount")

    def __init__(self, name):
        self.name = name
        self.writes = {}
        self.reads = {}
        self.dsem = None
        self.dcount = 0


class TT:
    def __init__(self, t, name):
        self.t = t
        self.b = Buf(name)
        self.psum = False

    def __getitem__(self, k):
        return self.t[k]


class Eng:
    def __init__(self, fw, name, h):
        self.fw, self.name, self.h = fw, name, h
        self.sem = None
        self.count = 0
        self.waited = {}
        self.allsems = []

    def new_sem(self):
        self.sem = self.fw.alloc_sem(self.name)
        self.count = 0
        self.allsems.append(self.sem)


class FW:
    def __init__(self, nc, stack):
        self.nc = nc
        self.stack = stack
        self.nsem = 0
        self.pe = Eng(self, "pe", nc.tensor)
        self.act = Eng(self, "act", nc.scalar)
        self.dve = Eng(self, "dve", nc.vector)
        self.pool = Eng(self, "pool", nc.gpsimd)
        self.sp = Eng(self, "sp", nc.sync)
        self.engs = [self.pe, self.act, self.dve, self.pool, self.sp]
        for e in self.engs:
            e.new_sem()
        self.ninst = 0
        self.dma_bufs = []
        self.sem_pool = []
        self.retired = []
        self.uid = 0

    def alloc_sem(self, name):
        self.nsem += 1
        return self.stack.enter_context(self.nc.semaphore(f"s{self.nsem}_{name}"))

    def tile(self, st, name, shape, dt):
        self.uid += 1
        nm = f"{name}_{self.uid}"
        tt = TT(st.enter_context(self.nc.sbuf_tensor(nm, shape, dt)), nm)
        if st is not self.stack:
            st.callback(self.release, tt)
        return tt

    def release(self, tt):
        b = tt.b
        if b.dsem is not None:
            if b in self.dma_bufs:
                self.dma_bufs.remove(b)
            if b.dcount < SEM_CAP - 4000:
                self.sem_pool.append((b.dsem, b.dcount))
            else:
                self.retired.append((b.dsem, b.dcount))
            b.dsem = None

    def ptile(self, st, name, shape, dt):
        self.uid += 1
        nm = f"{name}_{self.uid}"
        tt = TT(st.enter_context(self.nc.psum_tensor(nm, shape, dt)), nm)
        tt.psum = True
        return tt

    def _wait(self, eng, deps):
        for key, (sem, val) in deps.items():
            if eng.waited.get(key, 0) < val:
                eng.h.wait_ge(sem, val)
                eng.waited[key] = val

    def _deps(self, reads, writes, acc):
        deps = {}

        def add(d):
            for k, sv in d.items():
                if k not in deps or deps[k][1] < sv[1]:
                    deps[k] = sv
        for b in reads:
            add(b.b.writes)
            if b.psum:
                add(b.b.reads)
        for b in writes:
            add(b.b.reads)
            add(b.b.writes)
        return deps

    def op(self, eng, fn, reads=(), writes=(), acc=False, merge=False):
        deps = self._deps(reads, writes, acc)
        if acc:
            deps.pop(id(eng.sem), None)
        self._wait(eng, deps)
        if eng.count >= SEM_CAP:
            eng.new_sem()
        inst = fn()
        inst.then_inc(eng.sem, 1)
        eng.count += 1
        self.ninst += 1
        ev = (eng.sem, eng.count)
        key = id(eng.sem)
        for w in writes:
            if merge or acc:
                w.b.writes[key] = ev
            else:
                w.b.writes = {key: ev}
                w.b.reads = {}
        for r in reads:
            r.b.reads[key] = ev
        return inst

    def dma(self, eng, out, in_, reads=(), writes=(), sb=None, merge=False, **kw):
        deps = self._deps(reads, writes, False)
        self._wait(eng, deps)
        b = sb.b
        if b.dsem is not None and b.dcount >= SEM_CAP:
            self.retired.append((b.dsem, b.dcount))
            self.dma_bufs.remove(b)
            b.dsem = None
        if b.dsem is None:
            if self.sem_pool:
                b.dsem, b.dcount = self.sem_pool.pop()
            else:
                b.dsem = self.alloc_sem("d")
                b.dcount = 0
            self.dma_bufs.append(b)
        inst = eng.h.dma_start(out=out, in_=in_, **kw)
        inst.then_inc(b.dsem, 16)
        b.dcount += 16
        self.ninst += 1
        ev = (b.dsem, b.dcount)
        key = id(b.dsem)
        for w in writes:
            if merge:
                w.b.writes[key] = ev
            else:
                w.b.writes = {key: ev}
                w.b.reads = {}
        for r in reads:
            r.b.reads[key] = ev
        return inst

    def barrier(self):
        deps = {}
        for e in self.engs:
            if e.count > 0:
                deps[id(e.sem)] = (e.sem, e.count)
        for b in self.dma_bufs:
            if b.dcount > 0:
                deps[id(b.dsem)] = (b.dsem, b.dcount)
        for (s_, c_) in self.retired:
            deps[id(s_)] = (s_, c_)
        for e in self.engs:
            self._wait(e, deps)


def t5_thresholds():
    n = np.arange(0, 4096, dtype=np.int32)
    max_exact = 16
    nf = np.maximum(n, 1).astype(np.float32)
    large = max_exact + (np.log(nf / np.float32(max_exact)) / np.float32(math.log(128 / 16))
                         * np.float32(16)).astype(np.int32)
    large = np.minimum(large, 31)
    bucket = np.where(n < max_exact, n, large)
    thr = []
    for b in range(1, 32):
        thr.append(int(np.argmax(bucket >= b)))
    return thr


def build(stop_after=99, dbg=False, skip=()):
    nc = bass.Bass("TRN2", target_bir_lowering=False)

    def din(name, shape, dt=F32):
        return nc.dram_tensor(name, list(shape), dt, kind="ExternalInput").ap()

    def dscr(name, shape, dt):
        kind = "ExternalOutput" if dbg else "Internal"
        return nc.dram_tensor(name, list(shape), dt, kind=kind).ap()

    x_full = din("x_full", [T, D])
    x_own = din("x_own", [TL, D])
    mem = din("mem", [NMEM, D])
    pos_full = din("pos_full", [1, T], I32)
    pos_own = din("pos_own", [1, TL], I32)
    rel_bias = din("rel_bias", [1, 32 * 12])
    mix_g = din("mix_norm_g", [128, KC])
    w_in = din("w_in", [D, IN_COLS])
    dq_g = din("diff_q_norm_g", [128, 1])
    dk_g = din("diff_k_norm_g", [128, 1])
    lam_v = din("lam_v", [1, 4 * 128])
    subln_g = din("diff_subln_g", [1, 256])
    cq_g = din("mla_cq_norm_g", [128, 12])
    ckv_g = din("mla_ckv_norm_g", [128, 4])
    w_uq = din("mla_w_uq", [1536, 2304])
    w_ukv = din("mla_w_ukv", [512, 3072])
    mq_g = din("mla_q_norm_g", [192, 1])
    mk_g = din("mla_k_norm_g", [192, 1])
    mem_g = din("mem_norm_g", [128, KC])
    mem_wkv = din("mem_w_kv", [D, 2048])
    memq_g = din("mem_q_norm_g", [128, 2])
    memk_g = din("mem_k_norm_g", [128, 2])
    w_o_diff = din("w_o_diff", [1536, D])
    w_o_mla = din("w_o_mla", [1536, D])
    w_o_mem = din("w_o_mem", [1024, D])
    w_out = din("w_out", [D, D])
    ffn_g = din("ffn_norm_g", [128, KC])
    w_rt = din("w_rt", [D, 72])
    b_rt = din("b_rt", [1, 72])
    if stop_after >= 6:
        w_eg = din("w_exp_gate", [64, D, 512])
        w_eu = din("w_exp_up", [64, D, 512])
        w_ed = din("w_exp_down", [64 * 512, D])
    c_ident = din("c_ident", [128, 128])
    c_rot = din("c_rot", [64, 64])
    c_tri = din("c_tri", [128, 128])
    c_iota = din("c_iota", [128, NSLOT])
    c_invf = din("c_invf", [64, 1])
    c_goff = din("c_goff", [128, 8])
    out = nc.dram_tensor("out", [TL, D], F32, kind="ExternalOutput").ap()

    hT_all = dscr("hT_all", [KC, 128, T], BF16)
    QdT = dscr("QdT", [12, 128, TL], BF16)
    KdT = dscr("KdT", [12, 128, T], BF16)
    Vd1 = dscr("Vd1", [6, T, 257], BF16)
    QmT = dscr("QmT", [12, 192, TL], BF16)
    KmT = dscr("KmT", [12, 192, T], BF16)
    Vm1 = dscr("Vm1", [12, T, 129], BF16)
    QeT = dscr("QeT", [4, 256, TL], BF16)
    gT = dscr("gT", [96, 128, TL], F32)
    x1s = dscr("x1s", [TL, D], F32)
    ys = dscr("ys", [NSLOT, D], F32)

    thr = t5_thresholds()

    with ExitStack() as top:
        f = FW(nc, top)
        V, A, P, G = f.dve, f.act, f.pe, f.pool
        SP = f.sp
        dram = {n: TT(None, n) for n in ["hT_all", "QdT", "KdT", "Vd1", "QmT", "KmT", "Vm1", "QeT", "gT", "x1s", "ys", "out"]}

        def tl(st, name, shape, dt=F32):
            return f.tile(st, name, shape, dt)

        ident_f = tl(top, "ident_f", [128, 128])
        ident = tl(top, "ident", [128, 128], BF16)
        ones = tl(top, "ones", [128, 128], BF16)
        rotT = tl(top, "rotT", [64, 64], BF16)
        rot_f = tl(top, "rot_f", [64, 64])
        mixg = tl(top, "mixg", [128, KC])
        dqg = tl(top, "dqg", [128, 1])
        dkg = tl(top, "dkg", [128, 1])
        cqg = tl(top, "cqg", [128, 12])
        ckvg = tl(top, "ckvg", [128, 4])
        mqgA = tl(top, "mqgA", [128, 1])
        mqgB = tl(top, "mqgB", [64, 1])
        mkgA = tl(top, "mkgA", [128, 1])
        mkgB = tl(top, "mkgB", [64, 1])
        memqg = tl(top, "memqg", [128, 2])
        invf = tl(top, "invf", [64, 1])
        memkg = tl(top, "memkg", [128, 2])
        memg = tl(top, "memg", [128, KC])
        ffng = tl(top, "ffng", [128, KC])
        for dst, src in [(ident_f, c_ident), (rot_f, c_rot), (mixg, mix_g), (dqg, dq_g), (dkg, dk_g), (cqg, cq_g),
                         (ckvg, ckv_g), (mqgA, mq_g[0:128, :]), (mqgB, mq_g[128:192, :]), (mkgA, mk_g[0:128, :]),
                         (mkgB, mk_g[128:192, :]), (memqg, memq_g), (invf, c_invf), (memkg, memk_g), (memg, mem_g), (ffng, ffn_g)]:
            f.dma(SP, dst[:], src, writes=[dst], sb=dst)
        f.op(V, lambda: nc.vector.tensor_copy(out=ident[:], in_=ident_f[:]), reads=[ident_f], writes=[ident])
        f.op(V, lambda: nc.vector.tensor_copy(out=rotT[:], in_=rot_f[:]), reads=[rot_f], writes=[rotT])
        f.op(V, lambda: nc.vector.memset(ones[:], 1.0), writes=[ones])

        NQB = TL // 128
        NTT_ALL = T // 512
        NTT_OWN = TL // 512
        hT_ownD = dscr("hT_ownD", [KC, 128, TL], BF16)
        dram["hT_ownD"] = TT(None, "hT_ownD")
        epsb = tl(top, "epsb", [128, 1])
        f.op(V, lambda: nc.vector.memset(epsb[:], EPS), writes=[epsb])

        def rstd_from(ps_ss, out_t, inv_n, np_=128, n=512):
            f.op(A, lambda: nc.scalar.activation(out=out_t[0:np_, 0:n], in_=ps_ss[0:np_, 0:n], func=AF.Sqrt,
                                                  scale=inv_n, bias=epsb[0:np_, 0:1]),
                 reads=[ps_ss, epsb], writes=[out_t])
            f.op(V, lambda: nc.vector.reciprocal(out=out_t[0:np_, 0:n], in_=out_t[0:np_, 0:n]),
                 reads=[out_t], writes=[out_t])

        def phase_norm(src, ntiles, gain_t, dstT, dkey, ss_out=None):
            with ExitStack() as st:
                xts = [tl(st, f"xt{i}", [128, D]) for i in range(2)]
                xbs = [tl(st, f"xb{i}", [128, D], BF16) for i in range(2)]
                sss = [tl(st, f"ss{i}", [128, 1]) for i in range(2)]
                junk = tl(st, "junk", [128, D], BF16)
                hts = [tl(st, f"ht{i}", [128, KC, 128], BF16) for i in range(2)]
                pts = [f.ptile(st, f"pt{i}", [128, 8, 128], BF16) for i in range(4)]
                for i in range(ntiles):
                    xt, xb, ss, ht = xts[i % 2], xbs[i % 2], sss[i % 2], hts[i % 2]
                    f.dma(SP, xt[:], src[i * 128:(i + 1) * 128, :], writes=[xt], sb=xt)
                    f.op(A, lambda: nc.scalar.activation(out=junk[:], in_=xt[:], func=AF.Square, accum_out=ss[:]),
                         reads=[xt], writes=[junk, ss])
                    f.op(A, lambda: nc.scalar.activation(out=ss[:], in_=ss[:], func=AF.Sqrt, scale=1.0 / D, bias=epsb[:, 0:1]),
                         reads=[ss, epsb], writes=[ss])
                    f.op(V, lambda: nc.vector.reciprocal(out=ss[:], in_=ss[:]), reads=[ss], writes=[ss])
                    f.op(A, lambda: nc.scalar.activation(out=xb[:], in_=xt[:], func=AF.Copy, scale=ss[:, 0:1]),
                         reads=[xt, ss], writes=[xb])
                    for q in range(4):
                        pt = pts[q]
                        for k in range(8):
                            c = q * 8 + k
                            f.op(P, lambda: nc.tensor.transpose(out=pt[:, k, :], in_=xb[:, c * 128:(c + 1) * 128], identity=ident[:]),
                                 reads=[xb, ident], writes=[pt], acc=True)
                        gb = gain_t[:, q * 8:(q + 1) * 8].unsqueeze(2).to_broadcast([128, 8, 128])
                        f.op(V, lambda: nc.vector.tensor_tensor(out=ht[:, q * 8:(q + 1) * 8, :], in0=pt[:], in1=gb, op=ALU.mult),
                             reads=[pt, gain_t], writes=[ht], merge=True)
                    f.dma(A, dstT[:, :, i * 128:(i + 1) * 128].rearrange("c p t -> p c t"), ht[:],
                          reads=[ht], writes=[dram[dkey]], sb=ht, merge=True)
                f.barrier()

        if "p1" not in skip:
            phase_norm(x_full, T // 128, mixg, hT_all, "hT_all")
            phase_norm(x_own, TL // 128, mixg, hT_ownD, "hT_ownD")
        if stop_after <= 1:
            return finish(nc, f, dram)

        pan_serial = TT(None, "pan_serial")

        def load_panel(pan, W, r0, nk, c0, ncols):
            f.dma(G, pan[:, 0:nk, 0:ncols], W[r0:r0 + nk * 128, c0:c0 + ncols].rearrange("(k p) c -> p k c", p=128),
                  writes=[pan], sb=pan)

        def load_hT(ht, srcT, t0, nk=KC, n=512, k0=0):
            f.dma(SP, ht[:, 0:nk, 0:n], srcT[k0:k0 + nk, :, t0:t0 + n].rearrange("c p t -> p c t"),
                  reads=[dram_of[id(srcT)]] if id(srcT) in dram_of else [], writes=[ht], sb=ht)

        dram_of = {id(hT_all): dram["hT_all"], id(hT_ownD): dram["hT_ownD"]}

        def mm_fm(ps, np_, pan, c0, ht, nk, n=512, first=True, last=True, extra_reads=()):
            for k in range(nk):
                f.op(P, lambda: nc.tensor.matmul(ps[0:np_, 0:n], lhsT=pan[:, k, c0:c0 + np_], rhs=ht[:, k, 0:n],
                                                  start=(first and k == 0), stop=(last and k == nk - 1)),
                     reads=[pan, ht], writes=[ps], acc=True)

        def mm_tm(ps, ht, t0, pan, c0, ncols, nk):
            for k in range(nk):
                f.op(P, lambda: nc.tensor.matmul(ps[:, 0:ncols], lhsT=ht[:, k, t0:t0 + 128], rhs=pan[:, k, c0:c0 + ncols],
                                                  start=(k == 0), stop=(k == nk - 1)),
                     reads=[pan, ht], writes=[ps], acc=True)

        def fm_rms(st_tiles, chunks, dim, gains, outs, pre_ss=None, n=512):
            sq, pss, rstd = st_tiles
            nchk = len(chunks)
            for i, (ps, np_) in enumerate(chunks):
                f.op(A, lambda: nc.scalar.activation(out=sq[0:np_, 0:n], in_=ps[0:np_, 0:n], func=AF.Square),
                     reads=[ps], writes=[sq])
                f.op(P, lambda: nc.tensor.matmul(pss[:, 0:n], lhsT=ones[0:np_, :], rhs=sq[0:np_, 0:n],
                                                  start=(i == 0 and pre_ss is None), stop=(i == nchk - 1)),
                     reads=[ones, sq], writes=[pss], acc=True)
            rstd_from(pss, rstd, 1.0 / dim, n=n)
            for (ps, np_), g, (o, _) in zip(chunks, gains, outs):
                f.op(V, lambda: nc.vector.scalar_tensor_tensor(out=o[0:np_, 0:n], in0=ps[0:np_, 0:n], scalar=g, in1=rstd[0:np_, 0:n],
                                                                op0=ALU.mult, op1=ALU.mult),
                     reads=[ps, rstd], writes=[o])
            return rstd

        def pass_fm_rms(srcT, ntt, c0, nmaps, grp, dim, gain_fn, dstT, dkey, row_fn, W=None, n=512):
            W = w_in if W is None else W
            with ExitStack() as st:
                pans = [tl(st, f"pan{i}", [128, KC, 512], BF16) for i in range(2)]
                hts = [tl(st, f"ht{i}", [128, KC, 512], BF16) for i in range(2)]
                sqs_ = [tl(st, f"sq{i}", [128, 512], BF16) for i in range(2)]
                rstds_ = [tl(st, f"rstd{i}", [128, 512]) for i in range(2)]
                obs = [tl(st, f"ob{i}", [128, 512], BF16) for i in range(4)]
                psss_ = [f.ptile(st, f"pss{i}", [128, 512], F32) for i in range(2)]
                psc = [f.ptile(st, f"psc{i}", [128, 512], F32) for i in range(4)]
                ei = 0
                npan = (nmaps + 3) // 4
                it = 0
                for pi in range(npan):
                    pan = pans[pi % 2]
                    ncol = min(512, (nmaps - pi * 4) * 128)
                    load_panel(pan, W, 0, KC, c0 + pi * 512, ncol)
                    for tt in range(ntt):
                        ht = hts[it % 2]
                        it += 1
                        load_hT(ht, srcT, tt * n, n=n)
                        for sg in range(0, ncol // 128, grp):
                            chunks = []
                            for s_ in range(grp):
                                ps = psc[(sg + s_) % 4]
                                mm_fm(ps, 128, pan, (sg + s_) * 128, ht, KC, n=n)
                                chunks.append((ps, 128))
                            outs = [(obs[(sg + s_) % 4], 128) for s_ in range(grp)]
                            fm_rms((sqs_[ei % 2], psss_[ei % 2], rstds_[ei % 2]), chunks, dim, [gain_fn(s_) for s_ in range(grp)], outs, n=n)
                            ei += 1
                            for s_ in range(grp):
                                m = pi * 4 + sg + s_
                                f.dma(A, row_fn(m)[:, tt * n:(tt + 1) * n], outs[s_][0][:, 0:n],
                                      reads=[outs[s_][0]], writes=[dram[dkey]], sb=outs[s_][0], merge=True)
                f.barrier()

        if "p2" not in skip:
            pass_fm_rms(hT_ownD, NTT_OWN, C_DQ, 12, 1, 128, lambda s_: dqg[:, 0:1], QdT, "QdT", lambda m: QdT[m])
            pass_fm_rms(hT_all, NTT_ALL, C_DK, 12, 1, 128, lambda s_: dkg[:, 0:1], KdT, "KdT", lambda m: KdT[m])
            pass_fm_rms(hT_ownD, NTT_OWN, C_MQ, 8, 2, 256, lambda s_: memqg[:, s_:s_ + 1], QeT, "QeT",
                        lambda m: QeT[m // 2, (m % 2) * 128:(m % 2) * 128 + 128, :])
        if stop_after <= 2:
            return finish(nc, f, dram)

        if "p3" not in skip:
            with ExitStack() as st:
                pans = [tl(st, f"pan{i}", [128, KC, 512], BF16) for i in range(2)]
                hts = [tl(st, f"ht{i}", [128, KC, 512], BF16) for i in range(2)]
                gos = [tl(st, f"go{i}", [128, 512]) for i in range(4)]
                v1s = [tl(st, f"v1{i}", [128, 2, 257], BF16) for i in range(2)]
                psc = [f.ptile(st, f"psc{i}", [128, 512], F32) for i in range(4)]
                for v1 in v1s:
                    f.op(V, lambda: nc.vector.memset(v1[:], 1.0), writes=[v1])
                it = 0
                for pi in range(24):
                    pan = pans[pi % 2]
                    load_panel(pan, w_in, 0, KC, C_G + pi * 512, 512)
                    for tt in range(NTT_OWN):
                        ht = hts[it % 2]
                        it += 1
                        load_hT(ht, hT_ownD, tt * 512)
                        for sc in range(4):
                            ps, go = psc[sc], gos[sc]
                            mm_fm(ps, 128, pan, sc * 128, ht, KC)
                            f.op(A, lambda: nc.scalar.activation(out=go[:], in_=ps[:], func=AF.Sigmoid), reads=[ps], writes=[go])
                            f.dma(A, gT[pi * 4 + sc, :, tt * 512:(tt + 1) * 512], go[:], reads=[go], writes=[dram["gT"]], sb=go, merge=True)
                for pi in range(3):
                    pan = pans[pi % 2]
                    load_panel(pan, w_in, 0, KC, C_DV + pi * 512, 512)
                    for tt in range(NTT_ALL):
                        ht = hts[it % 2]
                        it += 1
                        load_hT(ht, hT_all, tt * 512)
                        for tb in range(4):
                            ps, v1 = psc[tb], v1s[tb % 2]
                            mm_tm(ps, ht, tb * 128, pan, 0, 512, KC)
                            f.op(V, lambda: nc.vector.tensor_copy(out=v1[:, :, 0:256], in_=ps[:].rearrange("p (h c) -> p h c", h=2)),
                                 reads=[ps], writes=[v1])
                            r0 = tt * 512 + tb * 128
                            f.dma(A, Vd1[pi * 2:pi * 2 + 2, r0:r0 + 128, :].rearrange("h t c -> t h c"), v1[:],
                                  reads=[v1], writes=[dram["Vd1"]], sb=v1, merge=True)
                f.barrier()
        if stop_after <= 3:
            return finish(nc, f, dram)

        NCg = NCORES
        NEM = NCg + 1
        OT_d = dscr("OT_d", [KC, 128, TL], BF16)
        dram["OT_d"] = TT(None, "OT_d")
        att_st = ExitStack()
        EM = tl(att_st, "EM", [128, 12, NEM * 128], BF16)
        MK = tl(att_st, "MK", [128, NEM * 128], BF16)
        rb = tl(att_st, "rb", [128, 32 * 12])
        lam = tl(att_st, "lam", [128, 1])
        nlam = tl(att_st, "nlam", [128, 1])
        sublng = tl(att_st, "sublng", [128, 256])
        with ExitStack() as st:
            posq_i = tl(st, "posq_i", [128, 128], I32)
            posk_i = tl(st, "posk_i", [128, NEM], I32)
            posq = tl(st, "posq", [128, 128])
            posk = tl(st, "posk", [128, NEM])
            dist = tl(st, "dist", [128, NEM * 128])
            mask = tl(st, "mask", [128, NEM * 128])
            ind = tl(st, "ind", [128, NEM * 128])
            acc = tl(st, "acc", [128, 12, NEM * 128])
            delta = tl(st, "delta", [128, 31 * 12])
            base = tl(st, "base", [128, 12])
            lv = tl(st, "lv", [128, 512])
            lp = tl(st, "lp", [128, 256])
            ls = tl(st, "ls", [128, 2])
            f.dma(SP, posq_i[:], pos_own[0:1, 128:256].partition_broadcast(128), writes=[posq_i], sb=posq_i)
            f.dma(SP, posk_i[:], pos_full[0, (NCg - 1) * 128:(2 * NCg) * 128].rearrange("(i p) -> p i", p=128),
                  writes=[posk_i], sb=posk_i, allow_slow_non_contiguous=True)
            f.dma(SP, rb[:], rel_bias[0:1, :].partition_broadcast(128), writes=[rb], sb=rb)
            f.dma(SP, lv[:], lam_v[0:1, :].partition_broadcast(128), writes=[lv], sb=lv)
            f.dma(SP, sublng[:], subln_g[0:1, :].partition_broadcast(128), writes=[sublng], sb=sublng)
            f.op(V, lambda: nc.vector.tensor_copy(out=posq[:], in_=posq_i[:]), reads=[posq_i], writes=[posq])
            f.op(V, lambda: nc.vector.tensor_copy(out=posk[:], in_=posk_i[:]), reads=[posk_i], writes=[posk])
            for i in range(NEM):
                f.op(V, lambda: nc.vector.tensor_scalar(out=dist[:, i * 128:(i + 1) * 128], in0=posq[:], scalar1=posk[:, i:i + 1],
                                                          scalar2=None, op0=ALU.subtract), reads=[posq, posk], writes=[dist], merge=True)
            f.op(V, lambda: nc.vector.tensor_single_scalar(out=mask[:], in_=dist[:], scalar=0.0, op=ALU.is_ge), reads=[dist], writes=[mask])
            f.op(V, lambda: nc.vector.tensor_copy(out=MK[:], in_=mask[:]), reads=[mask], writes=[MK])
            f.op(V, lambda: nc.vector.tensor_tensor(out=delta[:], in0=rb[:, 12:32 * 12], in1=rb[:, 0:31 * 12], op=ALU.subtract),
                 reads=[rb], writes=[delta])
            f.op(V, lambda: nc.vector.tensor_tensor(out=base[:], in0=rb[:, 0:12], in1=rb[:, 31 * 12:32 * 12], op=ALU.subtract),
                 reads=[rb], writes=[base])
            f.op(G, lambda: nc.gpsimd.memset(acc[:], 0.0), writes=[acc])
            for b in range(1, 32):
                f.op(V, lambda: nc.vector.tensor_single_scalar(out=ind[:], in_=dist[:], scalar=float(thr[b - 1]), op=ALU.is_ge),
                     reads=[dist], writes=[ind])
                for m in range(12):
                    eng = V
                    h_ = nc.vector
                    f.op(eng, lambda: h_.scalar_tensor_tensor(out=acc[:, m, :], in0=ind[:], scalar=delta[:, (b - 1) * 12 + m:(b - 1) * 12 + m + 1],
                                                               in1=acc[:, m, :], op0=ALU.mult, op1=ALU.add),
                         reads=[ind, delta, acc], writes=[acc], merge=True)
            for m in range(12):
                f.op(A, lambda: nc.scalar.activation(out=acc[:, m, :], in_=acc[:, m, :], func=AF.Exp, bias=base[:, m:m + 1]),
                     reads=[acc, base], writes=[acc], merge=True)
                f.op(V, lambda: nc.vector.tensor_tensor(out=EM[:, m, :], in0=acc[:, m, :], in1=mask[:], op=ALU.mult),
                     reads=[acc, mask], writes=[EM], merge=True)
            f.op(V, lambda: nc.vector.tensor_tensor(out=lp[:].rearrange("p (a c) -> p a c", a=2),
                                                      in0=lv[:].rearrange("p (a b c) -> p a b c", a=2, b=2)[:, :, 0, :],
                                                      in1=lv[:].rearrange("p (a b c) -> p a b c", a=2, b=2)[:, :, 1, :], op=ALU.mult),
                 reads=[lv], writes=[lp])
            f.op(V, lambda: nc.vector.reduce_sum(out=ls[:], in_=lp[:].rearrange("p (a c) -> p a c", a=2), axis=AX.X),
                 reads=[lp], writes=[ls])
            f.op(A, lambda: nc.scalar.activation(out=ls[:], in_=ls[:], func=AF.Exp), reads=[ls], writes=[ls])
            f.op(V, lambda: nc.vector.tensor_tensor(out=lam[:], in0=ls[:, 0:1], in1=ls[:, 1:2], op=ALU.subtract), reads=[ls], writes=[lam])
            f.op(V, lambda: nc.vector.tensor_scalar(out=nlam[:], in0=lam[:], scalar1=0.2, scalar2=-1.0, op0=ALU.add, op1=ALU.mult),
                 reads=[lam], writes=[nlam])
            f.op(V, lambda: nc.vector.tensor_scalar(out=sublng[:], in0=sublng[:], scalar1=0.8, scalar2=None, op0=ALU.mult),
                 reads=[sublng], writes=[sublng])
            f.barrier()

        def attention(st, QT_list, KT_list, V1, dv, nkb_fn, scale, bias_ap_fn, em_fn, units, epilogue):
            sps = [f.ptile(st, f"sps{i}", [128, 4, 128], F32) for i in range(3)]
            ops_ = [f.ptile(st, f"ops{i}", [128, 512], F32) for i in range(4)]
            pTs = [tl(st, f"pT{i}", [128, 4, 128], BF16) for i in range(4)]
            gi = 0
            for j in range(NQB):
                nkb = nkb_fn(j)
                for u in range(units):
                    ops = ops_[(j * units + u) % 4]
                    nch = len(QT_list[u])

                    def emit_qk(g0):
                        nonlocal gi
                        sp, pT = sps[gi % 3], pTs[gi % 4]
                        gi += 1
                        nb = min(4, nkb - g0)
                        for i in range(nb):
                            kb = g0 + i
                            for ci in range(nch):
                                qt, np_ = QT_list[u][ci]
                                kt, _ = KT_list[u][ci]
                                f.op(P, lambda: nc.tensor.matmul(sp[:, i, :], lhsT=kt[0:np_, kb * 128:(kb + 1) * 128],
                                                                  rhs=qt[0:np_, j * 128:(j + 1) * 128], start=(ci == 0), stop=(ci == nch - 1)),
                                     reads=[kt, qt], writes=[sp], acc=True)
                        return sp, pT, nb, g0

                    glist = list(range(0, nkb, 4))
                    pend = emit_qk(glist[0])
                    for gx in range(len(glist)):
                        sp, pT, nb, g0 = pend
                        if gx + 1 < len(glist):
                            pend = emit_qk(glist[gx + 1])
                        bap = bias_ap_fn(u)
                        f.op(A, lambda: nc.scalar.activation(out=pT[:, 0:nb, :], in_=sp[:, 0:nb, :], func=AF.Exp, scale=scale,
                                                              **({"bias": bap} if bap is not None else {})),
                             reads=[sp], writes=[pT])
                        for i in range(nb):
                            kb = g0 + i
                            em = em_fn(u, j, kb)
                            if em is not None:
                                f.op(V, lambda: nc.vector.tensor_tensor(out=pT[:, i, :], in0=pT[:, i, :], in1=em, op=ALU.mult),
                                     reads=[pT, EM, MK], writes=[pT], merge=True)
                        for i in range(nb):
                            kb = g0 + i
                            f.op(P, lambda: nc.tensor.matmul(ops[:, 0:dv + 1], lhsT=pT[:, i, :], rhs=V1[:, kb, :],
                                                              start=(kb == 0), stop=(kb == nkb - 1)),
                                 reads=[pT, V1], writes=[ops], acc=True)
                    epilogue(j, u, ops)

        def run_diff():
            for h in range(6):
                with ExitStack() as st:
                    KT = [tl(st, f"KT{i}", [128, T], BF16) for i in range(2)]
                    QT = [tl(st, f"QT{i}", [128, TL], BF16) for i in range(2)]
                    V1 = tl(st, "V1", [128, T // 128, 257], BF16)
                    o1 = tl(st, "o1", [128, 256])
                    ob = tl(st, "ob", [128, 256], BF16)
                    rr = tl(st, "rr", [128, 4])
                    jk = tl(st, "jk", [128, 256])
                    otb = tl(st, "otb", [128, 2, 128], BF16)
                    tps = f.ptile(st, "tps", [128, 2, 128], BF16)
                    for i, m in enumerate((h, h + 6)):
                        f.dma(SP, KT[i][:], KdT[m], reads=[dram["KdT"]], writes=[KT[i]], sb=KT[i])
                        f.dma(SP, QT[i][:], QdT[m], reads=[dram["QdT"]], writes=[QT[i]], sb=QT[i])
                    f.dma(SP, V1[:], Vd1[h].rearrange("(b p) c -> p b c", p=128), reads=[dram["Vd1"]], writes=[V1], sb=V1)
                    state = {}

                    def epi(j, u, ops):
                        state[u] = ops
                        if u == 0:
                            return
                        O1, O2 = state[0], state[1]
                        f.op(V, lambda: nc.vector.reciprocal(out=rr[:, 0:1], in_=O1[:, 256:257]), reads=[O1], writes=[rr])
                        f.op(V, lambda: nc.vector.reciprocal(out=rr[:, 1:2], in_=O2[:, 256:257]), reads=[O2], writes=[rr])
                        f.op(V, lambda: nc.vector.tensor_tensor(out=rr[:, 1:2], in0=rr[:, 1:2], in1=nlam[:], op=ALU.mult), reads=[rr, nlam], writes=[rr])
                        f.op(V, lambda: nc.vector.tensor_scalar(out=o1[:], in0=O1[:, 0:256], scalar1=rr[:, 0:1], scalar2=None, op0=ALU.mult),
                             reads=[O1, rr], writes=[o1])
                        f.op(V, lambda: nc.vector.scalar_tensor_tensor(out=o1[:], in0=O2[:, 0:256], scalar=rr[:, 1:2], in1=o1[:],
                                                                        op0=ALU.mult, op1=ALU.add), reads=[O2, rr, o1], writes=[o1])
                        f.op(A, lambda: nc.scalar.activation(out=jk[:], in_=o1[:], func=AF.Square, accum_out=rr[:, 2:3]), reads=[o1], writes=[jk, rr])
                        f.op(A, lambda: nc.scalar.activation(out=rr[:, 2:3], in_=rr[:, 2:3], func=AF.Sqrt, scale=1.0 / 256, bias=epsb[:, 0:1]),
                             reads=[rr, epsb], writes=[rr])
                        f.op(V, lambda: nc.vector.reciprocal(out=rr[:, 3:4], in_=rr[:, 2:3]), reads=[rr], writes=[rr])
                        f.op(V, lambda: nc.vector.scalar_tensor_tensor(out=ob[:], in0=o1[:], scalar=rr[:, 3:4], in1=sublng[:],
                                                                        op0=ALU.mult, op1=ALU.mult), reads=[o1, rr, sublng], writes=[ob])
                        for c2 in range(2):
                            f.op(P, lambda: nc.tensor.transpose(out=tps[:, c2, :], in_=ob[:, c2 * 128:(c2 + 1) * 128], identity=ident[:]),
                                 reads=[ob, ident], writes=[tps], acc=True)
                        f.op(V, lambda: nc.vector.tensor_copy(out=otb[:], in_=tps[:]), reads=[tps], writes=[otb])
                        f.dma(A, OT_d[2 * h:2 * h + 2, :, j * 128:(j + 1) * 128].rearrange("c p t -> p c t"), otb[:],
                              reads=[otb], writes=[dram["OT_d"]], sb=otb, merge=True)

                    def em_fn(u, j, kb):
                        i = kb - NCg * j
                        if i < -1:
                            return None
                        m = h + 6 * u
                        return EM[:, m, (i + 1) * 128:(i + 2) * 128]

                    attention(st, [[(QT[0], 128)], [(QT[1], 128)]], [[(KT[0], 128)], [(KT[1], 128)]], V1, 256,
                              lambda j: NCg * j + NCg, 128 ** -0.5, lambda u: rb[:, 31 * 12 + h + 6 * u:31 * 12 + h + 6 * u + 1],
                              em_fn, 2, epi)
                    f.barrier()

        if "diff" not in skip:
            run_diff()
        if stop_after <= 4:
            return finish(nc, f, dram)

        TWO_PI = 2.0 * math.pi
        CW1 = 6.28125
        CW2 = TWO_PI - CW1

        def trig(st_t, pos_ap, t0, cosT, sinT):
            pi_, pf, ang, kk, ki, mm = st_t
            f.dma(SP, pi_[:], pos_ap[0:1, t0:t0 + 512].partition_broadcast(64), writes=[pi_], sb=pi_)
            f.op(V, lambda: nc.vector.tensor_copy(out=pf[:], in_=pi_[:]), reads=[pi_], writes=[pf])
            for which, outT in ((0, sinT), (1, cosT)):
                f.op(V, lambda: nc.vector.tensor_scalar(out=ang[:], in0=pf[:], scalar1=invf[:, 0:1], scalar2=(0.5 * math.pi if which else 0.0),
                                                          op0=ALU.mult, op1=ALU.add), reads=[pf, invf], writes=[ang])
                f.op(V, lambda: nc.vector.tensor_scalar(out=kk[:], in0=ang[:], scalar1=1.0 / TWO_PI, scalar2=None, op0=ALU.mult),
                     reads=[ang], writes=[kk])
                f.op(V, lambda: nc.vector.tensor_copy(out=ki[:], in_=kk[:]), reads=[kk], writes=[ki])
                f.op(V, lambda: nc.vector.tensor_copy(out=kk[:], in_=ki[:]), reads=[ki], writes=[kk])
                f.op(V, lambda: nc.vector.scalar_tensor_tensor(out=ang[:], in0=kk[:], scalar=-CW1, in1=ang[:], op0=ALU.mult, op1=ALU.add),
                     reads=[kk, ang], writes=[ang])
                f.op(V, lambda: nc.vector.scalar_tensor_tensor(out=ang[:], in0=kk[:], scalar=-CW2, in1=ang[:], op0=ALU.mult, op1=ALU.add),
                     reads=[kk, ang], writes=[ang])
                f.op(V, lambda: nc.vector.tensor_single_scalar(out=mm[:], in_=ang[:], scalar=math.pi, op=ALU.is_gt), reads=[ang], writes=[mm])
                f.op(V, lambda: nc.vector.scalar_tensor_tensor(out=ang[:], in0=mm[:], scalar=-TWO_PI, in1=ang[:], op0=ALU.mult, op1=ALU.add),
                     reads=[mm, ang], writes=[ang])
                f.op(V, lambda: nc.vector.tensor_single_scalar(out=mm[:], in_=ang[:], scalar=-math.pi, op=ALU.is_lt), reads=[ang], writes=[mm])
                f.op(V, lambda: nc.vector.scalar_tensor_tensor(out=ang[:], in0=mm[:], scalar=TWO_PI, in1=ang[:], op0=ALU.mult, op1=ALU.add),
                     reads=[mm, ang], writes=[ang])
                f.op(V, lambda: nc.vector.tensor_scalar(out=ang[:], in0=ang[:], scalar1=3.14159, scalar2=-3.14159, op0=ALU.min, op1=ALU.max),
                     reads=[ang], writes=[ang])
                f.op(A, lambda: nc.scalar.activation(out=outT[:], in_=ang[:], func=AF.Sin), reads=[ang], writes=[outT])

        def trig_tiles(st):
            return (tl(st, "pi_", [64, 512], I32), tl(st, "pf", [64, 512]), tl(st, "ang", [64, 512]), tl(st, "kk", [64, 512]),
                    tl(st, "ki", [64, 512], I32), tl(st, "mm", [64, 512]))

        def rotary(xf, xb, psR, cosT, sinT, t1, outb):
            f.op(A, lambda: nc.scalar.copy(out=xb[:], in_=xf[0:64, :]), reads=[xf], writes=[xb])
            f.op(P, lambda: nc.tensor.matmul(psR[0:64, :], lhsT=rotT[:], rhs=xb[:], start=True, stop=True), reads=[rotT, xb], writes=[psR], acc=True)
            f.op(V, lambda: nc.vector.tensor_tensor(out=t1[:], in0=xf[0:64, :], in1=cosT[:], op=ALU.mult), reads=[xf, cosT], writes=[t1])
            f.op(V, lambda: nc.vector.tensor_tensor(out=xf[0:64, :], in0=psR[0:64, :], in1=sinT[:], op=ALU.mult), reads=[psR, sinT], writes=[xf])
            f.op(V, lambda: nc.vector.tensor_tensor(out=outb[0:64, :], in0=t1[:], in1=xf[0:64, :], op=ALU.add), reads=[t1, xf], writes=[outb])

        def lat_norm(pb, pss, pans, ht, nch, gain_t, raw, sq, rstd, outb):
            for cch in range(nch):
                ps = pb[cch % 4]
                mm_fm(ps, 128, pans[cch // 4], (cch % 4) * 128, ht, KC)
                f.op(A, lambda: nc.scalar.activation(out=sq[:], in_=ps[:], func=AF.Square), reads=[ps], writes=[sq])
                f.op(P, lambda: nc.tensor.matmul(pss[:, :], lhsT=ones[:, :], rhs=sq[:], start=(cch == 0), stop=(cch == nch - 1)),
                     reads=[ones, sq], writes=[pss], acc=True)
                f.op(V, lambda: nc.vector.tensor_scalar(out=raw[:, cch, :], in0=ps[:], scalar1=gain_t[:, cch:cch + 1], scalar2=None, op0=ALU.mult),
                     reads=[ps, gain_t], writes=[raw], merge=True)
            rstd_from(pss, rstd, 1.0 / (nch * 128))
            f.op(V, lambda: nc.vector.tensor_tensor(out=outb[:, 0:nch, :], in0=raw[:, 0:nch, :],
                                                      in1=rstd[:].unsqueeze(1).to_broadcast([128, nch, 512]), op=ALU.mult),
                 reads=[raw, rstd], writes=[outb])

        cqnT_d = dscr("cqnT_d", [12, 128, TL], BF16)
        dram["cqnT_d"] = TT(None, "cqnT_d")
        dram_of[id(cqnT_d)] = dram["cqnT_d"]
        with ExitStack() as st:
            pans = [tl(st, f"pan{i}", [128, KC, 512], BF16) for i in range(3)]
            ht = tl(st, "ht", [128, KC, 512], BF16)
            raw = tl(st, "raw", [128, 12, 512])
            cqns = [tl(st, f"cqn{i}", [128, 12, 512], BF16) for i in range(1)] * 2
            sq = tl(st, "sq", [128, 512], BF16)
            rstd = tl(st, "rstd", [128, 512])
            pb = [f.ptile(st, f"pb{i}", [128, 512], F32) for i in range(6)]
            for i in range(3):
                load_panel(pans[i], w_in, 0, KC, C_CQ + i * 512, 512)
            if stop_after <= 4.25:
                f.barrier()
                return finish(nc, f, dram)
            for tt in range(NTT_OWN):
                cqn = cqns[tt % 2]
                load_hT(ht, hT_ownD, tt * 512)
                if stop_after <= 4.27:
                    f.barrier()
                    return finish(nc, f, dram)
                lat_norm(pb, pb[5], pans, ht, 12, cqg, raw, sq, rstd, cqn)
                if stop_after <= 4.29:
                    f.barrier()
                    return finish(nc, f, dram)
                f.dma(A, cqnT_d[:, :, tt * 512:(tt + 1) * 512].rearrange("c p t -> p c t"), cqn[:],
                      reads=[cqn], writes=[dram["cqnT_d"]], sb=cqn, merge=True)
            f.barrier()
        if stop_after <= 4.3:
            return finish(nc, f, dram)
        with ExitStack() as st:
            wq = tl(st, "wq", [128, 12, 2304], BF16)
            cqns = [tl(st, f"cqn{i}", [128, 12, 512], BF16) for i in range(2)]
            sq = tl(st, "sq", [128, 512], BF16)
            rstd2 = tl(st, "rstd2", [128, 512])
            cosT, sinT = tl(st, "cosT", [64, 512]), tl(st, "sinT", [64, 512])
            tt_ = trig_tiles(st)
            oAs = [tl(st, f"oA{i}", [128, 512], BF16) for i in range(2)]
            oBf = tl(st, "oBf", [64, 512])
            xb = tl(st, "xb", [64, 512], BF16)
            t1 = tl(st, "t1", [64, 512])
            oBs = [tl(st, f"oB{i}", [64, 512], BF16) for i in range(2)]
            pb = [f.ptile(st, f"pb{i}", [128, 512], F32) for i in range(8)]
            load_panel(wq, w_uq, 0, 12, 0, 2304)
            for tt in range(NTT_OWN):
                cqn = cqns[tt % 2]
                load_hT(cqn, cqnT_d, tt * 512, nk=12)
                trig(tt_, pos_own, tt * 512, cosT, sinT)
                for h in range(12):
                    psA, psB = pb[h % 2], pb[2 + h % 2]
                    oA, oB = oAs[h % 2], oBs[h % 2]
                    mm_fm(psA, 128, wq, h * 192, cqn, 12)
                    mm_fm(psB, 64, wq, h * 192 + 128, cqn, 12)
                    fm_rms((sq, pb[5], rstd2), [(psA, 128), (psB, 64)], 192, [mqgA[:, 0:1], mqgB[:, 0:1]], [(oA, 128), (oBf, 64)])
                    rotary(oBf, xb, pb[6], cosT, sinT, t1, oB)
                    f.dma(A, QmT[h, 0:128, tt * 512:(tt + 1) * 512], oA[:], reads=[oA], writes=[dram["QmT"]], sb=oA, merge=True)
                    f.dma(A, QmT[h, 128:192, tt * 512:(tt + 1) * 512], oB[:], reads=[oB], writes=[dram["QmT"]], sb=oB, merge=True)
            f.barrier()

        if stop_after <= 4.6:
            return finish(nc, f, dram)
        with ExitStack() as st:
            pan = tl(st, "pan", [128, KC, 576], BF16)
            hts = [tl(st, f"ht{i}", [128, KC, 512], BF16) for i in range(2)]
            wkv = tl(st, "wkv", [128, 4, 3072], BF16)
            raw = tl(st, "raw", [128, 4, 512])
            ckn = tl(st, "ckn", [128, 4, 512], BF16)
            sq = tl(st, "sq", [128, 512], BF16)
            sqk = tl(st, "sqk", [64, 512], BF16)
            rstd = tl(st, "rstd", [128, 512])
            rstd2 = tl(st, "rstd2", [128, 512])
            cosT, sinT = tl(st, "cosT", [64, 512]), tl(st, "sinT", [64, 512])
            tt_ = trig_tiles(st)
            krf = tl(st, "krf", [64, 512])
            krot = tl(st, "krot", [64, 512])
            krb = tl(st, "krb", [64, 512], BF16)
            xb = tl(st, "xb", [64, 512], BF16)
            t1 = tl(st, "t1", [64, 512])
            oAs = [tl(st, f"oA{i}", [128, 512], BF16) for i in range(2)]
            oBs = [tl(st, f"oB{i}", [64, 512], BF16) for i in range(2)]
            v1s = [tl(st, f"v1{i}", [128, 4, 129], BF16) for i in range(2)]
            pb = [f.ptile(st, f"pb{i}", [128, 512], F32) for i in range(8)]
            for v1 in v1s:
                f.op(V, lambda: nc.vector.memset(v1[:], 1.0), writes=[v1])
            load_panel(pan, w_in, 0, KC, C_CKV, 576)
            load_panel(wkv, w_ukv, 0, 4, 0, 3072)
            wkv_v = wkv[:].rearrange("p k (h c) -> p k h c", c=256)
            vi = 0
            for tt in range(NTT_ALL):
                ht = hts[tt % 2]
                load_hT(ht, hT_all, tt * 512)
                trig(tt_, pos_full, tt * 512, cosT, sinT)
                lat_norm(pb, pb[5], [pan], ht, 4, ckvg, raw, sq, rstd, ckn)
                mm_fm(pb[4], 64, pan, 512, ht, KC)
                f.op(A, lambda: nc.scalar.activation(out=sqk[:], in_=pb[4][0:64, :], func=AF.Square), reads=[pb[4]], writes=[sqk])
                f.op(V, lambda: nc.vector.tensor_scalar(out=krf[:], in0=pb[4][0:64, :], scalar1=mkgB[:, 0:1], scalar2=None, op0=ALU.mult),
                     reads=[pb[4], mkgB], writes=[krf])
                rotary(krf, xb, pb[6], cosT, sinT, t1, krb)
                f.op(V, lambda: nc.vector.tensor_copy(out=krot[:], in_=krb[:]), reads=[krb], writes=[krot])
                for h in range(12):
                    psN = pb[h % 2]
                    oA, oB = oAs[h % 2], oBs[h % 2]
                    mm_fm(psN, 128, wkv, h * 256, ckn, 4)
                    f.op(A, lambda: nc.scalar.activation(out=sq[:], in_=psN[:], func=AF.Square), reads=[psN], writes=[sq])
                    f.op(P, lambda: nc.tensor.matmul(pb[5][:, :], lhsT=ones[:, :], rhs=sq[:], start=True, stop=False),
                         reads=[ones, sq], writes=[pb[5]], acc=True)
                    f.op(P, lambda: nc.tensor.matmul(pb[5][:, :], lhsT=ones[0:64, :], rhs=sqk[:], start=False, stop=True),
                         reads=[ones, sqk], writes=[pb[5]], acc=True)
                    rstd_from(pb[5], rstd2, 1.0 / 192)
                    f.op(V, lambda: nc.vector.scalar_tensor_tensor(out=oA[:], in0=psN[:], scalar=mkgA[:, 0:1], in1=rstd2[:], op0=ALU.mult, op1=ALU.mult),
                         reads=[psN, mkgA, rstd2], writes=[oA])
                    f.op(V, lambda: nc.vector.tensor_tensor(out=oB[:], in0=krot[:], in1=rstd2[0:64, :], op=ALU.mult), reads=[krot, rstd2], writes=[oB])
                    f.dma(A, KmT[h, 0:128, tt * 512:(tt + 1) * 512], oA[:], reads=[oA], writes=[dram["KmT"]], sb=oA, merge=True)
                    f.dma(A, KmT[h, 128:192, tt * 512:(tt + 1) * 512], oB[:], reads=[oB], writes=[dram["KmT"]], sb=oB, merge=True)
                for tb in range(4):
                    for hg in range(3):
                        psV, v1 = pb[2 + (vi % 2)], v1s[vi % 2]
                        vi += 1
                        for k in range(4):
                            f.op(P, lambda: nc.tensor.matmul(psV[:, :].rearrange("p (h c) -> p h c", c=128), lhsT=ckn[:, k, tb * 128:(tb + 1) * 128],
                                                              rhs=wkv_v[:, k, hg * 4:(hg + 1) * 4, 128:256], start=(k == 0), stop=(k == 3)),
                                 reads=[ckn, wkv], writes=[psV], acc=True)
                        f.op(V, lambda: nc.vector.tensor_copy(out=v1[:, :, 0:128], in_=psV[:, :].rearrange("p (h c) -> p h c", c=128)),
                             reads=[psV], writes=[v1])
                        r0 = tt * 512 + tb * 128
                        f.dma(A, Vm1[hg * 4:(hg + 1) * 4, r0:r0 + 128, :].rearrange("h t c -> t h c"), v1[:],
                              reads=[v1], writes=[dram["Vm1"]], sb=v1, merge=True)
            f.barrier()
        if stop_after <= 5:
            return finish(nc, f, dram)

        def run_mla():
            for h in range(12):
                with ExitStack() as st:
                    KnT = tl(st, "KnT", [128, T], BF16)
                    KrT = tl(st, "KrT", [64, T], BF16)
                    QnT = tl(st, "QnT", [128, TL], BF16)
                    QrT = tl(st, "QrT", [64, TL], BF16)
                    V1 = tl(st, "V1", [128, T // 128, 129], BF16)
                    rr = tl(st, "rr", [128, 1])
                    ob = tl(st, "ob", [128, 128], BF16)
                    otb = tl(st, "otb", [128, 128], BF16)
                    tps = f.ptile(st, "tps", [128, 128], BF16)
                    f.dma(SP, KnT[:], KmT[h, 0:128, :], reads=[dram["KmT"]], writes=[KnT], sb=KnT)
                    f.dma(SP, KrT[:], KmT[h, 128:192, :], reads=[dram["KmT"]], writes=[KrT], sb=KrT)
                    f.dma(SP, QnT[:], QmT[h, 0:128, :], reads=[dram["QmT"]], writes=[QnT], sb=QnT)
                    f.dma(SP, QrT[:], QmT[h, 128:192, :], reads=[dram["QmT"]], writes=[QrT], sb=QrT)
                    f.dma(SP, V1[:], Vm1[h].rearrange("(b p) c -> p b c", p=128), reads=[dram["Vm1"]], writes=[V1], sb=V1)

                    def epi(j, u, O):
                        f.op(V, lambda: nc.vector.reciprocal(out=rr[:], in_=O[:, 128:129]), reads=[O], writes=[rr])
                        f.op(V, lambda: nc.vector.tensor_scalar(out=ob[:], in0=O[:, 0:128], scalar1=rr[:, 0:1], scalar2=None, op0=ALU.mult),
                             reads=[O, rr], writes=[ob])
                        f.op(P, lambda: nc.tensor.transpose(out=tps[:], in_=ob[:], identity=ident[:]), reads=[ob, ident], writes=[tps], acc=True)
                        f.op(V, lambda: nc.vector.tensor_copy(out=otb[:], in_=tps[:]), reads=[tps], writes=[otb])
                        f.dma(A, OT_d[12 + h, :, j * 128:(j + 1) * 128], otb[:], reads=[otb], writes=[dram["OT_d"]], sb=otb, merge=True)

                    def em_fn(u, j, kb):
                        i = kb - NCg * j
                        if i < 0:
                            return None
                        return MK[:, (i + 1) * 128:(i + 2) * 128]

                    attention(st, [[(QnT, 128), (QrT, 64)]], [[(KnT, 128), (KrT, 64)]], V1, 128,
                              lambda j: NCg * j + NCg, 192 ** -0.5, lambda u: None, em_fn, 1, epi)
                    f.barrier()

        if "mla" not in skip:
            run_mla()

        hT_memD = dscr("hT_memD", [KC, 128, NMEM], BF16)
        KeT = dscr("KeT", [4, 256, NMEM], BF16)
        Ve1 = dscr("Ve1", [4, NMEM, 257], BF16)
        for nm, ap in (("hT_memD", hT_memD), ("KeT", KeT), ("Ve1", Ve1)):
            dram[nm] = TT(None, nm)
            dram_of[id(ap)] = dram[nm]
        phase_norm(mem, NMEM // 128, memg, hT_memD, "hT_memD")
        pass_fm_rms(hT_memD, 1, 0, 8, 2, 256, lambda s_: memkg[:, s_:s_ + 1], KeT, "KeT",
                    lambda m: KeT[m // 2, (m % 2) * 128:(m % 2) * 128 + 128, :], W=mem_wkv, n=NMEM)
        with ExitStack() as st:
            pans = [tl(st, f"pan{i}", [128, KC, 512], BF16) for i in range(2)]
            ht = tl(st, "ht", [128, KC, NMEM], BF16)
            v1s = [tl(st, f"v1{i}", [128, 2, 257], BF16) for i in range(2)]
            psc = [f.ptile(st, f"psc{i}", [128, 512], F32) for i in range(2)]
            for v1 in v1s:
                f.op(V, lambda: nc.vector.memset(v1[:], 1.0), writes=[v1])
            load_hT(ht, hT_memD, 0, n=NMEM)
            vi = 0
            for pi in range(2):
                load_panel(pans[pi], mem_wkv, 0, KC, 1024 + pi * 512, 512)
                for tb in range(NMEM // 128):
                    ps, v1 = psc[vi % 2], v1s[vi % 2]
                    vi += 1
                    mm_tm(ps, ht, tb * 128, pans[pi], 0, 512, KC)
                    f.op(V, lambda: nc.vector.tensor_copy(out=v1[:, :, 0:256], in_=ps[:].rearrange("p (h c) -> p h c", h=2)),
                         reads=[ps], writes=[v1])
                    f.dma(A, Ve1[pi * 2:pi * 2 + 2, tb * 128:(tb + 1) * 128, :].rearrange("h t c -> t h c"), v1[:],
                          reads=[v1], writes=[dram["Ve1"]], sb=v1, merge=True)
            f.barrier()
        for e in range(4):
            with ExitStack() as st:
                KT = [tl(st, f"KT{i}", [128, NMEM], BF16) for i in range(2)]
                QT = [tl(st, f"QT{i}", [128, TL], BF16) for i in range(2)]
                V1 = tl(st, "V1", [128, NMEM // 128, 257], BF16)
                rr = tl(st, "rr", [128, 1])
                ob = tl(st, "ob", [128, 256], BF16)
                otb = tl(st, "otb", [128, 2, 128], BF16)
                tps = f.ptile(st, "tps", [128, 2, 128], BF16)
                for i in range(2):
                    f.dma(SP, KT[i][:], KeT[e, i * 128:(i + 1) * 128, :], reads=[dram["KeT"]], writes=[KT[i]], sb=KT[i])
                    f.dma(SP, QT[i][:], QeT[e, i * 128:(i + 1) * 128, :], reads=[dram["QeT"]], writes=[QT[i]], sb=QT[i])
                f.dma(SP, V1[:], Ve1[e].rearrange("(b p) c -> p b c", p=128), reads=[dram["Ve1"]], writes=[V1], sb=V1)

                def epi(j, u, O):
                    f.op(V, lambda: nc.vector.reciprocal(out=rr[:], in_=O[:, 256:257]), reads=[O], writes=[rr])
                    f.op(V, lambda: nc.vector.tensor_scalar(out=ob[:], in0=O[:, 0:256], scalar1=rr[:, 0:1], scalar2=None, op0=ALU.mult),
                         reads=[O, rr], writes=[ob])
                    for c2 in range(2):
                        f.op(P, lambda: nc.tensor.transpose(out=tps[:, c2, :], in_=ob[:, c2 * 128:(c2 + 1) * 128], identity=ident[:]),
                             reads=[ob, ident], writes=[tps], acc=True)
                    f.op(V, lambda: nc.vector.tensor_copy(out=otb[:], in_=tps[:]), reads=[tps], writes=[otb])
                    f.dma(A, OT_d[24 + 2 * e:26 + 2 * e, :, j * 128:(j + 1) * 128].rearrange("c p t -> p c t"), otb[:],
                          reads=[otb], writes=[dram["OT_d"]], sb=otb, merge=True)

                attention(st, [[(QT[0], 128), (QT[1], 128)]], [[(KT[0], 128), (KT[1], 128)]], V1, 256,
                          lambda j: NMEM // 128, 256 ** -0.5, lambda u: None, lambda u, j, kb: None, 1, epi)
                f.barrier()
        f.barrier()
        att_st.close()
        if stop_after <= 6:
            return finish(nc, f, dram)

        mixT_d = dscr("mixT_d", [KC, 128, TL], BF16)
        dram["mixT_d"] = TT(None, "mixT_d")
        dram_of[id(mixT_d)] = dram["mixT_d"]
        dram_of[id(OT_d)] = dram["OT_d"]
        with ExitStack() as st:
            pw = [[tl(st, f"pw{i}{b}", [128, nk_, 512], BF16) for b, nk_ in enumerate((12, 12, 8))] for i in range(2)]
            ots = [tl(st, f"ot{i}", [128, KC, 512], BF16) for i in range(2)]
            gts = [[tl(st, f"gt{i}{b}", [128, 512]) for b in range(3)] for i in range(2)]
            mx = tl(st, "mx", [128, 512])
            mbs = [tl(st, f"mb{i}", [128, 512], BF16) for i in range(2)]
            psc = [f.ptile(st, f"psc{i}", [128, 512], F32) for i in range(6)]
            it = 0
            gi = 0
            for dp in range(8):
                pws = pw[dp % 2]
                for b, (Wb, nk_) in enumerate(((w_o_diff, 12), (w_o_mla, 12), (w_o_mem, 8))):
                    load_panel(pws[b], Wb, 0, nk_, dp * 512, 512)
                for tt in range(NTT_OWN):
                    ot = ots[it % 2]
                    it += 1
                    load_hT(ot, OT_d, tt * 512)
                    for sc in range(4):
                        dc = dp * 4 + sc
                        gt = gts[gi % 2]
                        mb = mbs[gi % 2]
                        pss_ = psc[(gi % 2) * 3:(gi % 2) * 3 + 3]
                        gi += 1
                        for b, (k0, nk_) in enumerate(((0, 12), (12, 12), (24, 8))):
                            f.dma(SP, gt[b][:], gT[b * 32 + dc, :, tt * 512:(tt + 1) * 512], reads=[dram["gT"]], writes=[gt[b]], sb=gt[b])
                            for k in range(nk_):
                                f.op(P, lambda: nc.tensor.matmul(pss_[b][:, :], lhsT=pws[b][:, k, sc * 128:(sc + 1) * 128], rhs=ot[:, k0 + k, :],
                                                                  start=(k == 0), stop=(k == nk_ - 1)),
                                     reads=[pws[b], ot], writes=[pss_[b]], acc=True)
                        f.op(V, lambda: nc.vector.tensor_tensor(out=mx[:], in0=pss_[0][:], in1=gt[0][:], op=ALU.mult), reads=[pss_[0], gt[0]], writes=[mx])
                        f.op(V, lambda: nc.vector.tensor_tensor(out=gt[1][:], in0=pss_[1][:], in1=gt[1][:], op=ALU.mult), reads=[pss_[1], gt[1]], writes=[gt[1]])
                        f.op(V, lambda: nc.vector.tensor_tensor(out=gt[2][:], in0=pss_[2][:], in1=gt[2][:], op=ALU.mult), reads=[pss_[2], gt[2]], writes=[gt[2]])
                        f.op(G, lambda: nc.gpsimd.tensor_tensor(out=mx[:], in0=mx[:], in1=gt[1][:], op=ALU.add), reads=[mx, gt[1]], writes=[mx])
                        f.op(G, lambda: nc.gpsimd.tensor_tensor(out=mb[:], in0=mx[:], in1=gt[2][:], op=ALU.add), reads=[mx, gt[2]], writes=[mb])
                        f.dma(A, mixT_d[dc, :, tt * 512:(tt + 1) * 512], mb[:], reads=[mb], writes=[dram["mixT_d"]], sb=mb, merge=True)
            f.barrier()
        with ExitStack() as st:
            pans = [tl(st, f"pan{i}", [128, KC, 512], BF16) for i in range(2)]
            mts = [tl(st, f"mt{i}", [128, KC, 512], BF16) for i in range(2)]
            xts = [tl(st, f"xt{i}", [128, 512]) for i in range(2)]
            psc = [f.ptile(st, f"psc{i}", [128, 512], F32) for i in range(2)]
            it = 0
            xi = 0
            for dp in range(8):
                pan = pans[dp % 2]
                load_panel(pan, w_out, 0, KC, dp * 512, 512)
                for tt in range(NTT_OWN):
                    mt = mts[it % 2]
                    it += 1
                    load_hT(mt, mixT_d, tt * 512)
                    for tb in range(4):
                        ps, xt = psc[xi % 2], xts[xi % 2]
                        xi += 1
                        r0 = tt * 512 + tb * 128
                        f.dma(SP, xt[:], x_own[r0:r0 + 128, dp * 512:(dp + 1) * 512], writes=[xt], sb=xt)
                        mm_tm(ps, mt, tb * 128, pan, 0, 512, KC)
                        f.op(V, lambda: nc.vector.tensor_tensor(out=xt[:], in0=ps[:], in1=xt[:], op=ALU.add), reads=[ps, xt], writes=[xt])
                        f.dma(A, x1s[r0:r0 + 128, dp * 512:(dp + 1) * 512], xt[:], reads=[xt], writes=[dram["x1s"]], sb=xt, merge=True)
            f.barrier()
        if stop_after <= 7:
            return finish(nc, f, dram)

        RS = R_SLOT
        NSB = NSLOT // 128
        SBG = RS // 128
        h2T_d = dscr("h2T_d", [KC, 128, TL], BF16)
        h2gT_d = dscr("h2gT_d", [8, KC, 128, RS], BF16)
        actT_d = dscr("actT_d", [64, 4, 128, RS], BF16)
        for nm, ap in (("h2T_d", h2T_d), ("actT_d", actT_d), ("h2gT_d", h2gT_d)):
            dram[nm] = TT(None, nm)
            dram_of[id(ap)] = dram[nm]
        with ExitStack() as s0:
            Perm = tl(s0, "Perm", [128, NQB, NSLOT], BF16)
            gws = tl(s0, "gws", [128, NSB, 64])
            with ExitStack() as s1:
                xb_all = tl(s1, "xb_all", [128, NQB, D], BF16)
                with ExitStack() as st:
                    xts = [tl(st, f"xt{i}", [128, D]) for i in range(2)]
                    sss = [tl(st, f"ss{i}", [128, 1]) for i in range(2)]
                    junk = tl(st, "junk", [128, D], BF16)
                    hts = [tl(st, f"ht{i}", [128, KC, 128], BF16) for i in range(2)]
                    pts = [f.ptile(st, f"pt{i}", [128, 8, 128], BF16) for i in range(4)]
                    for i in range(NQB):
                        xt, ss, ht = xts[i % 2], sss[i % 2], hts[i % 2]
                        f.dma(SP, xt[:], x1s[i * 128:(i + 1) * 128, :], writes=[xt], sb=xt)
                        f.op(A, lambda: nc.scalar.activation(out=junk[:], in_=xt[:], func=AF.Square, accum_out=ss[:]), reads=[xt], writes=[junk, ss])
                        f.op(A, lambda: nc.scalar.activation(out=ss[:], in_=ss[:], func=AF.Sqrt, scale=1.0 / D, bias=epsb[:, 0:1]), reads=[ss, epsb], writes=[ss])
                        f.op(V, lambda: nc.vector.reciprocal(out=ss[:], in_=ss[:]), reads=[ss], writes=[ss])
                        f.op(A, lambda: nc.scalar.activation(out=xb_all[:, i, :], in_=xt[:], func=AF.Copy, scale=ss[:, 0:1]), reads=[xt, ss], writes=[xb_all], merge=True)
                        for q in range(4):
                            pt = pts[q]
                            for k in range(8):
                                c = q * 8 + k
                                f.op(P, lambda: nc.tensor.transpose(out=pt[:, k, :], in_=xb_all[:, i, c * 128:(c + 1) * 128], identity=ident[:]),
                                     reads=[xb_all, ident], writes=[pt], acc=True)
                            gb = ffng[:, q * 8:(q + 1) * 8].unsqueeze(2).to_broadcast([128, 8, 128])
                            f.op(V, lambda: nc.vector.tensor_tensor(out=ht[:, q * 8:(q + 1) * 8, :], in0=pt[:], in1=gb, op=ALU.mult),
                                 reads=[pt, ffng], writes=[ht], merge=True)
                        f.dma(A, h2T_d[:, :, i * 128:(i + 1) * 128].rearrange("c p t -> p c t"), ht[:], reads=[ht], writes=[dram["h2T_d"]], sb=ht, merge=True)
                    f.barrier()
                with ExitStack() as st:
                    wrt = tl(st, "wrt", [128, KC, 72], BF16)
                    brt = tl(st, "brt", [128, 72])
                    hbs = [tl(st, f"hb{i}", [128, KC, 128], BF16) for i in range(2)]
                    gw = tl(st, "gw", [128, NQB, 64])
                    gwh = tl(st, "gwh", [128, NQB, 64], BF16)
                    gwr = tl(st, "gwr", [128, NQB, 64])
                    gwl = tl(st, "gwl", [128, NQB, 64], BF16)
                    GmA = tl(st, "GmA", [128, NQB, 8])
                    Gmb = tl(st, "Gmb", [128, NQB, 8], BF16)
                    tri_f = tl(st, "tri_f", [128, 128])
                    tri_b = tl(st, "tri_b", [128, 128], BF16)
                    goff = tl(st, "goff", [128, 8])
                    iota = tl(st, "iota", [128, NSLOT])
                    slot = tl(st, "slot", [128, NQB])
                    tmp8 = tl(st, "tmp8", [128, 8])
                    lg = tl(st, "lg", [128, 72])
                    sm = tl(st, "sm", [128, 16])
                    t8 = tl(st, "t8", [128, 8])
                    ex8 = tl(st, "ex8", [128, 8])
                    lem = tl(st, "lem", [128, 64])
                    lem2 = tl(st, "lem2", [128, 64])
                    mk1 = tl(st, "mk1", [128, 64])
                    mk2 = tl(st, "mk2", [128, 64])
                    psr = f.ptile(st, "psr", [128, 512], F32)
                    psk = f.ptile(st, "psk", [128, 512], F32)
                    psg_ = [f.ptile(st, f"psgw{i}", [128, 512], F32) for i in range(2)]
                    load_panel(wrt, w_rt, 0, KC, 0, 72)
                    f.dma(SP, brt[:], b_rt[0:1, :].partition_broadcast(128), writes=[brt], sb=brt)
                    f.dma(SP, tri_f[:], c_tri, writes=[tri_f], sb=tri_f)
                    f.dma(SP, goff[:], c_goff, writes=[goff], sb=goff)
                    f.dma(SP, iota[:], c_iota, writes=[iota], sb=iota)
                    f.op(V, lambda: nc.vector.tensor_copy(out=tri_b[:], in_=tri_f[:]), reads=[tri_f], writes=[tri_b])
                    S = lambda i: sm[:, i:i + 1]
                    for tb in range(NQB):
                        hb = hbs[tb % 2]
                        load_hT(hb, h2T_d, tb * 128, n=128)
                        mm_tm(psr, hb, 0, wrt, 0, 72, KC)
                        f.op(V, lambda: nc.vector.tensor_tensor(out=lg[:], in0=psr[:, 0:72], in1=brt[:], op=ALU.add), reads=[psr, brt], writes=[lg])
                        f.op(V, lambda: nc.vector.reduce_max(out=S(0), in_=lg[:, 0:8], axis=AX.X), reads=[lg], writes=[sm])
                        f.op(V, lambda: nc.vector.tensor_scalar(out=GmA[:, tb, :], in0=lg[:, 0:8], scalar1=S(0), scalar2=None, op0=ALU.is_equal), reads=[lg, sm], writes=[GmA], merge=True)
                        f.op(V, lambda: nc.vector.tensor_scalar(out=S(1), in0=S(0), scalar1=-1.0, scalar2=None, op0=ALU.mult), reads=[sm], writes=[sm])
                        f.op(A, lambda: nc.scalar.activation(out=ex8[:], in_=lg[:, 0:8], func=AF.Exp, bias=S(1), accum_out=S(2)), reads=[lg, sm], writes=[ex8, sm])
                        f.op(V, lambda: nc.vector.reciprocal(out=S(3), in_=S(2)), reads=[sm], writes=[sm])
                        f.op(V, lambda: nc.vector.tensor_scalar(out=t8[:], in0=GmA[:, tb, :], scalar1=-1.0, scalar2=1e30, op0=ALU.add, op1=ALU.mult), reads=[GmA], writes=[t8])
                        f.op(V, lambda: nc.vector.tensor_tensor(out=lem[:].rearrange("p (g e) -> p g e", g=8), in0=lg[:, 8:72].rearrange("p (g e) -> p g e", g=8),
                                                                  in1=t8[:].unsqueeze(2).to_broadcast([128, 8, 8]), op=ALU.add), reads=[lg, t8], writes=[lem])
                        f.op(V, lambda: nc.vector.reduce_max(out=S(4), in_=lem[:], axis=AX.X), reads=[lem], writes=[sm])
                        f.op(V, lambda: nc.vector.tensor_scalar(out=mk1[:], in0=lem[:], scalar1=S(4), scalar2=None, op0=ALU.is_equal), reads=[lem, sm], writes=[mk1])
                        f.op(V, lambda: nc.vector.scalar_tensor_tensor(out=lem2[:], in0=mk1[:], scalar=-1e30, in1=lem[:], op0=ALU.mult, op1=ALU.add), reads=[mk1, lem], writes=[lem2])
                        f.op(V, lambda: nc.vector.reduce_max(out=S(5), in_=lem2[:], axis=AX.X), reads=[lem2], writes=[sm])
                        f.op(V, lambda: nc.vector.tensor_scalar(out=mk2[:], in0=lem2[:], scalar1=S(5), scalar2=None, op0=ALU.is_equal), reads=[lem2, sm], writes=[mk2])
                        f.op(V, lambda: nc.vector.tensor_scalar(out=S(6), in0=S(4), scalar1=-1.0, scalar2=None, op0=ALU.mult), reads=[sm], writes=[sm])
                        f.op(A, lambda: nc.scalar.activation(out=S(7), in_=S(5), func=AF.Exp, bias=S(6)), reads=[sm], writes=[sm])
                        f.op(V, lambda: nc.vector.tensor_scalar(out=S(8), in0=S(7), scalar1=1.0, scalar2=None, op0=ALU.add), reads=[sm], writes=[sm])
                        f.op(V, lambda: nc.vector.reciprocal(out=S(9), in_=S(8)), reads=[sm], writes=[sm])
                        f.op(V, lambda: nc.vector.tensor_tensor(out=S(10), in0=S(3), in1=S(9), op=ALU.mult), reads=[sm], writes=[sm])
                        f.op(V, lambda: nc.vector.tensor_tensor(out=S(11), in0=S(10), in1=S(7), op=ALU.mult), reads=[sm], writes=[sm])
                        f.op(V, lambda: nc.vector.tensor_scalar(out=gw[:, tb, :], in0=mk1[:], scalar1=S(10), scalar2=None, op0=ALU.mult), reads=[mk1, sm], writes=[gw], merge=True)
                        f.op(V, lambda: nc.vector.scalar_tensor_tensor(out=gw[:, tb, :], in0=mk2[:], scalar=S(11), in1=gw[:, tb, :], op0=ALU.mult, op1=ALU.add),
                             reads=[mk2, sm, gw], writes=[gw], merge=True)
                    f.op(V, lambda: nc.vector.tensor_copy(out=gwh[:], in_=gw[:]), reads=[gw], writes=[gwh])
                    f.op(V, lambda: nc.vector.tensor_tensor(out=gwr[:], in0=gw[:], in1=gwh[:], op=ALU.subtract), reads=[gw, gwh], writes=[gwr])
                    f.op(V, lambda: nc.vector.tensor_copy(out=gwl[:], in_=gwr[:]), reads=[gwr], writes=[gwl])
                    f.op(V, lambda: nc.vector.tensor_copy(out=Gmb[:], in_=GmA[:]), reads=[GmA], writes=[Gmb])
                    for tb in range(NQB):
                        f.op(P, lambda: nc.tensor.matmul(psk[:, 0:8], lhsT=tri_b[:], rhs=Gmb[:, tb, :], start=True, stop=(tb == 0)),
                             reads=[tri_b, Gmb], writes=[psk], acc=True)
                        for t2 in range(tb):
                            f.op(P, lambda: nc.tensor.matmul(psk[:, 0:8], lhsT=ones[:], rhs=Gmb[:, t2, :], start=False, stop=(t2 == tb - 1)),
                                 reads=[ones, Gmb], writes=[psk], acc=True)
                        f.op(V, lambda: nc.vector.tensor_tensor(out=tmp8[:], in0=psk[:, 0:8], in1=goff[:], op=ALU.add), reads=[psk, goff], writes=[tmp8])
                        f.op(V, lambda: nc.vector.tensor_tensor(out=tmp8[:], in0=tmp8[:], in1=GmA[:, tb, :], op=ALU.mult), reads=[tmp8, GmA], writes=[tmp8])
                        f.op(V, lambda: nc.vector.reduce_sum(out=slot[:, tb:tb + 1], in_=tmp8[:], axis=AX.X), reads=[tmp8], writes=[slot], merge=True)
                        f.op(V, lambda: nc.vector.tensor_scalar(out=Perm[:, tb, :], in0=iota[:], scalar1=slot[:, tb:tb + 1], scalar2=None, op0=ALU.is_equal),
                             reads=[iota, slot], writes=[Perm], merge=True)
                    for sb in range(NSB):
                        pg2 = psg_[sb % 2]
                        n_ = 0
                        for part in (gwh, gwl):
                            for tb in range(NQB):
                                f.op(P, lambda: nc.tensor.matmul(pg2[:, 0:64], lhsT=Perm[:, tb, sb * 128:(sb + 1) * 128], rhs=part[:, tb, :],
                                                                  start=(n_ == 0), stop=(n_ == 2 * NQB - 1)), reads=[Perm, part], writes=[pg2], acc=True)
                                n_ += 1
                        f.op(V, lambda: nc.vector.tensor_copy(out=gws[:, sb, :], in_=pg2[:, 0:64]), reads=[pg2], writes=[gws], merge=True)
                    f.barrier()
                with ExitStack() as st:
                    hgs = [tl(st, f"hg{i}", [128, 4, RS], BF16) for i in range(2)]
                    pps = [f.ptile(st, f"pp{i}", [128, 512], F32) for i in range(4)]
                    hi = 0
                    for g in range(8):
                        for c4 in range(0, KC, 4):
                            hg = hgs[hi % 2]
                            hi += 1
                            for cc in range(4):
                                c = c4 + cc
                                pp = pps[cc]
                                for tb in range(NQB):
                                    f.op(P, lambda: nc.tensor.matmul(pp[:, 0:RS], lhsT=xb_all[:, tb, c * 128:(c + 1) * 128], rhs=Perm[:, tb, g * RS:(g + 1) * RS],
                                                                      start=(tb == 0), stop=(tb == NQB - 1)), reads=[xb_all, Perm], writes=[pp], acc=True)
                                f.op(V, lambda: nc.vector.tensor_scalar(out=hg[:, cc, :], in0=pp[:, 0:RS], scalar1=ffng[:, c:c + 1], scalar2=None, op0=ALU.mult),
                                     reads=[pp, ffng], writes=[hg], merge=True)
                            f.dma(A, h2gT_d[g, c4:c4 + 4, :, :].rearrange("c p t -> p c t"), hg[:], reads=[hg], writes=[dram["h2gT_d"]], sb=hg, merge=True)
                    f.barrier()
            with ExitStack() as st:
                pans = [tl(st, f"pan{i}", [128, KC, 512], BF16) for i in range(3)]
                h2gs = [tl(st, f"h2g{i}", [128, KC, RS], BF16) for i in range(2)]
                sgs = [tl(st, f"sg{i}", [128, 512]) for i in range(2)]
                acts = [tl(st, f"act{i}", [128, 512], BF16) for i in range(2)]
                atbs = [tl(st, f"atb{i}", [128, 4, 128], BF16) for i in range(2)]
                psg = [f.ptile(st, f"psg{i}", [128, 512], F32) for i in range(2)]
                psu = [f.ptile(st, f"psu{i}", [128, 512], F32) for i in range(2)]
                tpss = [f.ptile(st, f"tpa{i}", [128, 4, 128], BF16) for i in range(2)]
                pi_ = 0
                ci = 0
                for g in range(8):
                    h2g = h2gs[g % 2]
                    f.dma(SP, h2g[:], h2gT_d[g].rearrange("c p t -> p c t"), writes=[h2g], sb=h2g)
                    for el in range(8):
                        e = g * 8 + el
                        pg_, pu_ = pans[pi_ % 3], pans[(pi_ + 1) % 3]
                        pi_ += 2
                        load_panel(pg_, w_eg[e], 0, KC, 0, 512)
                        load_panel(pu_, w_eu[e], 0, KC, 0, 512)
                        for sl in range(SBG):
                            sb = g * SBG + sl
                            pG, pU, sg, act, atb, tpa = psg[ci % 2], psu[ci % 2], sgs[ci % 2], acts[ci % 2], atbs[ci % 2], tpss[ci % 2]
                            ci += 1
                            mm_tm(pG, h2g, sl * 128, pg_, 0, 512, KC)
                            mm_tm(pU, h2g, sl * 128, pu_, 0, 512, KC)
                            f.op(A, lambda: nc.scalar.activation(out=sg[:], in_=pG[:], func=AF.Silu), reads=[pG], writes=[sg])
                            f.op(V, lambda: nc.vector.scalar_tensor_tensor(out=act[:], in0=pU[:], scalar=gws[:, sb, e:e + 1], in1=sg[:], op0=ALU.mult, op1=ALU.mult),
                                 reads=[pU, gws, sg], writes=[act])
                            for k in range(4):
                                f.op(P, lambda: nc.tensor.transpose(out=tpa[:, k, :], in_=act[:, k * 128:(k + 1) * 128], identity=ident[:]),
                                     reads=[act, ident], writes=[tpa], acc=True)
                            f.op(V, lambda: nc.vector.tensor_copy(out=atb[:], in_=tpa[:]), reads=[tpa], writes=[atb])
                            f.dma(A, actT_d[e, :, :, sl * 128:(sl + 1) * 128].rearrange("c p t -> p c t"), atb[:],
                                  reads=[atb], writes=[dram["actT_d"]], sb=atb, merge=True)
                f.barrier()
            if stop_after <= 8:
                return finish(nc, f, dram)
            with ExitStack() as st:
                PermT = tl(st, "PermT", [128, NSB, TL], BF16)
                ysh = tl(st, "ysh", [128, NSB, 512], BF16)
                ysl = tl(st, "ysl", [128, NSB, 512], BF16)
                ysr = tl(st, "ysr", [128, 512])
                wds = [tl(st, f"wd{i}", [128, 4, 512], BF16) for i in range(3)]
                ats = [tl(st, f"at{i}", [128, 4, RS], BF16) for i in range(3)]
                xts = [tl(st, f"xt{i}", [128, 512]) for i in range(2)]
                pys = [f.ptile(st, f"py{i}", [128, 512], F32) for i in range(4)]
                pos_ = [f.ptile(st, f"po{i}", [128, 512], F32) for i in range(2)]
                ptp = [f.ptile(st, f"ptp{i}", [128, 8, 128], BF16) for i in range(2)]
                for sb in range(NSB):
                    pt_ = ptp[sb % 2]
                    for tb in range(NQB):
                        f.op(P, lambda: nc.tensor.transpose(out=pt_[:, tb, :], in_=Perm[:, tb, sb * 128:(sb + 1) * 128], identity=ident[:]),
                             reads=[Perm, ident], writes=[pt_], acc=True)
                    f.op(V, lambda: nc.vector.tensor_copy(out=PermT[:, sb, :], in_=pt_[:].rearrange("p a b -> p (a b)")), reads=[pt_], writes=[PermT], merge=True)
                wi = 0
                xi = 0
                for dp in range(8):
                    for g in range(8):
                        py = pys[(g % 2) * SBG:(g % 2) * SBG + SBG]
                        for el in range(8):
                            e = g * 8 + el
                            wd, at = wds[wi % 3], ats[wi % 3]
                            wi += 1
                            load_panel(wd, w_ed, e * 512, 4, dp * 512, 512)
                            f.dma(SP, at[:], actT_d[e].rearrange("c p t -> p c t"), writes=[at], sb=at)
                            for sl in range(SBG):
                                for k in range(4):
                                    f.op(P, lambda: nc.tensor.matmul(py[sl][:, :], lhsT=at[:, k, sl * 128:(sl + 1) * 128], rhs=wd[:, k, :],
                                                                      start=(el == 0 and k == 0), stop=(el == 7 and k == 3)),
                                         reads=[at, wd], writes=[py[sl]], acc=True)
                        for sl in range(SBG):
                            sb = g * SBG + sl
                            f.op(A, lambda: nc.scalar.copy(out=ysh[:, sb, :], in_=py[sl][:]), reads=[py[sl]], writes=[ysh], merge=True)
                            f.op(V, lambda: nc.vector.tensor_tensor(out=ysr[:], in0=py[sl][:], in1=ysh[:, sb, :], op=ALU.subtract), reads=[py[sl], ysh], writes=[ysr])
                            f.op(V, lambda: nc.vector.tensor_copy(out=ysl[:, sb, :], in_=ysr[:]), reads=[ysr], writes=[ysl], merge=True)
                    for tb in range(NQB):
                        po, xt = pos_[xi % 2], xts[xi % 2]
                        xi += 1
                        f.dma(SP, xt[:], x1s[tb * 128:(tb + 1) * 128, dp * 512:(dp + 1) * 512], writes=[xt], sb=xt)
                        n_ = 0
                        for part in (ysh, ysl):
                            for sb in range(NSB):
                                f.op(P, lambda: nc.tensor.matmul(po[:, :], lhsT=PermT[:, sb, tb * 128:(tb + 1) * 128], rhs=part[:, sb, :],
                                                                  start=(n_ == 0), stop=(n_ == 2 * NSB - 1)), reads=[PermT, part], writes=[po], acc=True)
                                n_ += 1
                        f.op(V, lambda: nc.vector.tensor_tensor(out=xt[:], in0=po[:], in1=xt[:], op=ALU.add), reads=[po, xt], writes=[xt])
                        f.dma(A, out[tb * 128:(tb + 1) * 128, dp * 512:(dp + 1) * 512], xt[:], reads=[xt], writes=[dram["out"]], sb=xt, merge=True)
                f.barrier()
        return finish(nc, f, dram)

    return nc


def finish(nc, f, dram):
    deps = {}
    for b in f.dma_bufs:
        if b.dcount > 0:
            deps[id(b.dsem)] = (b.dsem, b.dcount)
    for (s_, c_) in f.sem_pool + f.retired:
        deps[id(s_)] = (s_, c_)
    for e in f.engs:
        if e.count > 0:
            deps[id(e.sem)] = (e.sem, e.count)
    f._wait(f.sp, deps)
    return nc


def host_consts():
    ident = np.eye(128, dtype=np.float32)
    rot = np.zeros((64, 64), np.float32)
    for m in range(32):
        rot[m + 32, m] = -1.0
    for m in range(32, 64):
        rot[m - 32, m] = 1.0
    tri = np.triu(np.ones((128, 128), np.float32), 1)
    iota = np.broadcast_to(np.arange(NSLOT, dtype=np.float32), (128, NSLOT)).copy()
    half = 32
    inv = (np.float32(10000.0) ** (-np.arange(half, dtype=np.float32) / np.float32(half))).astype(np.float32)
    invf = np.concatenate([inv, inv]).reshape(64, 1).astype(np.float32)
    goff = np.broadcast_to(np.arange(8, dtype=np.float32) * R_SLOT, (128, 8)).copy()
    return {"c_goff": goff, "c_ident": ident, "c_rot": rot, "c_tri": tri, "c_iota": iota, "c_invf": invf}


def chunked(v, nch):
    return np.ascontiguousarray(np.asarray(v, np.float32).reshape(nch, 128).T)


def make_in_maps(inp, cores):
    a = {k: np.asarray(v) for k, v in inp.items()}
    x = a["x"][0]
    pos = a["positions"][0].astype(np.int32)
    shared = {
        "x_full": x,
        "mem": a["mem"][0],
        "pos_full": pos.reshape(1, T),
        "rel_bias": a["rel_bias"].reshape(1, 32 * 12),
        "mix_norm_g": chunked(a["mix_norm_g"][0], KC),
        "w_in": a["w_in"][0],
        "diff_q_norm_g": a["diff_q_norm_g"][0].reshape(128, 1),
        "diff_k_norm_g": a["diff_k_norm_g"][0].reshape(128, 1),
        "lam_v": np.concatenate([a["diff_lambda_q1"][0], a["diff_lambda_k1"][0], a["diff_lambda_q2"][0],
                                 a["diff_lambda_k2"][0]]).reshape(1, 512),
        "diff_subln_g": a["diff_subln_g"][0].reshape(1, 256),
        "mla_cq_norm_g": chunked(a["mla_cq_norm_g"][0], 12),
        "mla_ckv_norm_g": chunked(a["mla_ckv_norm_g"][0], 4),
        "mla_w_uq": a["mla_w_uq"][0],
        "mla_w_ukv": a["mla_w_ukv"][0],
        "mla_q_norm_g": a["mla_q_norm_g"][0].reshape(192, 1),
        "mla_k_norm_g": a["mla_k_norm_g"][0].reshape(192, 1),
        "mem_norm_g": chunked(a["mem_norm_g"][0], KC),
        "mem_w_kv": a["mem_w_kv"][0],
        "mem_q_norm_g": chunked(a["mem_q_norm_g"][0], 2),
        "mem_k_norm_g": chunked(a["mem_k_norm_g"][0], 2),
        "w_o_diff": a["w_o_diff"][0],
        "w_o_mla": a["w_o_mla"][0],
        "w_o_mem": a["w_o_mem"][0],
        "w_out": a["w_out"][0],
        "ffn_norm_g": chunked(a["ffn_norm_g"][0], KC),
        "w_rt": np.ascontiguousarray(np.concatenate([a["w_route_group"][0], a["w_route_expert"][0]], axis=1)),
        "b_rt": np.concatenate([a["b_route_group"][0], a["b_route_expert"][0]]).reshape(1, 72),
        "w_exp_gate": a["w_exp_gate"][0],
        "w_exp_up": a["w_exp_up"][0],
        "w_exp_down": a["w_exp_down"][0].reshape(64 * 512, D),
    }
    shared.update(host_consts())
    maps = []
    xb = x.reshape(T // 128, 128, D)
    pb = pos.reshape(T // 128, 128)
    for c in cores:
        m = dict(shared)
        m["x_own"] = np.ascontiguousarray(xb[c::NCORES].reshape(TL, D))
        m["pos_own"] = np.ascontiguousarray(pb[c::NCORES].reshape(1, TL))
        maps.append(m)
    return maps


def kernel(**inputs):
    nc = build()
    cores = list(range(NCORES))
    in_maps = make_in_maps(inputs, cores)
    res = run_bass_kernel_spmd(nc, in_maps, core_ids=cores)
    outb = np.zeros((T // 128, 128, D), np.float32)
    for c in cores:
        outb[c::NCORES] = np.asarray(res.results[c]["out"]).reshape(TL // 128, 128, D)
    return outb.reshape(1, T, D)
```
